# Optimizing a Trainium2 kernel written in Bass

```python
import math
import jax, jax.numpy as jnp
from jax import lax
import numpy as np

D_MODEL = 1024
BATCH = 32
SEQ = 2048
DEPTH = 1

CHUNK = 64
Q_BLOCK = 128
HEAD_DIM = 64
MIX_WIDTH = D_MODEL
A_WIDTH = MIX_WIDTH // 2
B_WIDTH = MIX_WIDTH - A_WIDTH
A_HEADS = A_WIDTH // HEAD_DIM
B_HEADS = B_WIDTH // HEAD_DIM
KV_LATENT = 128
IDX_HEADS = 4
IDX_DIM = 32
TOPK_MAX = 256
NUM_BUCKETS = 32
MAX_DISTANCE = 128
ROPE_BASE = 10000.0
RMS_EPS = 1e-6
GN_EPS = 1e-5
FFN_HIDDEN = ((-(-(8 * D_MODEL) // 3) + 255) // 256) * 256

IN_SIZES = [A_WIDTH, KV_LATENT, IDX_HEADS * IDX_DIM, IDX_DIM, IDX_HEADS,
            B_WIDTH, B_WIDTH, B_WIDTH, B_WIDTH]
IN_WIDTH = sum(IN_SIZES)
IN_OFFSETS = [int(o) for o in np.cumsum(IN_SIZES)[:-1]]

kernel_name = "hybrid_dsa_retention_chunk_causal"


def rmsnorm(x, w):
    xf = x.astype(jnp.float32)
    y = xf * lax.rsqrt(jnp.mean(xf * xf, axis=-1, keepdims=True) + RMS_EPS)
    return (y * w.astype(jnp.float32)).astype(x.dtype)


def t5_bucket(rel):
    half = NUM_BUCKETS // 2
    max_exact = half // 2
    ret = jnp.where(rel > 0, half, 0)
    n = jnp.abs(rel)
    nf = jnp.maximum(n, 1).astype(jnp.float32)
    large = max_exact + (jnp.log(nf / max_exact) / math.log(MAX_DISTANCE / max_exact)
                         * (half - max_exact)).astype(jnp.int32)
    large = jnp.minimum(large, half - 1)
    return ret + jnp.where(n < max_exact, n, large)


def rotary(x, pos):
    half = x.shape[-1] // 2
    inv = ROPE_BASE ** (-jnp.arange(half, dtype=jnp.float32) / half)
    ang = pos.astype(jnp.float32)[:, None] * inv[None, :]
    cos = jnp.cos(ang)[:, None, :]
    sin = jnp.sin(ang)[:, None, :]
    x1, x2 = x[..., :half], x[..., half:]
    return jnp.concatenate([x1 * cos - x2 * sin, x1 * sin + x2 * cos], axis=-1)


def indexed_sparse_attention(q, k, v, iq, ik, iw, rel_bias):
    b, s, h, dh = q.shape
    topk = min(TOPK_MAX, s // 4)
    n_blocks = s // Q_BLOCK
    key_pos = jnp.arange(s)
    idx_scale = IDX_DIM ** -0.5
    w_scale = IDX_HEADS ** -0.5

    def block(i):
        t0 = i * Q_BLOCK
        qb = lax.dynamic_slice_in_dim(q, t0, Q_BLOCK, axis=1)
        iqb = lax.dynamic_slice_in_dim(iq, t0, Q_BLOCK, axis=1)
        iwb = lax.dynamic_slice_in_dim(iw, t0, Q_BLOCK, axis=1)
        qpos = t0 + jnp.arange(Q_BLOCK)
        limit = (qpos // CHUNK + 1) * CHUNK
        admissible = key_pos[None, :] < limit[:, None]
        dots = jnp.einsum('bthd,bsd->bths', iqb, ik).astype(jnp.float32) * idx_scale
        score = jnp.einsum('bth,bths->bts', iwb.astype(jnp.float32) * w_scale,
                           jax.nn.relu(dots))
        score = jnp.where(admissible[None], score, -jnp.inf)
        _, idx = lax.top_k(score, topk)
        valid = idx < limit[None, :, None]
        kg = jax.vmap(lambda kb, ib: kb[ib])(k, idx)
        vg = jax.vmap(lambda vb, ib: vb[ib])(v, idx)
        logits = jnp.einsum('bthd,btkd->bhtk', qb, kg).astype(jnp.float32) * dh ** -0.5
        bias = rel_bias[t5_bucket(idx - qpos[None, :, None])]
        logits = logits + jnp.transpose(bias, (0, 3, 1, 2)).astype(jnp.float32)
        logits = jnp.where(valid[:, None], logits, -jnp.inf)
        p = jax.nn.softmax(logits, axis=-1)
        return jnp.einsum('bhtk,btkd->bthd', p.astype(v.dtype), vg)

    outs = lax.map(block, jnp.arange(n_blocks))
    return jnp.moveaxis(outs, 0, 1).reshape(b, s, h * dh)


def chunkwise_retention(q, k, v):
    b, s, h, dh = q.shape
    nc = s // CHUNK
    log_g = jnp.log1p(-(2.0 ** (-5.0 - jnp.arange(h, dtype=jnp.float32))))
    j = jnp.arange(CHUNK, dtype=jnp.float32)
    rel = j[:, None] - j[None, :]
    inner_decay = jnp.where(rel[None] >= 0,
                            jnp.exp(jnp.maximum(rel, 0.0)[None] * log_g[:, None, None]), 0.0)
    query_decay = jnp.exp((j + 1.0)[:, None] * log_g[None, :])[None, :, :, None]
    key_decay = jnp.exp((CHUNK - 1.0 - j)[:, None] * log_g[None, :])[None, :, :, None]
    chunk_decay = jnp.exp(CHUNK * log_g)[None, :, None, None]

    def to_chunks(t):
        return jnp.transpose(t.reshape(b, nc, CHUNK, h, dh), (1, 0, 2, 3, 4))

    def step(state, xs):
        qc, kc, vc = xs
        scores = jnp.einsum('bihd,bjhd->bhij', qc, kc) * inner_decay[None]
        inner = jnp.einsum('bhij,bjhd->bihd', scores, vc)
        cross = jnp.einsum('bihd,bhde->bihe', qc, state) * query_decay
        new_state = state * chunk_decay + jnp.einsum('bjhd,bjhe->bhde', kc * key_decay, vc)
        return new_state, inner + cross

    state0 = jnp.zeros((b, h, dh, dh), jnp.float32)
    _, ys = lax.scan(step, state0, (to_chunks(q), to_chunks(k), to_chunks(v)))
    return jnp.transpose(ys, (1, 0, 2, 3, 4)).reshape(b, s, h, dh)


def setup_inputs(seed: int = 0) -> dict:
    key = jax.random.key(seed)
    ks = jax.random.split(key, 20)
    f32 = jnp.float32

    def nrm(k, shape, fan_in):
        return jax.random.normal(k, shape, f32) * fan_in ** -0.5

    def gain(k, shape):
        return 1.0 + 0.02 * jax.random.normal(k, shape, f32)

    return {
        "x": jax.random.normal(ks[0], (BATCH, SEQ, D_MODEL), f32),
        "norm1_w": gain(ks[1], (DEPTH, D_MODEL)),
        "w_in": nrm(ks[2], (DEPTH, D_MODEL, IN_WIDTH), D_MODEL),
        "q_norm_w": gain(ks[3], (DEPTH, HEAD_DIM)),
        "k_norm_w": gain(ks[4], (DEPTH, HEAD_DIM)),
        "kv_norm_w": gain(ks[5], (DEPTH, KV_LATENT)),
        "w_uk": nrm(ks[6], (DEPTH, KV_LATENT, HEAD_DIM), KV_LATENT),
        "w_uv": nrm(ks[7], (DEPTH, KV_LATENT, HEAD_DIM), KV_LATENT),
        "rel_bias": 0.1 * jax.random.normal(ks[8], (NUM_BUCKETS, A_HEADS), f32),
        "ret_gn_w": gain(ks[9], (DEPTH, B_WIDTH)),
        "ret_gn_b": 0.02 * jax.random.normal(ks[10], (DEPTH, B_WIDTH), f32),
        "w_out": nrm(ks[11], (DEPTH, MIX_WIDTH, D_MODEL), MIX_WIDTH),
        "norm2_w": gain(ks[12], (DEPTH, D_MODEL)),
        "w_gate": nrm(ks[13], (DEPTH, D_MODEL, FFN_HIDDEN), D_MODEL),
        "w_up": nrm(ks[14], (DEPTH, D_MODEL, FFN_HIDDEN), D_MODEL),
        "w_down": nrm(ks[15], (DEPTH, FFN_HIDDEN, D_MODEL), FFN_HIDDEN),
    }


def reference(x, norm1_w, w_in, q_norm_w, k_norm_w, kv_norm_w, w_uk, w_uv, rel_bias,
              ret_gn_w, ret_gn_b, w_out, norm2_w, w_gate, w_up, w_down):
    b, s, _ = x.shape
    pos = jnp.arange(s)
    for l in range(DEPTH):
        h = rmsnorm(x, norm1_w[l])
        z = h @ w_in[l]
        qa, c_kv, iq, ik, iw, qb, kb, vb, gb = jnp.split(z, IN_OFFSETS, axis=-1)

        qa = rmsnorm(qa.reshape(b, s, A_HEADS, HEAD_DIM), q_norm_w[l])
        c = rmsnorm(c_kv, kv_norm_w[l])
        ka = rmsnorm(c @ w_uk[l], k_norm_w[l])
        va = c @ w_uv[l]
        out_a = indexed_sparse_attention(qa, ka, va,
                                         iq.reshape(b, s, IDX_HEADS, IDX_DIM), ik, iw, rel_bias)

        qr = rotary(qb.reshape(b, s, B_HEADS, HEAD_DIM).astype(jnp.float32), pos)
        kr = rotary(kb.reshape(b, s, B_HEADS, HEAD_DIM).astype(jnp.float32), pos) * HEAD_DIM ** -0.5
        vr = vb.reshape(b, s, B_HEADS, HEAD_DIM).astype(jnp.float32)
        y = chunkwise_retention(qr, kr, vr)
        mu = jnp.mean(y, axis=-1, keepdims=True)
        var = jnp.mean(jnp.square(y - mu), axis=-1, keepdims=True)
        y = ((y - mu) * lax.rsqrt(var + GN_EPS)).reshape(b, s, B_WIDTH)
        y = y * ret_gn_w[l].astype(jnp.float32) + ret_gn_b[l].astype(jnp.float32)
        out_b = (jax.nn.silu(gb.astype(jnp.float32)) * y).astype(x.dtype)

        x = x + jnp.concatenate([out_a, out_b], axis=-1) @ w_out[l]

        h2 = rmsnorm(x, norm2_w[l])
        x = x + (jax.nn.silu(h2 @ w_gate[l]) * (h2 @ w_up[l])) @ w_down[l]
    return x
```

```python
import contextlib
import math
import numpy as np
import concourse.bass as bass
import concourse.mybir as mybir
from concourse.bass_utils import run_bass_kernel_spmd

F32 = mybir.dt.float32
BF16 = mybir.dt.bfloat16
AF = mybir.ActivationFunctionType
ALU = mybir.AluOpType
AX = mybir.AxisListType

D_MODEL = 1024
HID = 2816
NHC = HID // 128
IN_W = 2852
RMS_EPS = 1e-6
GN_EPS = 1e-5
KIT = 20
CW = 3328
NEG = -3.0e38

ENGS = ("pe", "act", "dve", "pool", "sp")
CH = 20000
DCH = 1500


class Prog:
    def __init__(self, nc):
        self.nc = nc
        self.q = {e: [] for e in ENGS}
        self.n = {e: 0 for e in ENGS}
        self.seen = {e: {} for e in ENGS}
        self.lastw = {}
        self.readers = {}
        self.dcount = {}
        self.dbatch = {}
        self.dopen = {}

    def _deps(self, eng, reads, writes):
        deps = {}

        def add(src, idx, raw):
            if src == eng and eng in ("pe", "sp"):
                return
            if deps.get(src, -1) < idx:
                deps[src] = idx

        for r in reads:
            if r in self.lastw:
                add(*self.lastw[r], True)
        for w in writes:
            if w in self.lastw:
                add(*self.lastw[w], False)
            for (s, i) in self.readers.get(w, ()):
                add(s, i, False)
        out = []
        for src, idx in deps.items():
            if self.seen[eng].get(src, -1) >= idx:
                continue
            self.seen[eng][src] = idx
            out.append((src, idx))
        return out

    def _commit(self, sig, reads, writes):
        for r in reads:
            self.readers.setdefault(r, []).append(sig)
        for w in writes:
            self.lastw[w] = sig
            self.readers[w] = []

    def op(self, eng, fn, reads=(), writes=()):
        waits = self._deps(eng, reads, writes)
        idx = self.n[eng]
        self.n[eng] += 1
        self.q[eng].append(("op", fn, waits, idx))
        self._commit((eng, idx), reads, writes)

    def dma(self, queue, fn, stream, reads=(), writes=(), batch=False):
        waits = self._deps(queue, reads, writes)
        idx = self.dcount.get(stream, 0)
        self.dcount[stream] = idx + 1
        self.dbatch.setdefault(stream, []).append(None)
        self.dopen.setdefault(stream, []).append(idx)
        if not batch:
            self.close(stream)
        self.q[queue].append(("dma", fn, waits, (stream, idx)))
        self._commit(("dma:" + stream, idx), reads, writes)

    def close(self, stream):
        ids = self.dopen.get(stream, [])
        if ids:
            for i in ids:
                self.dbatch[stream][i] = ids[-1]
            self.dopen[stream] = []

    def emit(self):
        nc = self.nc
        for s in list(self.dopen):
            self.close(s)
        with contextlib.ExitStack() as st:
            sems = {}
            for e in ENGS:
                if e == "sp":
                    continue
                ne = max(1, (self.n[e] + CH - 1) // CH)
                sems[e] = [st.enter_context(nc.semaphore(f"s_{e}_{k}")) for k in range(ne)]
            for s, c in self.dcount.items():
                ne = max(1, (c + DCH - 1) // DCH)
                sems["dma:" + s] = [st.enter_context(nc.semaphore(f"d_{s}_{k}")) for k in range(ne)]

            def wait_args(src, idx):
                if src.startswith("dma:"):
                    end = self.dbatch[src[4:]][idx]
                    return sems[src][end // DCH], 16 * (end % DCH + 1)
                return sems[src][idx // CH], idx % CH + 1

            def run(name, eng):
                for kind, fn, waits, info in self.q[name]:
                    for (src, idx) in waits:
                        sem, val = wait_args(src, idx)
                        eng.wait_ge(sem, val)
                    ins = fn(eng)
                    if kind == "op":
                        ins.then_inc(sems[name][info // CH], 1)
                    else:
                        stream, idx = info
                        ins.then_inc(sems["dma:" + stream][idx // DCH], 16)
                if name == "sp":
                    for s, c in self.dcount.items():
                        sem, val = wait_args("dma:" + s, c - 1)
                        eng.wait_ge(sem, val)

            with nc.Block() as block:
                @block.tensor
                def _(eng):
                    run("pe", eng)

                @block.scalar
                def _(eng):
                    run("act", eng)

                @block.vector
                def _(eng):
                    run("dve", eng)

                @block.gpsimd
                def _(eng):
                    run("pool", eng)

                @block.sync
                def _(eng):
                    run("sp", eng)


def build(nc, S, NSEQ, KTOP):
    NT = S // 128
    GT = min(4, NT)
    NG = NT // GT
    LS = 2 if GT >= 2 else 1
    P = Prog(nc)

    def din(name, shape, dt=F32):
        return nc.dram_tensor(name, list(shape), dt, kind="ExternalInput").ap()

    x_d = din("x", [NSEQ * S, D_MODEL])
    w_in_d = din("w_in", [D_MODEL, IN_W])
    w_out_d = din("w_out", [D_MODEL, D_MODEL])
    w_gate_d = din("w_gate", [D_MODEL, HID])
    w_up_d = din("w_up", [D_MODEL, HID])
    w_down_d = din("w_down", [HID, D_MODEL])
    cst_d = din("cst", [128, CW])
    bias_d = din("bias_t", [3, 128, 1024])
    tb_d = din("tb", [128, S])
    rot_d = din("rot", [NT, 128, 128])
    y_d = nc.dram_tensor("y", [NSEQ * S, D_MODEL], F32, kind="ExternalOutput").ap()
    wg_s = nc.dram_tensor("wg_s", [128, NHC, 8, 128], BF16, kind="Internal").ap()
    wu_s = nc.dram_tensor("wu_s", [128, NHC, 8, 128], BF16, kind="Internal").ap()
    wd_s = nc.dram_tensor("wd_s", [NHC, 128, D_MODEL], BF16, kind="Internal").ap()

    def sb(name, shape, dt):
        return nc.alloc_sbuf_tensor(name, list(shape), dt)

    Win = sb("Win", [128, 8, IN_W], BF16)
    Wout = sb("Wout", [128, 8, D_MODEL], BF16)
    Wukv = sb("Wukv", [128, 128], BF16)
    CST = sb("CST", [128, CW], F32)
    c_off = {}
    o = 0
    for nm, w in [("ident", 128), ("n1w", 8), ("n2w", 8), ("kvnw", 1), ("qnw", 64), ("knw", 64),
                  ("gnw", 512), ("gnb", 512), ("wuk", 64), ("wuv", 64), ("DQ", 512), ("DK", 512),
                  ("DKtok", 8), ("DMASK", 128), ("GC", 4), ("BM", 512), ("LASTM", 128), ("pw", 32),
                  ("negh", 8)]:
        c_off[nm] = (o, w)
        o += w
    assert o <= CW

    def C(nm, a=0, b=None):
        o0, w = c_off[nm]
        return CST[:, o0 + a: o0 + (w if b is None else b)]

    identb = sb("identb", [128, 128], BF16)
    Bp = sb("Bp", [128, 2, 1024], F32)
    TB = sb("TB", [128, S], F32)
    ONESM = sb("ONESM", [128, 128], BF16)
    LASTM = sb("LASTMb", [128, 128], BF16)
    kT = sb("kT", [64, S], BF16)
    vA = sb("vA", [128, NT, 65], BF16)
    ikT4 = sb("ikT4", [128, S], BF16)
    stf = sb("stf", [128, 4, 64], F32)
    stb = sb("stb", [128, 4, 64], BF16)
    X = sb("X", [128, GT, D_MODEL], F32)
    xT = sb("xT", [128, 8, GT * 128], BF16)
    qT = sb("qT", [64, GT, 1024], BF16)
    iqTb = sb("iqTb", [128, GT, 4, 128], BF16)
    obT = sb("obT", [128, GT, 4, 128], BF16)
    aw = sb("aw", [128, GT, 4], F32)
    sgn = sb("sgn", [128, GT, 4], F32)
    ROT = sb("ROT", [128, 128], F32)
    xb = sb("xb", [128, D_MODEL], BF16)
    NF = 4
    ftmp = [sb(f"ft{i}", [128, 512], F32) for i in range(NF)]
    NB = 6
    btmp = [sb(f"bt{i}", [128, 512], BF16) for i in range(NB)]
    qbT = sb("qbT", [128, 4, 128], BF16)
    kbT = sb("kbT", [128, 4, 128], BF16)
    cT = sb("cT", [128, 128], BF16)
    scm = [sb(f"scm{i}", [128, 128], BF16) for i in range(2)]
    dS = sb("dS", [128, 64], F32)
    catT = sb("catT", [128, 8, 128], BF16)
    Eb = [sb(f"E{i}", [128, 512], BF16) for i in range(2)]
    Etmp = [sb(f"Et{i}", [128, 512], F32) for i in range(2)]
    Pm = [sb(f"Pm{i}", [128, 512], BF16) for i in range(2)]
    st = sb("st", [128, 64], F32)
    bs = sb("bs", [128, 8 * LS + LS * KIT + 16], F32)
    RW = 8704
    R = sb("R", [128, RW], F32)
    Rb = R[:].bitcast(BF16)
    sc = R[:, 0:LS * S].rearrange("p (a s) -> p a s", a=LS)
    msk = Rb[:, 2 * LS * S: 2 * LS * S + S]
    mskT = Rb[:, 2 * LS * S + S: 2 * LS * S + 2 * S].rearrange("p (a b) -> p a b", b=128)
    assert 2 * LS * S + 2 * S <= 2 * RW
    hT = Rb[:, 0:NHC * GT * 128].rearrange("p (c t) -> p c t", c=NHC)
    o_w = NHC * 512
    wgs = [Rb[:, o_w + s * 2048: o_w + s * 2048 + 1024].rearrange("p (k j) -> p k j", k=8) for s in range(2)]
    wus = [Rb[:, o_w + s * 2048 + 1024: o_w + s * 2048 + 2048].rearrange("p (k j) -> p k j", k=8) for s in range(2)]
    wds = [Rb[:, o_w + 4096 + s * 1024: o_w + 4096 + (s + 1) * 1024] for s in range(2)]
    assert o_w + 4096 + 2048 <= 2 * RW
    stg = [R[:, s * IN_W:(s + 1) * IN_W] for s in range(2)]
    stgb = [Rb[:, 2 * 2 * IN_W + s * HID: 2 * 2 * IN_W + (s + 1) * HID] for s in range(2)]
    assert 4 * IN_W + 2 * HID <= 2 * RW
    Gp = [nc.alloc_psum_tensor(f"G{i}", [128, 512], F32) for i in range(4)]
    PV = [nc.alloc_psum_tensor(f"PV{i}", [128, 512], F32) for i in range(2)]
    Tp = nc.alloc_psum_tensor("Tp", [128, 1024], BF16)
    Yp = nc.alloc_psum_tensor("Yp", [128, 512], F32)
    gctr = [0]

    def nextG():
        i = gctr[0] % 4
        gctr[0] += 1
        return Gp[i], f"G{i}"

    fctr = [0]

    def nextF():
        i = fctr[0] % NF
        fctr[0] += 1
        return ftmp[i], f"ft{i}"

    bctr = [0]

    def nextB():
        i = bctr[0] % NB
        bctr[0] += 1
        return btmp[i], f"bt{i}"

    op = P.op
    RA = ["RA"]
    RB = ["RB"]

    P.dma("sp", lambda e: e.dma_start(out=CST[:], in_=cst_d), "init", writes=["CST"], batch=True)
    P.dma("sp", lambda e: e.dma_start(out=TB[:], in_=tb_d), "init", writes=["TB"], batch=True)
    P.dma("sp", lambda e: e.dma_start(out=Bp[:, 0, :], in_=bias_d[0]), "init", writes=["Bp0"], batch=True)
    P.dma("sp", lambda e: e.dma_start(out=Bp[:, 1, :], in_=bias_d[1]), "init", writes=["Bp1"], batch=True)
    P.dma("sp", lambda e: e.dma_start(out=Etmp[0][:], in_=bias_d[2][:, 0:512]), "init", writes=["Et0"], batch=True)
    P.dma("sp", lambda e: e.dma_start(out=Etmp[1][:], in_=bias_d[2][:, 512:1024]), "init", writes=["Et1"], batch=True)
    P.close("init")
    op("dve", lambda e: e.tensor_copy(out=identb[:], in_=C("ident")), ["CST"], ["identb"])
    op("dve", lambda e: e.tensor_copy(out=LASTM[:], in_=C("LASTM")), ["CST"], ["LASTM"])
    op("dve", lambda e: e.memset(ONESM[:], 1.0), [], ["ONESM"])
    op("dve", lambda e: e.memset(bs[:], 0.0), [], ["bs"])
    op("dve", lambda e: e.memset(st[:], 0.0), [], ["stall"])
    for kd in range(2):
        for hf in range(2):
            op("dve", lambda e, kd=kd, hf=hf: e.tensor_tensor(
                out=Bp[:, kd, hf * 512:(hf + 1) * 512], in0=Bp[:, kd, hf * 512:(hf + 1) * 512],
                in1=Etmp[hf][:], op=ALU.subtract), [f"Bp{kd}", f"Et{hf}"], [f"Bp{kd}"])
    op("dve", lambda e: e.tensor_scalar(out=Wukv[:], in0=CST[:, c_off["wuk"][0]: c_off["wuk"][0] + 128],
                                        scalar1=C("kvnw"), scalar2=None, op0=ALU.mult), ["CST"], ["Wukv"])
    op("pool", lambda e: e.memset(vA[:, :, 64:65], 1.0), [], [f"vA{t}" for t in range(NT)])
    ecyc = ["dve", "pool"]
    ei = [0]

    def ceng():
        ei[0] += 1
        return ecyc[ei[0] % 2]

    for k in range(8):
        s_ = k % 2
        P.dma("sp", lambda e, k=k, s_=s_: e.dma_start(out=stg[s_], in_=w_in_d[k * 128:(k + 1) * 128, :]),
              f"stg{s_}", reads=[], writes=[f"stg{s_}"])
        for (a, b, dst) in [(0, 512, 0), (512, 804, 2560), (804, 2852, 512)]:
            en = ceng()
            op(en, lambda e, k=k, s_=s_, a=a, b=b, dst=dst: e.tensor_scalar(
                out=Win[:, k, dst:dst + (b - a)], in0=stg[s_][:, a:b], scalar1=C("n1w", k, k + 1), scalar2=None,
                op0=ALU.mult), [f"stg{s_}", "CST"], ["Win"])
    for k in range(8):
        s_ = k % 2
        P.dma("sp", lambda e, k=k, s_=s_: e.dma_start(out=stg[s_][:, 0:1024], in_=w_out_d[k * 128:(k + 1) * 128, :]),
              f"stg{s_}", writes=[f"stg{s_}"])
        op(ceng(), lambda e, k=k, s_=s_: e.tensor_copy(out=Wout[:, k, :], in_=stg[s_][:, 0:1024]),
           [f"stg{s_}"], ["Wout"])
    wst_keys = []
    for (src_d, dst_d, nm) in [(w_gate_d, wg_s, "wg_s"), (w_up_d, wu_s, "wu_s")]:
        for k in range(8):
            s_ = k % 2
            P.dma("sp", lambda e, k=k, s_=s_, src_d=src_d: e.dma_start(out=stg[s_][:, 0:HID], in_=src_d[k * 128:(k + 1) * 128, :]),
                  f"stg{s_}", writes=[f"stg{s_}"])
            op(ceng(), lambda e, k=k, s_=s_: e.tensor_scalar(
                out=stgb[s_], in0=stg[s_][:, 0:HID], scalar1=C("n2w", k, k + 1), scalar2=None, op0=ALU.mult),
               [f"stg{s_}", "CST"], [f"stgb{s_}"])
            P.dma("sp", lambda e, k=k, s_=s_, dst_d=dst_d: e.dma_start(
                out=dst_d[:, :, k, :], in_=stgb[s_].rearrange("p (c j) -> p c j", j=128)),
                f"stgb{s_}", reads=[f"stgb{s_}"], writes=[f"{nm}{k}"])
            wst_keys.append(f"{nm}{k}")
    for c2 in range(NHC // 2):
        s_ = c2 % 2
        P.dma("sp", lambda e, c2=c2, s_=s_: e.dma_start(
            out=stg[s_][:, 0:2048].rearrange("p (c n) -> p c n", c=2),
            in_=w_down_d[c2 * 256:(c2 + 1) * 256, :].rearrange("(c p) n -> p c n", p=128)),
            f"stg{s_}", writes=[f"stg{s_}"])
        op(ceng(), lambda e, s_=s_: e.tensor_copy(out=stgb[s_][:, 0:2048], in_=stg[s_][:, 0:2048]),
           [f"stg{s_}"], [f"stgb{s_}"])
        P.dma("sp", lambda e, c2=c2, s_=s_: e.dma_start(
            out=wd_s[2 * c2:2 * c2 + 2].rearrange("c p n -> p c n"),
            in_=stgb[s_][:, 0:2048].rearrange("p (c n) -> p c n", c=2)),
            f"stgb{s_}", reads=[f"stgb{s_}"], writes=[f"wd_s{c2}"])
        wst_keys.append(f"wd_s{c2}")
    op("pool", lambda e: e.memset(st[:, 60:61], 0.0), wst_keys, ["stg0", "stg1", "stgb0", "stgb1", "RA", "RB", "wg_s", "wu_s", "wd_s"])

    def rstd_from_ss(col, inv_n, eps):
        op("dve", lambda e: e.tensor_scalar(out=st[:, col:col + 1], in0=st[:, col:col + 1], scalar1=inv_n,
                                            scalar2=eps, op0=ALU.mult, op1=ALU.add), [f"st{col}"], [f"st{col}"])
        op("pool", lambda e: e.tensor_tensor(out=st[:, col:col + 1], in0=st[:, col:col + 1], in1=C("negh", 0, 1),
                                             op=ALU.pow), [f"st{col}", "CST"], [f"st{col}"])

    def transposes(src_aps, dst_ap, dst_keys, src_keys, rows=128, cols=128, evac="act"):
        n = len(src_aps)
        for i, a in enumerate(src_aps):
            op("pe", lambda e, i=i, a=a: e.transpose(out=Tp[0:cols, i * 128:(i + 1) * 128], in_=a, identity=identb[:]),
               src_keys + ["identb"], ["Tp"])
        srcv = Tp[0:cols, 0:n * 128].rearrange("p (a b) -> p a b", b=128)
        if evac == "act":
            op("act", lambda e: e.copy(out=dst_ap, in_=srcv), ["Tp"], dst_keys)
        else:
            op("dve", lambda e: e.tensor_copy(out=dst_ap, in_=srcv), ["Tp"], dst_keys)

    for seq in range(NSEQ):
        op("pool", lambda e: e.memset(stf[:], 0.0), [], ["stf"])
        op("pool", lambda e: e.memset(stb[:], 0.0), [], ["stb"])
        for gi in range(NG):
            tiles = [gi * GT + j for j in range(GT)]
            op("pool", lambda e: e.memset(st[:, 61:62], 0.0), [], RA + RB)
            for j, ti in enumerate(tiles):
                row0 = seq * S + ti * 128
                pos = ti * 128
                Xj = f"X{j}"
                P.dma("sp", lambda e, j=j, row0=row0: e.dma_start(out=X[:, j, :], in_=x_d[row0:row0 + 128, :]),
                      f"x{j}", writes=[Xj])
                P.dma("sp", lambda e, ti=ti: e.dma_start(out=ROT[:], in_=rot_d[ti]), "rot", writes=["ROT"])
                op("act", lambda e, j=j: e.activation(out=xb[:], in_=X[:, j, :], func=AF.Square, accum_out=st[:, 0:1]),
                   [Xj], ["xb", "st0"])
                rstd_from_ss(0, 1.0 / D_MODEL, RMS_EPS)
                op("act", lambda e, j=j: e.activation(out=xb[:], in_=X[:, j, :], func=AF.Copy, scale=st[:, 0:1]),
                   [Xj, "st0"], ["xb"])
                transposes([xb[:, k * 128:(k + 1) * 128] for k in range(8)], xT[:, :, j * 128:(j + 1) * 128],
                           [f"xT{j}"], ["xb"])
                zb = []
                for b in range(6):
                    w_ = 512 if b < 5 else IN_W - 2560
                    g, gk = nextG()
                    for k in range(8):
                        op("pe", lambda e, g=g, k=k, j=j, b=b, w_=w_: e.matmul(
                            g[:, 0:w_], lhsT=xT[:, k, j * 128:(j + 1) * 128], rhs=Win[:, k, b * 512:b * 512 + w_],
                            start=(k == 0), stop=(k == 7)), [f"xT{j}", "Win"], [gk])
                    if b == 0:
                        f1, f1k = nextF()
                        op("act", lambda e, g=g, f1=f1: e.activation(out=f1[:], in_=g[:], func=AF.Square), [gk], [f1k])
                        op("dve", lambda e, f1=f1: e.tensor_reduce(out=st[:, 8:16], in_=f1[:].rearrange("p (h d) -> p h d", h=8),
                                                                   axis=AX.X, op=ALU.add), [f1k], ["st8"])
                        op("dve", lambda e: e.tensor_scalar(out=st[:, 8:16], in0=st[:, 8:16], scalar1=1.0 / 64, scalar2=RMS_EPS,
                                                            op0=ALU.mult, op1=ALU.add), ["st8"], ["st8"])
                        op("pool", lambda e: e.tensor_tensor(out=st[:, 8:16], in0=st[:, 8:16], in1=C("negh"), op=ALU.pow),
                           ["st8", "CST"], ["st8"])
                        f2, f2k = nextF()
                        op("dve", lambda e, g=g, f2=f2: e.tensor_tensor(
                            out=f2[:].rearrange("p (h d) -> p h d", h=8), in0=g[:].rearrange("p (h d) -> p h d", h=8),
                            in1=st[:, 8:16].unsqueeze(2).broadcast_to([128, 8, 64]), op=ALU.mult), [gk, "st8"], [f2k])
                        b1, b1k = nextB()
                        op("pool", lambda e, f2=f2, b1=b1: e.tensor_tensor(
                            out=b1[:].rearrange("p (h d) -> p h d", h=8), in0=f2[:].rearrange("p (h d) -> p h d", h=8),
                            in1=C("qnw").unsqueeze(1).broadcast_to([128, 8, 64]), op=ALU.mult), [f2k, "CST"], [b1k])
                        transposes([b1[:, h * 64:(h + 1) * 64] for h in range(8)],
                                   qT[:, j, :].rearrange("p (a b) -> p a b", b=128), [f"qT{j}"], [b1k], cols=64)
                    elif b in (1, 2):
                        fa, fak = nextF()
                        fb, fbk = nextF()
                        g3 = g[:].rearrange("p (h d) -> p h d", h=8)
                        op("dve", lambda e, fa=fa, g3=g3: e.tensor_tensor(
                            out=fa[:].rearrange("p (h d) -> p h d", h=8), in0=g3,
                            in1=ROT[:, 0:64].unsqueeze(1).broadcast_to([128, 8, 64]), op=ALU.mult), [gk, "ROT"], [fak])
                        op("dve", lambda e, fb=fb, g3=g3: e.tensor_tensor(
                            out=fb[:].rearrange("p (h d) -> p h d", h=8)[:, :, 0:32], in0=g3[:, :, 32:64],
                            in1=ROT[:, 64:96].unsqueeze(1).broadcast_to([128, 8, 32]), op=ALU.mult), [gk, "ROT"], [fbk])
                        op("dve", lambda e, fb=fb, g3=g3: e.tensor_tensor(
                            out=fb[:].rearrange("p (h d) -> p h d", h=8)[:, :, 32:64], in0=g3[:, :, 0:32],
                            in1=ROT[:, 96:128].unsqueeze(1).broadcast_to([128, 8, 32]), op=ALU.mult), [gk, "ROT", fbk], [fbk])
                        rb_, rbk = nextB()
                        op("pool", lambda e, fa=fa, fb=fb, rb_=rb_: e.tensor_tensor(out=rb_[:], in0=fa[:], in1=fb[:], op=ALU.add),
                           [fak, fbk], [rbk])
                        dstT, dk_, dcn = (qbT, "qbT", "DQ") if b == 1 else (kbT, "kbT", "DK")
                        for pq in range(4):
                            op("pe", lambda e, pq=pq, rb_=rb_: e.transpose(out=Tp[:, pq * 128:(pq + 1) * 128],
                                                                          in_=rb_[:, pq * 128:(pq + 1) * 128], identity=identb[:]),
                               [rbk, "identb"], ["Tp"])
                        op("dve", lambda e, dstT=dstT, dcn=dcn: e.tensor_tensor(
                            out=dstT[:].rearrange("p a b -> p (a b)"), in0=Tp[:, 0:512], in1=C(dcn), op=ALU.mult),
                           ["Tp", "CST"], [dk_])
                        if b == 2:
                            kt_, ktk = nextB()
                            op("pool", lambda e, rb_=rb_, kt_=kt_: e.tensor_tensor(
                                out=kt_[:].rearrange("p (h d) -> p h d", h=8), in0=rb_[:].rearrange("p (h d) -> p h d", h=8),
                                in1=C("DKtok").unsqueeze(2).broadcast_to([128, 8, 64]), op=ALU.mult), [rbk, "CST"], [ktk])
                            ktok, ktokk = kt_, ktk
                    elif b == 3:
                        vt_, vtk = nextB()
                        op("act", lambda e, g=g, vt_=vt_: e.copy(out=vt_[:], in_=g[:]), [gk], [vtk])
                        vtok, vtokk = vt_, vtk
                    elif b == 4:
                        th, thk = nextF()
                        op("act", lambda e, g=g, th=th: e.activation(out=th[:], in_=g[:], func=AF.Tanh, scale=0.5), [gk], [thk])
                        op("dve", lambda e, g=g, th=th: e.scalar_tensor_tensor(out=th[:], in0=th[:], scalar=1.0, in1=g[:],
                                                                               op0=ALU.add, op1=ALU.mult), [gk, thk], [thk])
                        sgt, sgtk = th, thk
                    else:
                        op("act", lambda e, g=g: e.activation(out=xb[:, 0:128], in_=g[:, 0:128], func=AF.Square,
                                                              accum_out=st[:, 1:2]), [gk], ["xb", "st1"])
                        rstd_from_ss(1, 1.0 / 128, RMS_EPS)
                        cb_, cbk = nextB()
                        op("act", lambda e, g=g, cb_=cb_: e.activation(out=cb_[:, 0:128], in_=g[:, 0:128], func=AF.Copy,
                                                                       scale=st[:, 1:2]), [gk, "st1"], [cbk])
                        op("act", lambda e, g=g, cb_=cb_: e.copy(out=cb_[:, 128:256], in_=g[:, 128:256]), [gk], [cbk])
                        op("act", lambda e, g=g, cb_=cb_: e.copy(
                            out=cb_[:, 256:384].rearrange("p (a b) -> p a b", a=4),
                            in_=g[:, 256:288].unsqueeze(1).broadcast_to([128, 4, 32])), [gk], [cbk])
                        op("dve", lambda e, g=g, j=j: e.tensor_scalar(out=sgn[:, j, :], in0=g[:, 288:292], scalar1=0.0,
                                                                      scalar2=-0.5, op0=ALU.is_ge, op1=ALU.add), [gk], [f"sgn{j}"])
                        op("dve", lambda e, g=g, j=j: e.scalar_tensor_tensor(out=aw[:, j, :], in0=g[:, 288:292], scalar=2.0,
                                                                             in1=sgn[:, j, :], op0=ALU.mult, op1=ALU.mult),
                           [gk, f"sgn{j}"], [f"aw{j}"])
                        op("pe", lambda e, cb_=cb_: e.transpose(out=Tp[:, 0:128], in_=cb_[:, 0:128], identity=identb[:]),
                           [cbk, "identb"], ["Tp"])
                        op("act", lambda e: e.copy(out=cT[:], in_=Tp[:, 0:128]), ["Tp"], ["cT"])
                        g2, g2k = nextG()
                        op("pe", lambda e, g2=g2: e.matmul(g2[:, 0:128], lhsT=cT[:], rhs=Wukv[:], start=True, stop=True),
                           ["cT", "Wukv"], [g2k])
                        op("act", lambda e, g2=g2: e.activation(out=xb[:, 0:64], in_=g2[:, 0:64], func=AF.Square,
                                                                accum_out=st[:, 2:3]), [g2k], ["xb", "st2"])
                        rstd_from_ss(2, 1.0 / 64, RMS_EPS)
                        op("dve", lambda e, g2=g2, cb_=cb_: e.scalar_tensor_tensor(
                            out=cb_[:, 384:448], in0=g2[:, 0:64], scalar=st[:, 2:3], in1=C("knw"), op0=ALU.mult, op1=ALU.mult),
                           [g2k, "st2", "CST"], [cbk])
                        op("act", lambda e, g2=g2, ti=ti: e.copy(out=vA[:, ti, 0:64], in_=g2[:, 64:128]), [g2k], [f"vA{ti}"])
                        op("pe", lambda e, cb_=cb_: e.transpose(out=Tp[0:64, 0:128], in_=cb_[:, 384:448], identity=identb[:]),
                           [cbk, "identb"], ["Tp"])
                        op("act", lambda e, pos=pos: e.copy(out=kT[:, pos:pos + 128], in_=Tp[0:64, 0:128]), ["Tp"], [f"kT{ti}"])
                        op("pe", lambda e, cb_=cb_: e.transpose(out=Tp[:, 0:128], in_=cb_[:, 128:256], identity=identb[:]),
                           [cbk, "identb"], ["Tp"])
                        op("dve", lambda e, j=j: e.tensor_tensor(
                            out=iqTb[:, j, :, :], in0=Tp[:, 0:128].unsqueeze(1).broadcast_to([128, 4, 128]),
                            in1=C("BM").rearrange("p (a b) -> p a b", a=4), op=ALU.mult), ["Tp", "CST"], [f"iqTb{j}"])
                        op("pe", lambda e, cb_=cb_: e.transpose(out=Tp[:, 0:128], in_=cb_[:, 256:384], identity=identb[:]),
                           [cbk, "identb"], ["Tp"])
                        op("act", lambda e, pos=pos: e.copy(out=ikT4[:, pos:pos + 128], in_=Tp[:, 0:128]), ["Tp"], [f"ik{ti}"])

                for pq in range(4):
                    for hh in range(2):
                        h = 2 * pq + hh
                        r0, r1 = hh * 64, hh * 64 + 64
                        g, gk = nextG()
                        op("pe", lambda e, g=g, pq=pq, r0=r0, r1=r1: e.matmul(
                            g[:, 0:128], lhsT=kbT[r0:r1, pq, :], rhs=qbT[r0:r1, pq, :], start=True, stop=True),
                           ["kbT", "qbT"], [gk])
                        sm = scm[h % 2]
                        smk = f"scm{h % 2}"
                        op("dve", lambda e, g=g, sm=sm: e.tensor_tensor(out=sm[:], in0=g[:, 0:128], in1=C("DMASK"), op=ALU.mult),
                           [gk, "CST"], [smk])
                        op("pe", lambda e, sm=sm, h=h, vtok=vtok: e.matmul(
                            Yp[:, h * 64:(h + 1) * 64], lhsT=sm[:], rhs=vtok[:, h * 64:(h + 1) * 64], start=True, stop=False),
                           [smk, vtokk], ["Yp"])
                        op("pe", lambda e, pq=pq, r0=r0, r1=r1, h=h: e.matmul(
                            Yp[:, h * 64:(h + 1) * 64], lhsT=qbT[r0:r1, pq, :], rhs=stb[r0:r1, pq, :], start=False, stop=True),
                           ["qbT", "stb"], ["Yp"])
                    g, gk = nextG()
                    op("pe", lambda e, g=g, pq=pq, ktok=ktok, vtok=vtok: e.matmul(
                        g[:, 0:128], lhsT=ktok[:, pq * 128:(pq + 1) * 128], rhs=vtok[:, pq * 128:(pq + 1) * 128],
                        start=True, stop=True), [ktokk, vtokk], [gk])
                    for hh in range(2):
                        r0, r1 = hh * 64, hh * 64 + 64
                        op("act", lambda e, g=g, r0=r0, r1=r1, pq=pq: e.activation(
                            out=dS[r0:r1, :], in_=g[r0:r1, r0:r1], func=AF.Copy, scale=C("GC", pq, pq + 1)[r0:r1, :]),
                           [gk, "CST"], ["dS"])
                    op("pool", lambda e, pq=pq: e.tensor_scalar(out=stf[:, pq, :], in0=stf[:, pq, :], scalar1=C("GC", pq, pq + 1),
                                                                scalar2=None, op0=ALU.mult), ["stf", "CST"], ["stf"])
                    op("pool", lambda e, pq=pq: e.tensor_tensor(out=stf[:, pq, :], in0=stf[:, pq, :], in1=dS[:], op=ALU.add),
                       ["stf", "dS"], ["stf"])
                    op("pool", lambda e, pq=pq: e.tensor_copy(out=stb[:, pq, :], in_=stf[:, pq, :]), ["stf"], ["stb"])
                ysq, ysqk = nextF()
                op("act", lambda e, ysq=ysq: e.activation(out=ysq[:], in_=Yp[:], func=AF.Square), ["Yp"], [ysqk])
                op("dve", lambda e: e.tensor_reduce(out=st[:, 16:24], in_=Yp[:].rearrange("p (h d) -> p h d", h=8),
                                                    axis=AX.X, op=ALU.add), ["Yp"], ["st16"])
                op("dve", lambda e, ysq=ysq: e.tensor_reduce(out=st[:, 24:32], in_=ysq[:].rearrange("p (h d) -> p h d", h=8),
                                                             axis=AX.X, op=ALU.add), [ysqk], ["st24"])
                op("dve", lambda e: e.tensor_scalar(out=st[:, 16:24], in0=st[:, 16:24], scalar1=1.0 / 64, scalar2=None,
                                                    op0=ALU.mult), ["st16"], ["st16"])
                op("dve", lambda e: e.tensor_tensor(out=st[:, 32:40], in0=st[:, 16:24], in1=st[:, 16:24], op=ALU.mult),
                   ["st16"], ["st32"])
                op("dve", lambda e: e.scalar_tensor_tensor(out=st[:, 24:32], in0=st[:, 24:32], scalar=1.0 / 64, in1=st[:, 32:40],
                                                           op0=ALU.mult, op1=ALU.subtract), ["st24", "st32"], ["st24"])
                op("dve", lambda e: e.tensor_scalar(out=st[:, 24:32], in0=st[:, 24:32], scalar1=GN_EPS, scalar2=None,
                                                    op0=ALU.add), ["st24"], ["st24"])
                op("pool", lambda e: e.tensor_tensor(out=st[:, 24:32], in0=st[:, 24:32], in1=C("negh"), op=ALU.pow),
                   ["st24", "CST"], ["st24"])
                op("dve", lambda e: e.scalar_tensor_tensor(out=st[:, 32:40], in0=st[:, 16:24], scalar=-1.0, in1=st[:, 24:32],
                                                           op0=ALU.mult, op1=ALU.mult), ["st16", "st24"], ["st32"])
                yn, ynk = ysq, ysqk
                op("dve", lambda e, yn=yn: e.tensor_tensor(
                    out=yn[:].rearrange("p (h d) -> p h d", h=8), in0=Yp[:].rearrange("p (h d) -> p h d", h=8),
                    in1=st[:, 24:32].unsqueeze(2).broadcast_to([128, 8, 64]), op=ALU.mult), ["Yp", "st24", ynk], [ynk])
                op("pool", lambda e, yn=yn: e.tensor_tensor(
                    out=yn[:].rearrange("p (h d) -> p h d", h=8), in0=yn[:].rearrange("p (h d) -> p h d", h=8),
                    in1=st[:, 32:40].unsqueeze(2).broadcast_to([128, 8, 64]), op=ALU.add), [ynk, "st32"], [ynk])
                op("pool", lambda e, yn=yn: e.tensor_tensor(out=yn[:], in0=yn[:], in1=C("gnw"), op=ALU.mult), [ynk, "CST"], [ynk])
                op("pool", lambda e, yn=yn: e.tensor_tensor(out=yn[:], in0=yn[:], in1=C("gnb"), op=ALU.add), [ynk, "CST"], [ynk])
                ob, obk = nextB()
                op("dve", lambda e, yn=yn, ob=ob, sgt=sgt: e.scalar_tensor_tensor(out=ob[:], in0=yn[:], scalar=0.5, in1=sgt[:],
                                                                                  op0=ALU.mult, op1=ALU.mult), [ynk, sgtk], [obk])
                transposes([ob[:, c * 128:(c + 1) * 128] for c in range(4)], obT[:, j, :, :], [f"obT{j}"], [obk])

                N = (ti + 1) * 128
                if N > KTOP:
                    ls = j % LS
                    for kb in range((N + 511) // 512):
                        k0 = kb * 512
                        w_ = min(512, N - k0)
                        for h in range(4):
                            g, gk = nextG()
                            op("pe", lambda e, g=g, j=j, h=h, k0=k0, w_=w_: e.matmul(
                                g[:, 0:w_], lhsT=iqTb[:, j, h, :], rhs=ikT4[:, k0:k0 + w_], start=True, stop=True),
                               [f"iqTb{j}"] + [f"ik{q}" for q in range(k0 // 128, (k0 + w_) // 128)], [gk])
                            op("act", lambda e, g=g, j=j, h=h, w_=w_: e.activation(
                                out=g[:, 0:w_], in_=g[:, 0:w_], func=AF.Relu, scale=aw[:, j, h:h + 1]), [gk, f"aw{j}"], [gk])
                            if h == 0:
                                op("dve", lambda e, g=g, j=j, ls=ls, k0=k0, w_=w_: e.scalar_tensor_tensor(
                                    out=sc[:, ls, k0:k0 + w_], in0=g[:, 0:w_], scalar=sgn[:, j, 0:1], in1=TB[:, k0:k0 + w_],
                                    op0=ALU.mult, op1=ALU.add), [gk, f"sgn{j}", "TB"] + RA, [f"sc{ls}"])
                            else:
                                op("dve", lambda e, g=g, j=j, ls=ls, k0=k0, w_=w_, h=h: e.scalar_tensor_tensor(
                                    out=sc[:, ls, k0:k0 + w_], in0=g[:, 0:w_], scalar=sgn[:, j, h:h + 1], in1=sc[:, ls, k0:k0 + w_],
                                    op0=ALU.mult, op1=ALU.add), [gk, f"sgn{j}", f"sc{ls}"] + RA, [f"sc{ls}"])

                if (j + 1) % LS == 0:
                    sub = list(range(j + 1 - LS, j + 1))
                    bl = [(jj, tiles[jj]) for jj in sub if (tiles[jj] + 1) * 128 > KTOP]
                    nb_ = len(bl)
                    MX, MN, RNG, MID, CNT, PMH, THR = 0, LS, 2 * LS, 3 * LS, 4 * LS, 5 * LS, 6 * LS
                    STP = 8 * LS
                    if nb_ > 0:
                        for (jj, tt) in bl:
                            ls = jj % LS
                            N = (tt + 1) * 128
                            op("dve", lambda e, ls=ls, N=N: e.tensor_reduce(out=bs[:, MX + ls:MX + ls + 1], in_=sc[:, ls, 0:N],
                                                                             axis=AX.X, op=ALU.max), [f"sc{ls}"], ["bs"])
                            op("dve", lambda e, ls=ls, N=N: e.tensor_reduce(out=bs[:, MN + ls:MN + ls + 1], in_=sc[:, ls, 0:N],
                                                                             axis=AX.X, op=ALU.min), [f"sc{ls}"], ["bs"])
                            op("dve", lambda e, ls=ls, N=N: e.memset(sc[0:64, ls, N - 64:N], NEG), [f"sc{ls}"], [f"sc{ls}"])
                        if nb_ < LS:
                            for ls in range(LS):
                                if ls not in [jj % LS for (jj, _) in bl]:
                                    op("dve", lambda e, ls=ls: e.memset(bs[:, MX + ls:MX + ls + 1], 1.0), [], ["bs"])
                                    op("dve", lambda e, ls=ls: e.memset(bs[:, MN + ls:MN + ls + 1], 0.0), [], ["bs"])
                        op("dve", lambda e: e.tensor_tensor(out=bs[:, RNG:RNG + LS], in0=bs[:, MX:MX + LS], in1=bs[:, MN:MN + LS],
                                                            op=ALU.subtract), ["bs"], ["bs"])
                        op("dve", lambda e: e.scalar_tensor_tensor(out=bs[:, MID:MID + LS], in0=bs[:, RNG:RNG + LS], scalar=0.5,
                                                                   in1=bs[:, MN:MN + LS], op0=ALU.mult, op1=ALU.add), ["bs"], ["bs"])
                        op("dve", lambda e: e.tensor_tensor(
                            out=bs[:, STP:STP + LS * KIT].rearrange("p (a k) -> p a k", a=LS),
                            in0=bs[:, RNG:RNG + LS].unsqueeze(2).broadcast_to([128, LS, KIT]),
                            in1=C("pw", 0, KIT).unsqueeze(1).broadcast_to([128, LS, KIT]), op=ALU.mult), ["bs", "CST"], ["bs"])
                        stp3 = bs[:, STP:STP + LS * KIT].rearrange("p (a k) -> p a k", a=LS)
                        for it in range(KIT):
                            for (jj, tt) in bl:
                                ls = jj % LS
                                N = (tt + 1) * 128
                                op("dve", lambda e, ls=ls, N=N: e.tensor_scalar(
                                    out=msk[:, 0:N], in0=sc[:, ls, 0:N], scalar1=bs[:, MID + ls:MID + ls + 1], scalar2=None,
                                    op0=ALU.is_ge, op1=ALU.add, accum_out=bs[:, CNT + ls:CNT + ls + 1]),
                                   [f"sc{ls}", "bs"] + RA, ["msk", "bs"])
                            op("dve", lambda e: e.tensor_scalar(out=bs[:, PMH:PMH + LS], in0=bs[:, CNT:CNT + LS],
                                                                scalar1=float(KTOP) - 0.5, scalar2=-0.5, op0=ALU.is_ge, op1=ALU.add),
                               ["bs"], ["bs"])
                            op("dve", lambda e, it=it: e.tensor_tensor(out=bs[:, PMH:PMH + LS], in0=bs[:, PMH:PMH + LS],
                                                                       in1=stp3[:, :, it], op=ALU.mult), ["bs"], ["bs"])
                            op("dve", lambda e: e.tensor_tensor(out=bs[:, MID:MID + LS], in0=bs[:, MID:MID + LS],
                                                                in1=bs[:, PMH:PMH + LS], op=ALU.add), ["bs"], ["bs"])
                        op("dve", lambda e: e.scalar_tensor_tensor(out=bs[:, THR:THR + LS], in0=bs[:, RNG:RNG + LS],
                                                                   scalar=-(2.0 ** -(KIT + 1)), in1=bs[:, MID:MID + LS],
                                                                   op0=ALU.mult, op1=ALU.add), ["bs"], ["bs"])
                    for jj in sub:
                        tt = tiles[jj]
                        nk = tt + 1
                        N = nk * 128
                        ls = jj % LS
                        bis = N > KTOP
                        if bis:
                            op("dve", lambda e, ls=ls, N=N: e.tensor_scalar(
                                out=msk[:, 0:N], in0=sc[:, ls, 0:N], scalar1=bs[:, THR + ls:THR + ls + 1], scalar2=None,
                                op0=ALU.is_ge), [f"sc{ls}", "bs"] + RA, ["msk"])
                            for c0 in range(0, nk, 8):
                                cn = min(8, nk - c0)
                                transposes([msk[:, (c0 + i) * 128:(c0 + i + 1) * 128] for i in range(cn)],
                                           mskT[:, c0:c0 + cn, :], ["mskT"], ["msk"] + RA)
                        for kt in range(nk):
                            if bis:
                                mk_ap, mkk = mskT[:, kt, :], "mskT"
                            elif kt == nk - 1:
                                mk_ap, mkk = LASTM[:], "LASTM"
                            else:
                                mk_ap, mkk = ONESM[:], "ONESM"
                            for hf in range(2):
                                g, gk = nextG()
                                op("pe", lambda e, g=g, kt=kt, jj=jj, hf=hf: e.matmul(
                                    g[:], lhsT=kT[:, kt * 128:(kt + 1) * 128], rhs=qT[:, jj, hf * 512:(hf + 1) * 512],
                                    start=True, stop=True), [f"kT{kt}", f"qT{jj}"], [gk])
                                ei_ = (kt * 2 + hf) % 2
                                E_, Ek = Eb[ei_], f"E{ei_}"
                                if kt >= tt - 1:
                                    kd = 0 if kt == tt else 1
                                    Et, Etk = Etmp[ei_], f"Et{ei_}"
                                    op("dve", lambda e, g=g, Et=Et, kd=kd, hf=hf: e.scalar_tensor_tensor(
                                        out=Et[:], in0=g[:], scalar=0.125, in1=Bp[:, kd, hf * 512:(hf + 1) * 512],
                                        op0=ALU.mult, op1=ALU.add), [gk, f"Bp{kd}"], [Etk])
                                    op("act", lambda e, Et=Et, E_=E_: e.activation(out=E_[:], in_=Et[:], func=AF.Exp), [Etk], [Ek])
                                else:
                                    op("act", lambda e, g=g, E_=E_: e.activation(out=E_[:], in_=g[:], func=AF.Exp, scale=0.125),
                                       [gk], [Ek])
                                Pm_, Pk = Pm[ei_], f"Pm{ei_}"
                                op("pool", lambda e, E_=E_, Pm_=Pm_, mk_ap=mk_ap: e.tensor_tensor(
                                    out=Pm_[:].rearrange("p (h t) -> p h t", h=4), in0=E_[:].rearrange("p (h t) -> p h t", h=4),
                                    in1=mk_ap.unsqueeze(1).broadcast_to([128, 4, 128]), op=ALU.mult), [Ek, mkk] + RA, [Pk])
                                for h4 in range(4):
                                    op("pe", lambda e, Pm_=Pm_, h4=h4, hf=hf, kt=kt, nk=nk: e.matmul(
                                        PV[hf][:, h4 * 65:(h4 + 1) * 65], lhsT=Pm_[:, h4 * 128:(h4 + 1) * 128], rhs=vA[:, kt, :],
                                        start=(kt == 0 and h4 == 0), stop=(kt == nk - 1 and h4 == 3), skip_group_check=True),
                                       [Pk, f"vA{kt}"], [f"PV{hf}"])
                        oa, oak = nextB()
                        for hf in range(2):
                            pv3 = PV[hf][:, 0:260].rearrange("p (h c) -> p h c", h=4)
                            op("dve", lambda e, pv3=pv3, hf=hf: e.reciprocal(out=st[:, 40 + hf * 4:44 + hf * 4], in_=pv3[:, :, 64]),
                               [f"PV{hf}"], [f"st4{hf}"])
                            op("dve", lambda e, pv3=pv3, hf=hf, oa=oa: e.tensor_tensor(
                                out=oa[:, hf * 256:(hf + 1) * 256].rearrange("p (h d) -> p h d", h=4), in0=pv3[:, :, 0:64],
                                in1=st[:, 40 + hf * 4:44 + hf * 4].unsqueeze(2).broadcast_to([128, 4, 64]), op=ALU.mult),
                               [f"PV{hf}", f"st4{hf}"], [oak])
                        transposes([oa[:, c * 128:(c + 1) * 128] for c in range(4)], catT[:, 0:4, :], ["catT"], [oak])
                        op("pool", lambda e, jj=jj: e.tensor_copy(out=catT[:, 4:8, :], in_=obT[:, jj, :, :]), [f"obT{jj}"], ["catT"])
                        for nbk in range(2):
                            g, gk = nextG()
                            for c in range(8):
                                op("pe", lambda e, g=g, c=c, nbk=nbk: e.matmul(
                                    g[:], lhsT=catT[:, c, :], rhs=Wout[:, c, nbk * 512:(nbk + 1) * 512], start=(c == 0), stop=(c == 7)),
                                   ["catT", "Wout"], [gk])
                            op("dve", lambda e, g=g, jj=jj, nbk=nbk: e.tensor_tensor(
                                out=X[:, jj, nbk * 512:(nbk + 1) * 512], in0=g[:], in1=X[:, jj, nbk * 512:(nbk + 1) * 512], op=ALU.add),
                               [gk, f"X{jj}"], [f"X{jj}"])
                        op("act", lambda e, jj=jj: e.activation(out=xb[:], in_=X[:, jj, :], func=AF.Square, accum_out=st[:, 3:4]),
                           [f"X{jj}"], ["xb", "st3"])
                        rstd_from_ss(3, 1.0 / D_MODEL, RMS_EPS)
                        op("act", lambda e, jj=jj: e.activation(out=xb[:], in_=X[:, jj, :], func=AF.Copy, scale=st[:, 3:4]),
                           [f"X{jj}", "st3"], ["xb"])
                        transposes([xb[:, k * 128:(k + 1) * 128] for k in range(8)], xT[:, :, jj * 128:(jj + 1) * 128],
                                   [f"xT{jj}"], ["xb"])
            op("pool", lambda e: e.memset(st[:, 62:63], 0.0), [], RA + RB)
            TW = GT * 128
            for c in range(NHC):
                s_ = c % 2
                P.dma("sp", lambda e, c=c, s_=s_: e.dma_start(out=wgs[s_], in_=wg_s[:, c]), f"wg{s_}",
                      reads=["wg_s"] + RB, writes=[f"wg{s_}"])
                P.dma("sp", lambda e, c=c, s_=s_: e.dma_start(out=wus[s_], in_=wu_s[:, c]), f"wu{s_}",
                      reads=["wu_s"] + RB, writes=[f"wu{s_}"])
                gg, ggk = nextG()
                gu, guk = nextG()
                for k in range(8):
                    op("pe", lambda e, gg=gg, k=k, s_=s_: e.matmul(gg[:, 0:TW], lhsT=wgs[s_][:, k, :], rhs=xT[:, k, :],
                                                                   start=(k == 0), stop=(k == 7)),
                       [f"wg{s_}"] + RB + [f"xT{q}" for q in range(GT)], [ggk])
                for k in range(8):
                    op("pe", lambda e, gu=gu, k=k, s_=s_: e.matmul(gu[:, 0:TW], lhsT=wus[s_][:, k, :], rhs=xT[:, k, :],
                                                                   start=(k == 0), stop=(k == 7)),
                       [f"wu{s_}"] + RB + [f"xT{q}" for q in range(GT)], [guk])
                sl, slk = nextF()
                op("act", lambda e, gg=gg, sl=sl: e.activation(out=sl[:, 0:TW], in_=gg[:, 0:TW], func=AF.Silu), [ggk], [slk])
                op("dve", lambda e, gu=gu, sl=sl, c=c: e.tensor_tensor(out=hT[:, c, :], in0=gu[:, 0:TW], in1=sl[:, 0:TW], op=ALU.mult),
                   [guk, slk] + RB, ["hT"])
            for pr in range((GT + 1) // 2):
                tl = [t_ for t_ in (2 * pr, 2 * pr + 1) if t_ < GT]
                accs = {}
                for t_ in tl:
                    for nbk in range(2):
                        accs[(t_, nbk)] = nextG()
                for c in range(NHC):
                    s_ = c % 2
                    P.dma("sp", lambda e, c=c, s_=s_: e.dma_start(out=wds[s_], in_=wd_s[c]), f"wd{s_}",
                          reads=["wd_s"] + RB, writes=[f"wd{s_}"])
                    for t_ in tl:
                        for nbk in range(2):
                            g, gk = accs[(t_, nbk)]
                            op("pe", lambda e, g=g, c=c, t_=t_, nbk=nbk, s_=s_: e.matmul(
                                g[:], lhsT=hT[:, c, t_ * 128:(t_ + 1) * 128], rhs=wds[s_][:, nbk * 512:(nbk + 1) * 512],
                                start=(c == 0), stop=(c == NHC - 1)), ["hT", f"wd{s_}"] + RB, [gk])
                for t_ in tl:
                    for nbk in range(2):
                        g, gk = accs[(t_, nbk)]
                        op("dve", lambda e, g=g, t_=t_, nbk=nbk: e.tensor_tensor(
                            out=X[:, t_, nbk * 512:(nbk + 1) * 512], in0=g[:], in1=X[:, t_, nbk * 512:(nbk + 1) * 512], op=ALU.add),
                           [gk, f"X{t_}"], [f"X{t_}"])
                    row0 = seq * S + tiles[t_] * 128
                    P.dma("sp", lambda e, t_=t_, row0=row0: e.dma_start(out=y_d[row0:row0 + 128, :], in_=X[:, t_, :]),
                          f"y{t_}", reads=[f"X{t_}"])
    P.emit()
    return P


C_LAYOUT = [("ident", 128), ("n1w", 8), ("n2w", 8), ("kvnw", 1), ("qnw", 64), ("knw", 64),
            ("gnw", 512), ("gnb", 512), ("wuk", 64), ("wuv", 64), ("DQ", 512), ("DK", 512),
            ("DKtok", 8), ("DMASK", 128), ("GC", 4), ("BM", 512), ("LASTM", 128), ("pw", 32),
            ("negh", 8)]


def _t5_bucket_np(rel):
    import jax
    import jax.numpy as jnp
    cpu = jax.devices("cpu")[0]
    with jax.default_device(cpu):
        rel = jnp.asarray(rel, dtype=jnp.int32)
        half, max_exact, max_distance = 16, 8, 128
        ret = jnp.where(rel > 0, half, 0)
        n = jnp.abs(rel)
        nf = jnp.maximum(n, 1).astype(jnp.float32)
        large = max_exact + (jnp.log(nf / max_exact) / math.log(max_distance / max_exact)
                             * (half - max_exact)).astype(jnp.int32)
        large = jnp.minimum(large, half - 1)
        out = ret + jnp.where(n < max_exact, n, large)
        return np.asarray(out)


def _rot_tables(S):
    import jax
    import jax.numpy as jnp
    cpu = jax.devices("cpu")[0]
    with jax.default_device(cpu):
        half = 32
        inv = 10000.0 ** (-jnp.arange(half, dtype=jnp.float32) / half)
        ang = jnp.arange(S).astype(jnp.float32)[:, None] * inv[None, :]
        cos = np.asarray(jnp.cos(ang))
        sin = np.asarray(jnp.sin(ang))
    rot = np.concatenate([cos, cos, -sin, sin], axis=1).astype(np.float32)
    return np.ascontiguousarray(rot.reshape(S // 128, 128, 128))


def _host_consts(inp, S):
    f32 = np.float32
    secs = {}
    secs["ident"] = np.eye(128, dtype=f32)
    secs["n1w"] = np.ascontiguousarray(inp["norm1_w"][0].reshape(8, 128).T)
    secs["n2w"] = np.ascontiguousarray(inp["norm2_w"][0].reshape(8, 128).T)
    secs["kvnw"] = inp["kv_norm_w"][0].reshape(128, 1)
    secs["qnw"] = np.broadcast_to(inp["q_norm_w"][0][None, :], (128, 64))
    secs["knw"] = np.broadcast_to(inp["k_norm_w"][0][None, :], (128, 64))
    secs["gnw"] = np.broadcast_to(inp["ret_gn_w"][0][None, :], (128, 512))
    secs["gnb"] = np.broadcast_to(inp["ret_gn_b"][0][None, :], (128, 512))
    secs["wuk"] = inp["w_uk"][0]
    secs["wuv"] = inp["w_uv"][0]
    hh = np.arange(8, dtype=np.float64)
    log_g = np.log1p(-(2.0 ** (-5.0 - hh)))
    t = np.arange(128, dtype=np.float64)
    row_h = np.arange(128) // 64
    DQ = np.zeros((128, 4, 128)); DK = np.zeros((128, 4, 128)); GC = np.zeros((128, 4))
    for pq in range(4):
        lg = log_g[2 * pq + row_h]
        DQ[:, pq, :] = np.exp((t[None, :] + 1.0) * lg[:, None])
        DK[:, pq, :] = np.exp(-(t[None, :] + 1.0) * lg[:, None]) * 0.125
        GC[:, pq] = np.exp(128.0 * lg)
    secs["DQ"] = DQ.reshape(128, 512)
    secs["DK"] = DK.reshape(128, 512)
    secs["DKtok"] = np.exp(-(t[:, None] + 1.0) * log_g[None, :]) * 0.125
    jj = np.arange(128)
    secs["DMASK"] = (jj[None, :] >= jj[:, None]).astype(f32)
    secs["GC"] = GC
    BM = np.zeros((128, 4, 128), f32)
    for h in range(4):
        BM[h * 32:(h + 1) * 32, h, :] = 1.0
    secs["BM"] = BM.reshape(128, 512)
    LM = np.ones((128, 128), f32)
    LM[64:, :64] = 0.0
    secs["LASTM"] = LM
    secs["pw"] = np.broadcast_to((2.0 ** -(np.arange(32, dtype=np.float64) + 1.0))[None, :], (128, 32))
    secs["negh"] = np.full((128, 8), -0.5)
    cst = np.zeros((128, CW), f32)
    o = 0
    for nm, w in C_LAYOUT:
        cst[:, o:o + w] = np.asarray(secs[nm], dtype=f32)
        o += w
    s_ = np.arange(128)[:, None]
    t_ = np.arange(128)[None, :]
    rb = inp["rel_bias"]
    b0 = rb[_t5_bucket_np(s_ - t_)]
    b1 = rb[_t5_bucket_np(s_ - 128 - t_)]
    cb = rb[_t5_bucket_np(np.full((128, 128), -1000))]
    bias_t = np.stack([np.transpose(b, (0, 2, 1)).reshape(128, 1024) for b in (b0, b1, cb)]).astype(f32)
    tb = np.broadcast_to((-1e-30 * np.arange(S, dtype=np.float64)).astype(f32)[None, :], (128, S))
    return cst, np.ascontiguousarray(bias_t), np.ascontiguousarray(tb), _rot_tables(S)


_NC_CACHE = {}


def run_cores(inp, xs, S, NSEQ, KTOP):
    key = (S, NSEQ, KTOP)
    nc = bass.Bass("TRN2", target_bir_lowering=False)
    build(nc, S, NSEQ, KTOP)
    cst, bias_t, tb, rot = _host_consts(inp, S)
    base = {
        "w_in": np.ascontiguousarray(inp["w_in"][0]), "w_out": np.ascontiguousarray(inp["w_out"][0]),
        "w_gate": np.ascontiguousarray(inp["w_gate"][0]), "w_up": np.ascontiguousarray(inp["w_up"][0]),
        "w_down": np.ascontiguousarray(inp["w_down"][0]), "cst": cst, "bias_t": bias_t, "tb": tb, "rot": rot,
    }
    in_maps = [dict(base, x=np.ascontiguousarray(x)) for x in xs]
    res = run_bass_kernel_spmd(nc, in_maps, core_ids=list(range(len(xs))))
    return [r["y"] for r in res.results]


def kernel(**inputs):
    inp = {k: np.asarray(v) for k, v in inputs.items()}
    x = inp["x"]
    B, S, D = x.shape
    ncores = 8
    nseq = B // ncores
    xs = [x[c * nseq:(c + 1) * nseq].reshape(nseq * S, D) for c in range(ncores)]
    ys = run_cores(inp, xs, S, nseq, min(256, S // 4))
    return np.concatenate([y.reshape(nseq, S, D) for y in ys], axis=0).astype(np.float32)
```

```python
import contextlib
import math
import numpy as np
import concourse.bass as bass
import concourse.mybir as mybir
from concourse.bass_utils import run_bass_kernel_spmd

F32 = mybir.dt.float32
BF16 = mybir.dt.bfloat16
AF = mybir.ActivationFunctionType
ALU = mybir.AluOpType
AX = mybir.AxisListType

D_MODEL = 1024
HID = 2816
NHC = HID // 128
IN_W = 2852
RMS_EPS = 1e-6
GN_EPS = 1e-5
KIT = 20
CW = 3272
NEG = -3.0e38

ENGS = ("pe", "act", "dve", "pool", "sp")
CH = 20000
DCH = 1500


class Prog:
    def __init__(self, nc):
        self.nc = nc
        self.q = {e: [] for e in ENGS}
        self.n = {e: 0 for e in ENGS}
        self.seen = {e: {} for e in ENGS}
        self.lastw = {}
        self.readers = {}
        self.dcount = {}
        self.dbatch = {}
        self.dopen = {}

    def _deps(self, eng, reads, writes):
        deps = {}

        def add(src, idx, raw):
            if src == eng and eng in ("pe", "sp"):
                return
            if deps.get(src, -1) < idx:
                deps[src] = idx

        for r in reads:
            if r in self.lastw:
                add(*self.lastw[r], True)
        for w in writes:
            if w in self.lastw:
                add(*self.lastw[w], False)
            for (s, i) in self.readers.get(w, ()):
                add(s, i, False)
        out = []
        for src, idx in deps.items():
            if self.seen[eng].get(src, -1) >= idx:
                continue
            self.seen[eng][src] = idx
            out.append((src, idx))
        return out

    def _commit(self, sig, reads, writes):
        for r in reads:
            self.readers.setdefault(r, []).append(sig)
        for w in writes:
            self.lastw[w] = sig
            self.readers[w] = []

    def op(self, eng, fn, reads=(), writes=()):
        waits = self._deps(eng, reads, writes)
        idx = self.n[eng]
        self.n[eng] += 1
        self.q[eng].append(("op", fn, waits, idx))
        self._commit((eng, idx), reads, writes)

    def dma(self, queue, fn, stream, reads=(), writes=(), batch=False):
        waits = self._deps(queue, reads, writes)
        idx = self.dcount.get(stream, 0)
        self.dcount[stream] = idx + 1
        self.dbatch.setdefault(stream, []).append(None)
        self.dopen.setdefault(stream, []).append(idx)
        if not batch:
            self.close(stream)
        self.q[queue].append(("dma", fn, waits, (stream, idx)))
        self._commit(("dma:" + stream, idx), reads, writes)

    def close(self, stream):
        ids = self.dopen.get(stream, [])
        if ids:
            for i in ids:
                self.dbatch[stream][i] = ids[-1]
            self.dopen[stream] = []

    def emit(self):
        nc = self.nc
        for s in list(self.dopen):
            self.close(s)
        with contextlib.ExitStack() as st:
            sems = {}
            for e in ENGS:
                if e == "sp":
                    continue
                ne = max(1, (self.n[e] + CH - 1) // CH)
                sems[e] = [st.enter_context(nc.semaphore(f"s_{e}_{k}")) for k in range(ne)]
            for s, c in self.dcount.items():
                ne = max(1, (c + DCH - 1) // DCH)
                sems["dma:" + s] = [st.enter_context(nc.semaphore(f"d_{s}_{k}")) for k in range(ne)]

            def wait_args(src, idx):
                if src.startswith("dma:"):
                    end = self.dbatch[src[4:]][idx]
                    return sems[src][end // DCH], 16 * (end % DCH + 1)
                return sems[src][idx // CH], idx % CH + 1

            def run(name, eng):
                for kind, fn, waits, info in self.q[name]:
                    for (src, idx) in waits:
                        sem, val = wait_args(src, idx)
                        eng.wait_ge(sem, val)
                    ins = fn(eng)
                    if kind == "op":
                        ins.then_inc(sems[name][info // CH], 1)
                    else:
                        stream, idx = info
                        ins.then_inc(sems["dma:" + stream][idx // DCH], 16)
                if name == "sp":
                    for s, c in self.dcount.items():
                        sem, val = wait_args("dma:" + s, c - 1)
                        eng.wait_ge(sem, val)

            with nc.Block() as block:
                @block.tensor
                def _(eng):
                    run("pe", eng)

                @block.scalar
                def _(eng):
                    run("act", eng)

                @block.vector
                def _(eng):
                    run("dve", eng)

                @block.gpsimd
                def _(eng):
                    run("pool", eng)

                @block.sync
                def _(eng):
                    run("sp", eng)


def build(nc, S, NSEQ, KTOP):
    NT = S // 128
    GT = min(4, NT)
    NG = NT // GT
    LS = 2 if GT >= 2 else 1
    P = Prog(nc)

    def din(name, shape, dt=F32):
        return nc.dram_tensor(name, list(shape), dt, kind="ExternalInput").ap()

    x_d = din("x", [NSEQ * S, D_MODEL])
    w_in_d = din("w_in", [D_MODEL, IN_W])
    w_out_d = din("w_out", [D_MODEL, D_MODEL])
    w_gate_d = din("w_gate", [D_MODEL, HID])
    w_up_d = din("w_up", [D_MODEL, HID])
    w_down_d = din("w_down", [HID, D_MODEL])
    cst_d = din("cst", [128, CW])
    bias_d = din("bias_t", [3, 128, 1024])
    tb_d = din("tb", [128, 512])
    rot_d = din("rot", [NT, 128, 128])
    y_d = nc.dram_tensor("y", [NSEQ * S, D_MODEL], F32, kind="ExternalOutput").ap()
    wg_s = nc.dram_tensor("wg_s", [128, NHC, 8, 128], BF16, kind="Internal").ap()
    wu_s = nc.dram_tensor("wu_s", [128, NHC, 8, 128], BF16, kind="Internal").ap()
    wd_s = nc.dram_tensor("wd_s", [NHC, 128, D_MODEL], BF16, kind="Internal").ap()

    def sb(name, shape, dt):
        return nc.alloc_sbuf_tensor(name, list(shape), dt)

    Win = sb("Win", [128, 8, IN_W], BF16)
    Wout = sb("Wout", [128, 8, D_MODEL], BF16)
    Wukv = sb("Wukv", [128, 128], BF16)
    CST = sb("CST", [128, CW], F32)
    c_off = {}
    o = 0
    for nm, w in [("ident", 128), ("n1w", 8), ("n2w", 8), ("kvnw", 1), ("qnw", 64), ("knw", 64),
                  ("gnw", 512), ("gnb", 512), ("wuk", 64), ("wuv", 64), ("DQ", 512), ("DK", 512),
                  ("DKtok", 8), ("DMASK", 128), ("GC", 4), ("BM", 512), ("LASTM", 128), ("pw", 32),
                  ("negh", 8)]:
        c_off[nm] = (o, w)
        o += w
    assert o <= CW

    def C(nm, a=0, b=None):
        o0, w = c_off[nm]
        return CST[:, o0 + a: o0 + (w if b is None else b)]

    identb = sb("identb", [128, 128], BF16)
    Bp = sb("Bp", [128, 2, 1024], F32)
    TB = sb("TB", [128, 512], F32)
    ONESM = sb("ONESM", [128, 128], BF16)
    LASTM = sb("LASTMb", [128, 128], BF16)
    kT = sb("kT", [64, S], BF16)
    vA = sb("vA", [128, NT, 65], BF16)
    ikT4 = sb("ikT4", [128, S], BF16)
    stf = sb("stf", [128, 4, 64], F32)
    stb = sb("stb", [128, 4, 64], BF16)
    X = sb("X", [128, GT, D_MODEL], F32)
    xT = sb("xT", [128, 8, GT * 128], BF16)
    qT = sb("qT", [64, GT, 1024], BF16)
    iqTb = sb("iqTb", [128, GT, 4, 128], BF16)
    obT = sb("obT", [128, GT, 4, 128], BF16)
    aw = sb("aw", [128, GT, 4], F32)
    sgn = sb("sgn", [128, GT, 4], F32)
    ROT = sb("ROT", [128, 128], F32)
    xb = sb("xb", [128, D_MODEL], BF16)
    NF = 4
    ftmp = [sb(f"ft{i}", [128, 512], F32) for i in range(NF)]
    NB = 4
    btmp = [sb(f"bt{i}", [128, 512], BF16) for i in range(NB)]
    qbTL = [sb(f"qbT{i}", [128, 4, 128], BF16) for i in range(2)]
    kbTL = [sb(f"kbT{i}", [128, 4, 128], BF16) for i in range(2)]
    vtokL = [sb(f"vtok{i}", [128, 512], BF16) for i in range(2)]
    ktokL = [sb(f"ktok{i}", [128, 512], BF16) for i in range(2)]
    sgtL = [sb(f"sgt{i}", [128, 512], BF16) for i in range(2)]
    scb = [sb("scb0", [128, 256], BF16)]
    xb2 = sb("xb2", [128, 128], BF16)
    DMASKb = sb("DMASKb", [128, 128], BF16)
    cT = sb("cT", [128, 128], BF16)
    scm = [sb(f"scm{i}", [128, 256], BF16) for i in range(2)]
    dS = sb("dS", [128, 64], F32)
    catT = sb("catT", [128, 8, 128], BF16)
    NE = 3
    Eb = [sb(f"E{i}", [128, 512], BF16) for i in range(NE)]
    Pm = [sb(f"Pm{i}", [128, 512], BF16) for i in range(NE)]
    st = sb("st", [128, 64], F32)
    bs = sb("bs", [128, 8 * LS + LS * KIT + 16], F32)
    RW = 9216
    R = sb("R", [128, RW], F32)
    Rb = R[:].bitcast(BF16)
    sc = R[:, 0:LS * S].rearrange("p (a s) -> p a s", a=LS)
    msk = Rb[:, 2 * LS * S: 2 * LS * S + S]
    mskT = Rb[:, 2 * LS * S + S: 2 * LS * S + 2 * S].rearrange("p (a b) -> p a b", b=128)
    assert 2 * LS * S + 2 * S <= 2 * RW
    hT = Rb[:, 0:NHC * GT * 128].rearrange("p (c t) -> p c t", c=NHC)
    o_w = NHC * 512
    wgs = [Rb[:, o_w + s * 2048: o_w + s * 2048 + 1024].rearrange("p (k j) -> p k j", k=8) for s in range(2)]
    wus = [Rb[:, o_w + s * 2048 + 1024: o_w + s * 2048 + 2048].rearrange("p (k j) -> p k j", k=8) for s in range(2)]
    NWD = 3
    wds = [Rb[:, o_w + 4096 + s * 1024: o_w + 4096 + (s + 1) * 1024] for s in range(NWD)]
    assert o_w + 4096 + NWD * 1024 <= 2 * RW
    stg = [R[:, s * IN_W:(s + 1) * IN_W] for s in range(2)]
    stgb = [Rb[:, 2 * 2 * IN_W + s * HID: 2 * 2 * IN_W + (s + 1) * HID] for s in range(2)]
    assert 4 * IN_W + 2 * HID <= 2 * RW
    Gp = [nc.alloc_psum_tensor(f"G{i}", [128, 512], F32) for i in range(4)]
    PV = [nc.alloc_psum_tensor(f"PV{i}", [128, 512], F32) for i in range(2)]
    Tp = nc.alloc_psum_tensor("Tp", [128, 1024], BF16)
    Yp = nc.alloc_psum_tensor("Yp", [128, 512], F32)
    gctr = [0]

    def nextG():
        i = gctr[0] % 4
        gctr[0] += 1
        return Gp[i], f"G{i}"

    fctr = [0]

    def nextF():
        i = fctr[0] % NF
        fctr[0] += 1
        return ftmp[i], f"ft{i}"

    bctr = [0]

    def nextB():
        i = bctr[0] % NB
        bctr[0] += 1
        return btmp[i], f"bt{i}"

    op = P.op
    RA = ["RA"]
    RB = ["RB"]

    P.dma("sp", lambda e: e.dma_start(out=CST[:], in_=cst_d), "init", writes=["CST"], batch=True)
    P.dma("sp", lambda e: e.dma_start(out=TB[:], in_=tb_d), "init", writes=["TB"], batch=True)
    P.dma("sp", lambda e: e.dma_start(out=Bp[:, 0, :], in_=bias_d[0]), "init", writes=["Bp0"], batch=True)
    P.dma("sp", lambda e: e.dma_start(out=Bp[:, 1, :], in_=bias_d[1]), "init", writes=["Bp1"], batch=True)
    P.dma("sp", lambda e: e.dma_start(out=ftmp[0][:], in_=bias_d[2][:, 0:512]), "init", writes=["ft0"], batch=True)
    P.dma("sp", lambda e: e.dma_start(out=ftmp[1][:], in_=bias_d[2][:, 512:1024]), "init", writes=["ft1"], batch=True)
    P.close("init")
    op("dve", lambda e: e.tensor_copy(out=identb[:], in_=C("ident")), ["CST"], ["identb"])
    op("dve", lambda e: e.tensor_copy(out=LASTM[:], in_=C("LASTM")), ["CST"], ["LASTM"])
    op("dve", lambda e: e.memset(ONESM[:], 1.0), [], ["ONESM"])
    op("dve", lambda e: e.tensor_copy(out=DMASKb[:], in_=C("DMASK")), ["CST"], ["DMASKb"])
    op("dve", lambda e: e.tensor_scalar(out=C("gnw"), in0=C("gnw"), scalar1=0.5, scalar2=None, op0=ALU.mult), ["CST"], ["CST"])
    op("dve", lambda e: e.tensor_scalar(out=C("gnb"), in0=C("gnb"), scalar1=0.5, scalar2=None, op0=ALU.mult), ["CST"], ["CST"])
    op("dve", lambda e: e.memset(bs[:], 0.0), [], ["bs"])
    op("dve", lambda e: e.memset(st[:], 0.0), [], ["stall"])
    for kd in range(2):
        for hf in range(2):
            op("dve", lambda e, kd=kd, hf=hf: e.tensor_tensor(
                out=Bp[:, kd, hf * 512:(hf + 1) * 512], in0=Bp[:, kd, hf * 512:(hf + 1) * 512],
                in1=ftmp[hf][:], op=ALU.subtract), [f"Bp{kd}", f"ft{hf}"], [f"Bp{kd}"])
    op("dve", lambda e: e.tensor_scalar(out=Wukv[:], in0=CST[:, c_off["wuk"][0]: c_off["wuk"][0] + 128],
                                        scalar1=C("kvnw"), scalar2=None, op0=ALU.mult), ["CST"], ["Wukv"])
    op("pool", lambda e: e.memset(vA[:, :, 64:65], 1.0), [], [f"vA{t}" for t in range(NT)])
    ecyc = ["dve", "pool"]
    ei = [0]

    def ceng():
        ei[0] += 1
        return ecyc[ei[0] % 2]

    for k in range(8):
        s_ = k % 2
        P.dma("sp", lambda e, k=k, s_=s_: e.dma_start(out=stg[s_], in_=w_in_d[k * 128:(k + 1) * 128, :]),
              f"stg{s_}", reads=[], writes=[f"stg{s_}"])
        for (a, b, dst) in [(0, 512, 0), (512, 804, 2560), (804, 2852, 512)]:
            en = ceng()
            op(en, lambda e, k=k, s_=s_, a=a, b=b, dst=dst: e.tensor_scalar(
                out=Win[:, k, dst:dst + (b - a)], in0=stg[s_][:, a:b], scalar1=C("n1w", k, k + 1), scalar2=None,
                op0=ALU.mult), [f"stg{s_}", "CST"], ["Win"])
    for k in range(8):
        s_ = k % 2
        P.dma("sp", lambda e, k=k, s_=s_: e.dma_start(out=stg[s_][:, 0:1024], in_=w_out_d[k * 128:(k + 1) * 128, :]),
              f"stg{s_}", writes=[f"stg{s_}"])
        op(ceng(), lambda e, k=k, s_=s_: e.tensor_copy(out=Wout[:, k, :], in_=stg[s_][:, 0:1024]),
           [f"stg{s_}"], ["Wout"])
    wst_keys = []
    for (src_d, dst_d, nm) in [(w_gate_d, wg_s, "wg_s"), (w_up_d, wu_s, "wu_s")]:
        for k in range(8):
            s_ = k % 2
            P.dma("sp", lambda e, k=k, s_=s_, src_d=src_d: e.dma_start(out=stg[s_][:, 0:HID], in_=src_d[k * 128:(k + 1) * 128, :]),
                  f"stg{s_}", writes=[f"stg{s_}"])
            op(ceng(), lambda e, k=k, s_=s_: e.tensor_scalar(
                out=stgb[s_], in0=stg[s_][:, 0:HID], scalar1=C("n2w", k, k + 1), scalar2=None, op0=ALU.mult),
               [f"stg{s_}", "CST"], [f"stgb{s_}"])
            P.dma("sp", lambda e, k=k, s_=s_, dst_d=dst_d: e.dma_start(
                out=dst_d[:, :, k, :], in_=stgb[s_].rearrange("p (c j) -> p c j", j=128)),
                f"stgb{s_}", reads=[f"stgb{s_}"], writes=[f"{nm}{k}"])
            wst_keys.append(f"{nm}{k}")
    for c2 in range(NHC // 2):
        s_ = c2 % 2
        P.dma("sp", lambda e, c2=c2, s_=s_: e.dma_start(
            out=stg[s_][:, 0:2048].rearrange("p (c n) -> p c n", c=2),
            in_=w_down_d[c2 * 256:(c2 + 1) * 256, :].rearrange("(c p) n -> p c n", p=128)),
            f"stg{s_}", writes=[f"stg{s_}"])
        op(ceng(), lambda e, s_=s_: e.tensor_copy(out=stgb[s_][:, 0:2048], in_=stg[s_][:, 0:2048]),
           [f"stg{s_}"], [f"stgb{s_}"])
        P.dma("sp", lambda e, c2=c2, s_=s_: e.dma_start(
            out=wd_s[2 * c2:2 * c2 + 2].rearrange("c p n -> p c n"),
            in_=stgb[s_][:, 0:2048].rearrange("p (c n) -> p c n", c=2)),
            f"stgb{s_}", reads=[f"stgb{s_}"], writes=[f"wd_s{c2}"])
        wst_keys.append(f"wd_s{c2}")
    op("pool", lambda e: e.memset(st[:, 60:61], 0.0), wst_keys, ["stg0", "stg1", "stgb0", "stgb1", "RA", "RB", "wg_s", "wu_s", "wd_s"])

    def rstd_from_ss(col, inv_n, eps):
        op("dve", lambda e: e.tensor_scalar(out=st[:, col:col + 1], in0=st[:, col:col + 1], scalar1=inv_n,
                                            scalar2=eps, op0=ALU.mult, op1=ALU.add), [f"st{col}"], [f"st{col}"])
        op("pool", lambda e: e.tensor_tensor(out=st[:, col:col + 1], in0=st[:, col:col + 1], in1=C("negh", 0, 1),
                                             op=ALU.pow), [f"st{col}", "CST"], [f"st{col}"])

    def transposes(src_aps, dst_ap, dst_keys, src_keys, rows=128, cols=128, evac="act"):
        n = len(src_aps)
        for i, a in enumerate(src_aps):
            op("pe", lambda e, i=i, a=a: e.transpose(out=Tp[0:cols, i * 128:(i + 1) * 128], in_=a, identity=identb[:]),
               src_keys + ["identb"], ["Tp"])
        srcv = Tp[0:cols, 0:n * 128].rearrange("p (a b) -> p a b", b=128)
        if evac == "act":
            op("act", lambda e: e.copy(out=dst_ap, in_=srcv), ["Tp"], dst_keys)
        else:
            op("dve", lambda e: e.tensor_copy(out=dst_ap, in_=srcv), ["Tp"], dst_keys)


    def retention(j, ti):
        l = j % 2
        qb_, kb_, vt_, kt_, sg_ = qbTL[l], kbTL[l], vtokL[l], ktokL[l], sgtL[l]
        for pq in range(4):
            sm = scm[pq % 2]
            smk = f"scm{pq % 2}"
            sb_, sbk = scb[0], "scb0"
            for hh in range(2):
                r0, r1 = hh * 64, hh * 64 + 64
                g, gk = nextG()
                op("pe", lambda e, g=g, pq=pq, r0=r0, r1=r1: e.matmul(
                    g[:, 0:128], lhsT=kb_[r0:r1, pq, :], rhs=qb_[r0:r1, pq, :], start=True, stop=True),
                   [f"kbT{l}", f"qbT{l}"], [gk])
                op("act", lambda e, g=g, sb_=sb_, hh=hh: e.copy(out=sb_[:, hh * 128:(hh + 1) * 128], in_=g[:, 0:128]), [gk], [sbk])
            op("pool", lambda e, sb_=sb_, sm=sm: e.tensor_tensor(
                out=sm[:].rearrange("p (a b) -> p a b", a=2), in0=sb_[:].rearrange("p (a b) -> p a b", a=2),
                in1=DMASKb[:].unsqueeze(1).broadcast_to([128, 2, 128]), op=ALU.mult), [sbk, "DMASKb"], [smk])
            for hh in range(2):
                h = 2 * pq + hh
                r0, r1 = hh * 64, hh * 64 + 64
                op("pe", lambda e, sm=sm, h=h, hh=hh: e.matmul(
                    Yp[:, h * 64:(h + 1) * 64], lhsT=sm[:, hh * 128:(hh + 1) * 128], rhs=vt_[:, h * 64:(h + 1) * 64],
                    start=True, stop=False), [smk, f"vtok{l}"], ["Yp"])
                op("pe", lambda e, pq=pq, r0=r0, r1=r1, h=h: e.matmul(
                    Yp[:, h * 64:(h + 1) * 64], lhsT=qb_[r0:r1, pq, :], rhs=stb[r0:r1, pq, :], start=False, stop=True),
                   [f"qbT{l}", "stb"], ["Yp"])
            g, gk = nextG()
            op("pe", lambda e, g=g, pq=pq: e.matmul(
                g[:, 0:128], lhsT=kt_[:, pq * 128:(pq + 1) * 128], rhs=vt_[:, pq * 128:(pq + 1) * 128],
                start=True, stop=True), [f"ktok{l}", f"vtok{l}"], [gk])
            for hh in range(2):
                r0, r1 = hh * 64, hh * 64 + 64
                op("act", lambda e, g=g, r0=r0, r1=r1, pq=pq: e.activation(
                    out=dS[r0:r1, :], in_=g[r0:r1, r0:r1], func=AF.Copy, scale=C("GC", pq, pq + 1)[r0:r1, :]),
                   [gk, "CST"], ["dS"])
            op("pool", lambda e, pq=pq: e.tensor_scalar(out=stf[:, pq, :], in0=stf[:, pq, :], scalar1=C("GC", pq, pq + 1),
                                                        scalar2=None, op0=ALU.mult), ["stf", "CST"], ["stf"])
            op("pool", lambda e, pq=pq: e.tensor_tensor(out=stf[:, pq, :], in0=stf[:, pq, :], in1=dS[:], op=ALU.add),
               ["stf", "dS"], ["stf"])
            op("pool", lambda e, pq=pq: e.tensor_copy(out=stb[:, pq, :], in_=stf[:, pq, :]), ["stf"], ["stb"])
        for h in range(8):
            op("act", lambda e, h=h: e.activation(out=xb2[:, 0:64], in_=Yp[:, h * 64:(h + 1) * 64], func=AF.Identity,
                                                  accum_out=st[:, 16 + h:17 + h]), ["Yp"], ["xb2", "st16"])
            op("act", lambda e, h=h: e.activation(out=xb2[:, 64:128], in_=Yp[:, h * 64:(h + 1) * 64], func=AF.Square,
                                                  accum_out=st[:, 24 + h:25 + h]), ["Yp"], ["xb2", "st24"])
        op("pool", lambda e: e.tensor_scalar(out=st[:, 16:24], in0=st[:, 16:24], scalar1=1.0 / 64, scalar2=None,
                                             op0=ALU.mult), ["st16"], ["st16"])
        op("pool", lambda e: e.tensor_tensor(out=st[:, 32:40], in0=st[:, 16:24], in1=st[:, 16:24], op=ALU.mult),
           ["st16"], ["st32"])
        op("pool", lambda e: e.tensor_scalar(out=st[:, 24:32], in0=st[:, 24:32], scalar1=1.0 / 64, scalar2=GN_EPS,
                                             op0=ALU.mult, op1=ALU.add), ["st24"], ["st24"])
        op("pool", lambda e: e.tensor_tensor(out=st[:, 24:32], in0=st[:, 24:32], in1=st[:, 32:40], op=ALU.subtract),
           ["st24", "st32"], ["st24"])
        op("pool", lambda e: e.tensor_tensor(out=st[:, 24:32], in0=st[:, 24:32], in1=C("negh"), op=ALU.pow),
           ["st24", "CST"], ["st24"])
        op("pool", lambda e: e.tensor_tensor(out=st[:, 32:40], in0=st[:, 16:24], in1=st[:, 24:32], op=ALU.mult),
           ["st16", "st24"], ["st32"])
        op("pool", lambda e: e.tensor_scalar(out=st[:, 32:40], in0=st[:, 32:40], scalar1=-1.0, scalar2=None, op0=ALU.mult),
           ["st32"], ["st32"])
        yn, ynk = nextF()
        for h in range(8):
            op("act", lambda e, h=h, yn=yn: e.activation(out=yn[:, h * 64:(h + 1) * 64], in_=Yp[:, h * 64:(h + 1) * 64],
                                                         func=AF.Identity, scale=st[:, 24 + h:25 + h], bias=st[:, 32 + h:33 + h]),
               ["Yp", "st24", "st32"], [ynk])
        op("pool", lambda e, yn=yn: e.tensor_tensor(out=yn[:], in0=yn[:], in1=C("gnw"), op=ALU.mult), [ynk, "CST"], [ynk])
        op("pool", lambda e, yn=yn: e.tensor_tensor(out=yn[:], in0=yn[:], in1=C("gnb"), op=ALU.add), [ynk, "CST"], [ynk])
        ob, obk = nextB()
        op("pool", lambda e, yn=yn, ob=ob: e.tensor_tensor(out=ob[:], in0=yn[:], in1=sg_[:], op=ALU.mult), [ynk, f"sgt{l}"], [obk])
        transposes([ob[:, c * 128:(c + 1) * 128] for c in range(4)], obT[:, j, :, :], [f"obT{j}"], [obk])

    for seq in range(NSEQ):
        op("pool", lambda e: e.memset(stf[:], 0.0), [], ["stf"])
        op("pool", lambda e: e.memset(stb[:], 0.0), [], ["stb"])
        for gi in range(NG):
            tiles = [gi * GT + j for j in range(GT)]
            op("pool", lambda e: e.memset(st[:, 61:62], 0.0), [], RA + RB)
            for j, ti in enumerate(tiles):
                row0 = seq * S + ti * 128
                pos = ti * 128
                Xj = f"X{j}"
                P.dma("sp", lambda e, j=j, row0=row0: e.dma_start(out=X[:, j, :], in_=x_d[row0:row0 + 128, :]),
                      f"x{j}", writes=[Xj])
                P.dma("sp", lambda e, ti=ti: e.dma_start(out=ROT[:], in_=rot_d[ti]), "rot", writes=["ROT"])
                op("act", lambda e, j=j: e.activation(out=xb[:], in_=X[:, j, :], func=AF.Square, accum_out=st[:, 0:1]),
                   [Xj], ["xb", "st0"])
                rstd_from_ss(0, 1.0 / D_MODEL, RMS_EPS)
                op("act", lambda e, j=j: e.activation(out=xb[:], in_=X[:, j, :], func=AF.Copy, scale=st[:, 0:1]),
                   [Xj, "st0"], ["xb"])
                transposes([xb[:, k * 128:(k + 1) * 128] for k in range(8)], xT[:, :, j * 128:(j + 1) * 128],
                           [f"xT{j}"], ["xb"])
                zb = []
                for b in range(6):
                    w_ = 512 if b < 5 else IN_W - 2560
                    g, gk = nextG()
                    for k in range(8):
                        op("pe", lambda e, g=g, k=k, j=j, b=b, w_=w_: e.matmul(
                            g[:, 0:w_], lhsT=xT[:, k, j * 128:(j + 1) * 128], rhs=Win[:, k, b * 512:b * 512 + w_],
                            start=(k == 0), stop=(k == 7)), [f"xT{j}", "Win"], [gk])
                    if b == 0:
                        f1, f1k = nextF()
                        op("act", lambda e, g=g, f1=f1: e.activation(out=f1[:], in_=g[:], func=AF.Square), [gk], [f1k])
                        op("dve", lambda e, f1=f1: e.tensor_reduce(out=st[:, 8:16], in_=f1[:].rearrange("p (h d) -> p h d", h=8),
                                                                   axis=AX.X, op=ALU.add), [f1k], ["st8"])
                        op("dve", lambda e: e.tensor_scalar(out=st[:, 8:16], in0=st[:, 8:16], scalar1=1.0 / 64, scalar2=RMS_EPS,
                                                            op0=ALU.mult, op1=ALU.add), ["st8"], ["st8"])
                        op("pool", lambda e: e.tensor_tensor(out=st[:, 8:16], in0=st[:, 8:16], in1=C("negh"), op=ALU.pow),
                           ["st8", "CST"], ["st8"])
                        f2, f2k = nextF()
                        op("dve", lambda e, g=g, f2=f2: e.tensor_tensor(
                            out=f2[:].rearrange("p (h d) -> p h d", h=8), in0=g[:].rearrange("p (h d) -> p h d", h=8),
                            in1=st[:, 8:16].unsqueeze(2).broadcast_to([128, 8, 64]), op=ALU.mult), [gk, "st8"], [f2k])
                        b1, b1k = nextB()
                        op("pool", lambda e, f2=f2, b1=b1: e.tensor_tensor(
                            out=b1[:].rearrange("p (h d) -> p h d", h=8), in0=f2[:].rearrange("p (h d) -> p h d", h=8),
                            in1=C("qnw").unsqueeze(1).broadcast_to([128, 8, 64]), op=ALU.mult), [f2k, "CST"], [b1k])
                        transposes([b1[:, h * 64:(h + 1) * 64] for h in range(8)],
                                   qT[:, j, :].rearrange("p (a b) -> p a b", b=128), [f"qT{j}"], [b1k], cols=64)
                    elif b in (1, 2):
                        fa, fak = nextF()
                        fb, fbk = nextF()
                        g3 = g[:].rearrange("p (h d) -> p h d", h=8)
                        op("dve", lambda e, fa=fa, g3=g3: e.tensor_tensor(
                            out=fa[:].rearrange("p (h d) -> p h d", h=8), in0=g3,
                            in1=ROT[:, 0:64].unsqueeze(1).broadcast_to([128, 8, 64]), op=ALU.mult), [gk, "ROT"], [fak])
                        op("dve", lambda e, fb=fb, g3=g3: e.tensor_tensor(
                            out=fb[:].rearrange("p (h d) -> p h d", h=8)[:, :, 0:32], in0=g3[:, :, 32:64],
                            in1=ROT[:, 64:96].unsqueeze(1).broadcast_to([128, 8, 32]), op=ALU.mult), [gk, "ROT"], [fbk])
                        op("dve", lambda e, fb=fb, g3=g3: e.tensor_tensor(
                            out=fb[:].rearrange("p (h d) -> p h d", h=8)[:, :, 32:64], in0=g3[:, :, 0:32],
                            in1=ROT[:, 96:128].unsqueeze(1).broadcast_to([128, 8, 32]), op=ALU.mult), [gk, "ROT", fbk], [fbk])
                        rb_, rbk = nextB()
                        op("pool", lambda e, fa=fa, fb=fb, rb_=rb_: e.tensor_tensor(out=rb_[:], in0=fa[:], in1=fb[:], op=ALU.add),
                           [fak, fbk], [rbk])
                        dstT, dk_, dcn = (qbTL[j % 2], f"qbT{j % 2}", "DQ") if b == 1 else (kbTL[j % 2], f"kbT{j % 2}", "DK")
                        for pq in range(4):
                            op("pe", lambda e, pq=pq, rb_=rb_: e.transpose(out=Tp[:, pq * 128:(pq + 1) * 128],
                                                                          in_=rb_[:, pq * 128:(pq + 1) * 128], identity=identb[:]),
                               [rbk, "identb"], ["Tp"])
                        op("dve", lambda e, dstT=dstT, dcn=dcn: e.tensor_tensor(
                            out=dstT[:].rearrange("p a b -> p (a b)"), in0=Tp[:, 0:512], in1=C(dcn), op=ALU.mult),
                           ["Tp", "CST"], [dk_])
                        if b == 2:
                            kt_, ktk = ktokL[j % 2], f"ktok{j % 2}"
                            op("pool", lambda e, rb_=rb_, kt_=kt_: e.tensor_tensor(
                                out=kt_[:].rearrange("p (h d) -> p h d", h=8), in0=rb_[:].rearrange("p (h d) -> p h d", h=8),
                                in1=C("DKtok").unsqueeze(2).broadcast_to([128, 8, 64]), op=ALU.mult), [rbk, "CST"], [ktk])
                    elif b == 3:
                        vt_, vtk = vtokL[j % 2], f"vtok{j % 2}"
                        op("act", lambda e, g=g, vt_=vt_: e.copy(out=vt_[:], in_=g[:]), [gk], [vtk])
                    elif b == 4:
                        th, thk = nextF()
                        op("act", lambda e, g=g, th=th: e.activation(out=th[:], in_=g[:], func=AF.Tanh, scale=0.5), [gk], [thk])
                        op("dve", lambda e, g=g, th=th, j=j: e.scalar_tensor_tensor(out=sgtL[j % 2][:], in0=th[:], scalar=1.0, in1=g[:],
                                                                                    op0=ALU.add, op1=ALU.mult), [gk, thk], [f"sgt{j % 2}"])
                    else:
                        op("act", lambda e, g=g: e.activation(out=xb[:, 0:128], in_=g[:, 0:128], func=AF.Square,
                                                              accum_out=st[:, 1:2]), [gk], ["xb", "st1"])
                        rstd_from_ss(1, 1.0 / 128, RMS_EPS)
                        cb_, cbk = nextB()
                        op("act", lambda e, g=g, cb_=cb_: e.activation(out=cb_[:, 0:128], in_=g[:, 0:128], func=AF.Copy,
                                                                       scale=st[:, 1:2]), [gk, "st1"], [cbk])
                        op("act", lambda e, g=g, cb_=cb_: e.copy(out=cb_[:, 128:256], in_=g[:, 128:256]), [gk], [cbk])
                        op("act", lambda e, g=g, cb_=cb_: e.copy(
                            out=cb_[:, 256:384].rearrange("p (a b) -> p a b", a=4),
                            in_=g[:, 256:288].unsqueeze(1).broadcast_to([128, 4, 32])), [gk], [cbk])
                        op("dve", lambda e, g=g, j=j: e.tensor_scalar(out=sgn[:, j, :], in0=g[:, 288:292], scalar1=0.0,
                                                                      scalar2=-0.5, op0=ALU.is_ge, op1=ALU.add), [gk], [f"sgn{j}"])
                        op("dve", lambda e, g=g, j=j: e.scalar_tensor_tensor(out=aw[:, j, :], in0=g[:, 288:292], scalar=2.0,
                                                                             in1=sgn[:, j, :], op0=ALU.mult, op1=ALU.mult),
                           [gk, f"sgn{j}"], [f"aw{j}"])
                        op("pe", lambda e, cb_=cb_: e.transpose(out=Tp[:, 0:128], in_=cb_[:, 0:128], identity=identb[:]),
                           [cbk, "identb"], ["Tp"])
                        op("act", lambda e: e.copy(out=cT[:], in_=Tp[:, 0:128]), ["Tp"], ["cT"])
                        g2, g2k = nextG()
                        op("pe", lambda e, g2=g2: e.matmul(g2[:, 0:128], lhsT=cT[:], rhs=Wukv[:], start=True, stop=True),
                           ["cT", "Wukv"], [g2k])
                        op("act", lambda e, g2=g2: e.activation(out=xb[:, 0:64], in_=g2[:, 0:64], func=AF.Square,
                                                                accum_out=st[:, 2:3]), [g2k], ["xb", "st2"])
                        rstd_from_ss(2, 1.0 / 64, RMS_EPS)
                        op("dve", lambda e, g2=g2, cb_=cb_: e.scalar_tensor_tensor(
                            out=cb_[:, 384:448], in0=g2[:, 0:64], scalar=st[:, 2:3], in1=C("knw"), op0=ALU.mult, op1=ALU.mult),
                           [g2k, "st2", "CST"], [cbk])
                        op("act", lambda e, g2=g2, ti=ti: e.copy(out=vA[:, ti, 0:64], in_=g2[:, 64:128]), [g2k], [f"vA{ti}"])
                        op("pe", lambda e, cb_=cb_: e.transpose(out=Tp[0:64, 0:128], in_=cb_[:, 384:448], identity=identb[:]),
                           [cbk, "identb"], ["Tp"])
                        op("act", lambda e, pos=pos: e.copy(out=kT[:, pos:pos + 128], in_=Tp[0:64, 0:128]), ["Tp"], [f"kT{ti}"])
                        op("pe", lambda e, cb_=cb_: e.transpose(out=Tp[:, 0:128], in_=cb_[:, 128:256], identity=identb[:]),
                           [cbk, "identb"], ["Tp"])
                        op("dve", lambda e, j=j: e.tensor_tensor(
                            out=iqTb[:, j, :, :], in0=Tp[:, 0:128].unsqueeze(1).broadcast_to([128, 4, 128]),
                            in1=C("BM").rearrange("p (a b) -> p a b", a=4), op=ALU.mult), ["Tp", "CST"], [f"iqTb{j}"])
                        op("pe", lambda e, cb_=cb_: e.transpose(out=Tp[:, 0:128], in_=cb_[:, 256:384], identity=identb[:]),
                           [cbk, "identb"], ["Tp"])
                        op("act", lambda e, pos=pos: e.copy(out=ikT4[:, pos:pos + 128], in_=Tp[:, 0:128]), ["Tp"], [f"ik{ti}"])

                N = (ti + 1) * 128
                if N > KTOP:
                    ls = j % LS
                    for kb in range((N + 511) // 512):
                        k0 = kb * 512
                        w_ = min(512, N - k0)
                        for h in range(4):
                            g, gk = nextG()
                            op("pe", lambda e, g=g, j=j, h=h, k0=k0, w_=w_: e.matmul(
                                g[:, 0:w_], lhsT=iqTb[:, j, h, :], rhs=ikT4[:, k0:k0 + w_], start=True, stop=True),
                               [f"iqTb{j}"] + [f"ik{q}" for q in range(k0 // 128, (k0 + w_) // 128)], [gk])
                            op("act", lambda e, g=g, j=j, h=h, w_=w_: e.activation(
                                out=g[:, 0:w_], in_=g[:, 0:w_], func=AF.Relu, scale=aw[:, j, h:h + 1]), [gk, f"aw{j}"], [gk])
                            if h == 0:
                                op("dve", lambda e, g=g, j=j, ls=ls, k0=k0, w_=w_: e.scalar_tensor_tensor(
                                    out=sc[:, ls, k0:k0 + w_], in0=g[:, 0:w_], scalar=sgn[:, j, 0:1], in1=TB[:, 0:w_],
                                    op0=ALU.mult, op1=ALU.add), [gk, f"sgn{j}", "TB"] + RA, [f"sc{ls}"])
                            else:
                                op("dve", lambda e, g=g, j=j, ls=ls, k0=k0, w_=w_, h=h: e.scalar_tensor_tensor(
                                    out=sc[:, ls, k0:k0 + w_], in0=g[:, 0:w_], scalar=sgn[:, j, h:h + 1], in1=sc[:, ls, k0:k0 + w_],
                                    op0=ALU.mult, op1=ALU.add), [gk, f"sgn{j}", f"sc{ls}"] + RA, [f"sc{ls}"])
                        if kb > 0:
                            op("pool", lambda e, ls=ls, k0=k0, w_=w_, kb=kb: e.tensor_scalar(
                                out=sc[:, ls, k0:k0 + w_], in0=sc[:, ls, k0:k0 + w_], scalar1=-1e-30 * 512 * kb, scalar2=None,
                                op0=ALU.add), [f"sc{ls}"] + RA, [f"sc{ls}"])

                if (j + 1) % LS == 0:
                    sub = list(range(j + 1 - LS, j + 1))
                    bl = [(jj, tiles[jj]) for jj in sub if (tiles[jj] + 1) * 128 > KTOP]
                    nb_ = len(bl)
                    MX, MN, RNG, MID, CNT, PMH, THR = 0, LS, 2 * LS, 3 * LS, 4 * LS, 5 * LS, 6 * LS
                    STP = 8 * LS
                    if nb_ > 0:
                        for (jj, tt) in bl:
                            ls = jj % LS
                            N = (tt + 1) * 128
                            op("dve", lambda e, ls=ls, N=N: e.tensor_reduce(out=bs[:, MX + ls:MX + ls + 1], in_=sc[:, ls, 0:N],
                                                                             axis=AX.X, op=ALU.max), [f"sc{ls}"], ["bs"])
                            op("dve", lambda e, ls=ls, N=N: e.tensor_reduce(out=bs[:, MN + ls:MN + ls + 1], in_=sc[:, ls, 0:N],
                                                                             axis=AX.X, op=ALU.min), [f"sc{ls}"], ["bs"])
                            op("dve", lambda e, ls=ls, N=N: e.memset(sc[0:64, ls, N - 64:N], NEG), [f"sc{ls}"], [f"sc{ls}"])
                        if nb_ < LS:
                            for ls in range(LS):
                                if ls not in [jj % LS for (jj, _) in bl]:
                                    op("dve", lambda e, ls=ls: e.memset(bs[:, MX + ls:MX + ls + 1], 1.0), [], ["bs"])
                                    op("dve", lambda e, ls=ls: e.memset(bs[:, MN + ls:MN + ls + 1], 0.0), [], ["bs"])
                        op("dve", lambda e: e.tensor_tensor(out=bs[:, RNG:RNG + LS], in0=bs[:, MX:MX + LS], in1=bs[:, MN:MN + LS],
                                                            op=ALU.subtract), ["bs"], ["bs"])
                        op("dve", lambda e: e.scalar_tensor_tensor(out=bs[:, MID:MID + LS], in0=bs[:, RNG:RNG + LS], scalar=0.5,
                                                                   in1=bs[:, MN:MN + LS], op0=ALU.mult, op1=ALU.add), ["bs"], ["bs"])
                        op("dve", lambda e: e.tensor_tensor(
                            out=bs[:, STP:STP + LS * KIT].rearrange("p (a k) -> p a k", a=LS),
                            in0=bs[:, RNG:RNG + LS].unsqueeze(2).broadcast_to([128, LS, KIT]),
                            in1=C("pw", 0, KIT).unsqueeze(1).broadcast_to([128, LS, KIT]), op=ALU.mult), ["bs", "CST"], ["bs"])
                        stp3 = bs[:, STP:STP + LS * KIT].rearrange("p (a k) -> p a k", a=LS)
                        for it in range(KIT):
                            for (jj, tt) in bl:
                                ls = jj % LS
                                N = (tt + 1) * 128
                                op("dve", lambda e, ls=ls, N=N: e.tensor_scalar(
                                    out=msk[:, 0:N], in0=sc[:, ls, 0:N], scalar1=bs[:, MID + ls:MID + ls + 1], scalar2=None,
                                    op0=ALU.is_ge, op1=ALU.add, accum_out=bs[:, CNT + ls:CNT + ls + 1]),
                                   [f"sc{ls}", "bs"] + RA, ["msk", "bs"])
                            op("dve", lambda e: e.tensor_scalar(out=bs[:, PMH:PMH + LS], in0=bs[:, CNT:CNT + LS],
                                                                scalar1=float(KTOP) - 0.5, scalar2=-0.5, op0=ALU.is_ge, op1=ALU.add),
                               ["bs"], ["bs"])
                            op("dve", lambda e, it=it: e.tensor_tensor(out=bs[:, PMH:PMH + LS], in0=bs[:, PMH:PMH + LS],
                                                                       in1=stp3[:, :, it], op=ALU.mult), ["bs"], ["bs"])
                            op("dve", lambda e: e.tensor_tensor(out=bs[:, MID:MID + LS], in0=bs[:, MID:MID + LS],
                                                                in1=bs[:, PMH:PMH + LS], op=ALU.add), ["bs"], ["bs"])
                        op("dve", lambda e: e.scalar_tensor_tensor(out=bs[:, THR:THR + LS], in0=bs[:, RNG:RNG + LS],
                                                                   scalar=-(2.0 ** -(KIT + 1)), in1=bs[:, MID:MID + LS],
                                                                   op0=ALU.mult, op1=ALU.add), ["bs"], ["bs"])
                    for jj in sub:
                        retention(jj, tiles[jj])
                    for jj in sub:
                        tt = tiles[jj]
                        nk = tt + 1
                        N = nk * 128
                        ls = jj % LS
                        bis = N > KTOP
                        if bis:
                            op("dve", lambda e, ls=ls, N=N: e.tensor_scalar(
                                out=msk[:, 0:N], in0=sc[:, ls, 0:N], scalar1=bs[:, THR + ls:THR + ls + 1], scalar2=None,
                                op0=ALU.is_ge), [f"sc{ls}", "bs"] + RA, ["msk"])
                            for c0 in range(0, nk, 8):
                                cn = min(8, nk - c0)
                                transposes([msk[:, (c0 + i) * 128:(c0 + i + 1) * 128] for i in range(cn)],
                                           mskT[:, c0:c0 + cn, :], ["mskT"], ["msk"] + RA)
                        for kt in range(nk):
                            if bis:
                                mk_ap, mkk = mskT[:, kt, :], "mskT"
                            elif kt == nk - 1:
                                mk_ap, mkk = LASTM[:], "LASTM"
                            else:
                                mk_ap, mkk = ONESM[:], "ONESM"
                            for hf in range(2):
                                g, gk = nextG()
                                op("pe", lambda e, g=g, kt=kt, jj=jj, hf=hf: e.matmul(
                                    g[:], lhsT=kT[:, kt * 128:(kt + 1) * 128], rhs=qT[:, jj, hf * 512:(hf + 1) * 512],
                                    start=True, stop=True), [f"kT{kt}", f"qT{jj}"], [gk])
                                ei_ = (kt * 2 + hf) % NE
                                E_, Ek = Eb[ei_], f"E{ei_}"
                                if kt >= tt - 1:
                                    kd = 0 if kt == tt else 1
                                    op("dve", lambda e, g=g, kd=kd, hf=hf: e.scalar_tensor_tensor(
                                        out=g[:], in0=g[:], scalar=0.125, in1=Bp[:, kd, hf * 512:(hf + 1) * 512],
                                        op0=ALU.mult, op1=ALU.add), [gk, f"Bp{kd}"], [gk])
                                    op("act", lambda e, g=g, E_=E_: e.activation(out=E_[:], in_=g[:], func=AF.Exp), [gk], [Ek])
                                else:
                                    op("act", lambda e, g=g, E_=E_: e.activation(out=E_[:], in_=g[:], func=AF.Exp, scale=0.125),
                                       [gk], [Ek])
                                Pm_, Pk = Pm[ei_], f"Pm{ei_}"
                                op("dve" if hf == 0 else "pool", lambda e, E_=E_, Pm_=Pm_, mk_ap=mk_ap: e.tensor_tensor(
                                    out=Pm_[:].rearrange("p (h t) -> p h t", h=4), in0=E_[:].rearrange("p (h t) -> p h t", h=4),
                                    in1=mk_ap.unsqueeze(1).broadcast_to([128, 4, 128]), op=ALU.mult), [Ek, mkk] + RA, [Pk])
                                for h4 in range(4):
                                    op("pe", lambda e, Pm_=Pm_, h4=h4, hf=hf, kt=kt, nk=nk: e.matmul(
                                        PV[hf][:, h4 * 65:(h4 + 1) * 65], lhsT=Pm_[:, h4 * 128:(h4 + 1) * 128], rhs=vA[:, kt, :],
                                        start=(kt == 0 and h4 == 0), stop=(kt == nk - 1 and h4 == 3), skip_group_check=True),
                                       [Pk, f"vA{kt}"], [f"PV{hf}"])
                        oa, oak = nextB()
                        for hf in range(2):
                            pv3 = PV[hf][:, 0:260].rearrange("p (h c) -> p h c", h=4)
                            op("dve", lambda e, pv3=pv3, hf=hf: e.reciprocal(out=st[:, 40 + hf * 4:44 + hf * 4], in_=pv3[:, :, 64]),
                               [f"PV{hf}"], [f"st4{hf}"])
                            op("dve", lambda e, pv3=pv3, hf=hf, oa=oa: e.tensor_tensor(
                                out=oa[:, hf * 256:(hf + 1) * 256].rearrange("p (h d) -> p h d", h=4), in0=pv3[:, :, 0:64],
                                in1=st[:, 40 + hf * 4:44 + hf * 4].unsqueeze(2).broadcast_to([128, 4, 64]), op=ALU.mult),
                               [f"PV{hf}", f"st4{hf}"], [oak])
                        transposes([oa[:, c * 128:(c + 1) * 128] for c in range(4)], catT[:, 0:4, :], ["catT"], [oak])
                        op("pool", lambda e, jj=jj: e.tensor_copy(out=catT[:, 4:8, :], in_=obT[:, jj, :, :]), [f"obT{jj}"], ["catT"])
                        for nbk in range(2):
                            g, gk = nextG()
                            for c in range(8):
                                op("pe", lambda e, g=g, c=c, nbk=nbk: e.matmul(
                                    g[:], lhsT=catT[:, c, :], rhs=Wout[:, c, nbk * 512:(nbk + 1) * 512], start=(c == 0), stop=(c == 7)),
                                   ["catT", "Wout"], [gk])
                            op("dve", lambda e, g=g, jj=jj, nbk=nbk: e.tensor_tensor(
                                out=X[:, jj, nbk * 512:(nbk + 1) * 512], in0=g[:], in1=X[:, jj, nbk * 512:(nbk + 1) * 512], op=ALU.add),
                               [gk, f"X{jj}"], [f"X{jj}"])
                        op("act", lambda e, jj=jj: e.activation(out=xb[:], in_=X[:, jj, :], func=AF.Square, accum_out=st[:, 3:4]),
                           [f"X{jj}"], ["xb", "st3"])
                        rstd_from_ss(3, 1.0 / D_MODEL, RMS_EPS)
                        op("act", lambda e, jj=jj: e.activation(out=xb[:], in_=X[:, jj, :], func=AF.Copy, scale=st[:, 3:4]),
                           [f"X{jj}", "st3"], ["xb"])
                        transposes([xb[:, k * 128:(k + 1) * 128] for k in range(8)], xT[:, :, jj * 128:(jj + 1) * 128],
                                   [f"xT{jj}"], ["xb"])
            op("pool", lambda e: e.memset(st[:, 62:63], 0.0), [], RA + RB)
            TW = GT * 128
            for c in range(NHC):
                s_ = c % 2
                P.dma("sp", lambda e, c=c, s_=s_: e.dma_start(out=wgs[s_], in_=wg_s[:, c]), f"wg{s_}",
                      reads=["wg_s"] + RB, writes=[f"wg{s_}"])
                P.dma("sp", lambda e, c=c, s_=s_: e.dma_start(out=wus[s_], in_=wu_s[:, c]), f"wu{s_}",
                      reads=["wu_s"] + RB, writes=[f"wu{s_}"])
                gg, ggk = nextG()
                gu, guk = nextG()
                for k in range(8):
                    op("pe", lambda e, gg=gg, k=k, s_=s_: e.matmul(gg[:, 0:TW], lhsT=wgs[s_][:, k, :], rhs=xT[:, k, :],
                                                                   start=(k == 0), stop=(k == 7)),
                       [f"wg{s_}"] + RB + [f"xT{q}" for q in range(GT)], [ggk])
                for k in range(8):
                    op("pe", lambda e, gu=gu, k=k, s_=s_: e.matmul(gu[:, 0:TW], lhsT=wus[s_][:, k, :], rhs=xT[:, k, :],
                                                                   start=(k == 0), stop=(k == 7)),
                       [f"wu{s_}"] + RB + [f"xT{q}" for q in range(GT)], [guk])
                sl, slk = nextF()
                op("act", lambda e, gg=gg, sl=sl: e.activation(out=sl[:, 0:TW], in_=gg[:, 0:TW], func=AF.Silu), [ggk], [slk])
                op("dve", lambda e, gu=gu, sl=sl, c=c: e.tensor_tensor(out=hT[:, c, :], in0=gu[:, 0:TW], in1=sl[:, 0:TW], op=ALU.mult),
                   [guk, slk] + RB, ["hT"])
            TpF = Tp[:].bitcast(F32)
            acc_banks = [(Gp[0][:], "G0"), (Gp[1][:], "G1"), (Gp[2][:], "G2"), (Gp[3][:], "G3"),
                         (PV[0][:], "PV0"), (PV[1][:], "PV1"), (Yp[:], "Yp"), (TpF, "Tp")]
            accs = {}
            for t_ in range(GT):
                for nbk in range(2):
                    accs[(t_, nbk)] = acc_banks[t_ * 2 + nbk]
            for c in range(NHC):
                s_ = c % NWD
                P.dma("sp", lambda e, c=c, s_=s_: e.dma_start(out=wds[s_], in_=wd_s[c]), f"wd{s_}",
                      reads=["wd_s"] + RB, writes=[f"wd{s_}"])
                for t_ in range(GT):
                    for nbk in range(2):
                        g, gk = accs[(t_, nbk)]
                        op("pe", lambda e, g=g, c=c, t_=t_, nbk=nbk, s_=s_: e.matmul(
                            g, lhsT=hT[:, c, t_ * 128:(t_ + 1) * 128], rhs=wds[s_][:, nbk * 512:(nbk + 1) * 512],
                            start=(c == 0), stop=(c == NHC - 1)), ["hT", f"wd{s_}"] + RB, [gk])
            for t_ in range(GT):
                for nbk in range(2):
                    g, gk = accs[(t_, nbk)]
                    op("dve", lambda e, g=g, t_=t_, nbk=nbk: e.tensor_tensor(
                        out=X[:, t_, nbk * 512:(nbk + 1) * 512], in0=g, in1=X[:, t_, nbk * 512:(nbk + 1) * 512], op=ALU.add),
                       [gk, f"X{t_}"], [f"X{t_}"])
                row0 = seq * S + tiles[t_] * 128
                P.dma("sp", lambda e, t_=t_, row0=row0: e.dma_start(out=y_d[row0:row0 + 128, :], in_=X[:, t_, :]),
                      f"y{t_}", reads=[f"X{t_}"])
    P.emit()
    return P


C_LAYOUT = [("ident", 128), ("n1w", 8), ("n2w", 8), ("kvnw", 1), ("qnw", 64), ("knw", 64),
            ("gnw", 512), ("gnb", 512), ("wuk", 64), ("wuv", 64), ("DQ", 512), ("DK", 512),
            ("DKtok", 8), ("DMASK", 128), ("GC", 4), ("BM", 512), ("LASTM", 128), ("pw", 32),
            ("negh", 8)]


def _t5_bucket_np(rel):
    import jax
    import jax.numpy as jnp
    cpu = jax.devices("cpu")[0]
    with jax.default_device(cpu):
        rel = jnp.asarray(rel, dtype=jnp.int32)
        half, max_exact, max_distance = 16, 8, 128
        ret = jnp.where(rel > 0, half, 0)
        n = jnp.abs(rel)
        nf = jnp.maximum(n, 1).astype(jnp.float32)
        large = max_exact + (jnp.log(nf / max_exact) / math.log(max_distance / max_exact)
                             * (half - max_exact)).astype(jnp.int32)
        large = jnp.minimum(large, half - 1)
        out = ret + jnp.where(n < max_exact, n, large)
        return np.asarray(out)


def _rot_tables(S):
    import jax
    import jax.numpy as jnp
    cpu = jax.devices("cpu")[0]
    with jax.default_device(cpu):
        half = 32
        inv = 10000.0 ** (-jnp.arange(half, dtype=jnp.float32) / half)
        ang = jnp.arange(S).astype(jnp.float32)[:, None] * inv[None, :]
        cos = np.asarray(jnp.cos(ang))
        sin = np.asarray(jnp.sin(ang))
    rot = np.concatenate([cos, cos, -sin, sin], axis=1).astype(np.float32)
    return np.ascontiguousarray(rot.reshape(S // 128, 128, 128))


def _host_consts(inp, S):
    f32 = np.float32
    secs = {}
    secs["ident"] = np.eye(128, dtype=f32)
    secs["n1w"] = np.ascontiguousarray(inp["norm1_w"][0].reshape(8, 128).T)
    secs["n2w"] = np.ascontiguousarray(inp["norm2_w"][0].reshape(8, 128).T)
    secs["kvnw"] = inp["kv_norm_w"][0].reshape(128, 1)
    secs["qnw"] = np.broadcast_to(inp["q_norm_w"][0][None, :], (128, 64))
    secs["knw"] = np.broadcast_to(inp["k_norm_w"][0][None, :], (128, 64))
    secs["gnw"] = np.broadcast_to(inp["ret_gn_w"][0][None, :], (128, 512))
    secs["gnb"] = np.broadcast_to(inp["ret_gn_b"][0][None, :], (128, 512))
    secs["wuk"] = inp["w_uk"][0]
    secs["wuv"] = inp["w_uv"][0]
    hh = np.arange(8, dtype=np.float64)
    log_g = np.log1p(-(2.0 ** (-5.0 - hh)))
    t = np.arange(128, dtype=np.float64)
    row_h = np.arange(128) // 64
    DQ = np.zeros((128, 4, 128)); DK = np.zeros((128, 4, 128)); GC = np.zeros((128, 4))
    for pq in range(4):
        lg = log_g[2 * pq + row_h]
        DQ[:, pq, :] = np.exp((t[None, :] + 1.0) * lg[:, None])
        DK[:, pq, :] = np.exp(-(t[None, :] + 1.0) * lg[:, None]) * 0.125
        GC[:, pq] = np.exp(128.0 * lg)
    secs["DQ"] = DQ.reshape(128, 512)
    secs["DK"] = DK.reshape(128, 512)
    secs["DKtok"] = np.exp(-(t[:, None] + 1.0) * log_g[None, :]) * 0.125
    jj = np.arange(128)
    secs["DMASK"] = (jj[None, :] >= jj[:, None]).astype(f32)
    secs["GC"] = GC
    BM = np.zeros((128, 4, 128), f32)
    for h in range(4):
        BM[h * 32:(h + 1) * 32, h, :] = 1.0
    secs["BM"] = BM.reshape(128, 512)
    LM = np.ones((128, 128), f32)
    LM[64:, :64] = 0.0
    secs["LASTM"] = LM
    secs["pw"] = np.broadcast_to((2.0 ** -(np.arange(32, dtype=np.float64) + 1.0))[None, :], (128, 32))
    secs["negh"] = np.full((128, 8), -0.5)
    cst = np.zeros((128, CW), f32)
    o = 0
    for nm, w in C_LAYOUT:
        cst[:, o:o + w] = np.asarray(secs[nm], dtype=f32)
        o += w
    s_ = np.arange(128)[:, None]
    t_ = np.arange(128)[None, :]
    rb = inp["rel_bias"]
    b0 = rb[_t5_bucket_np(s_ - t_)]
    b1 = rb[_t5_bucket_np(s_ - 128 - t_)]
    cb = rb[_t5_bucket_np(np.full((128, 128), -1000))]
    bias_t = np.stack([np.transpose(b, (0, 2, 1)).reshape(128, 1024) for b in (b0, b1, cb)]).astype(f32)
    tb = np.broadcast_to((-1e-30 * np.arange(512, dtype=np.float64)).astype(f32)[None, :], (128, 512))
    return cst, np.ascontiguousarray(bias_t), np.ascontiguousarray(tb), _rot_tables(S)


_NC_CACHE = {}


def run_cores(inp, xs, S, NSEQ, KTOP):
    key = (S, NSEQ, KTOP)
    nc = bass.Bass("TRN2", target_bir_lowering=False)
    build(nc, S, NSEQ, KTOP)
    cst, bias_t, tb, rot = _host_consts(inp, S)
    base = {
        "w_in": np.ascontiguousarray(inp["w_in"][0]), "w_out": np.ascontiguousarray(inp["w_out"][0]),
        "w_gate": np.ascontiguousarray(inp["w_gate"][0]), "w_up": np.ascontiguousarray(inp["w_up"][0]),
        "w_down": np.ascontiguousarray(inp["w_down"][0]), "cst": cst, "bias_t": bias_t, "tb": tb, "rot": rot,
    }
    in_maps = [dict(base, x=np.ascontiguousarray(x)) for x in xs]
    res = run_bass_kernel_spmd(nc, in_maps, core_ids=list(range(len(xs))))
    return [r["y"] for r in res.results]


def kernel(**inputs):
    inp = {k: np.asarray(v) for k, v in inputs.items()}
    x = inp["x"]
    B, S, D = x.shape
    ncores = 8
    nseq = B // ncores
    xs = [x[c * nseq:(c + 1) * nseq].reshape(nseq * S, D) for c in range(ncores)]
    ys = run_cores(inp, xs, S, nseq, min(256, S // 4))
    return np.concatenate([y.reshape(nseq, S, D) for y in ys], axis=0).astype(np.float32)
```

```python
import contextlib
import math
import numpy as np
import concourse.bass as bass
import concourse.mybir as mybir
from concourse.bass_utils import run_bass_kernel_spmd

F32 = mybir.dt.float32
BF16 = mybir.dt.bfloat16
AF = mybir.ActivationFunctionType
ALU = mybir.AluOpType
AX = mybir.AxisListType

D_MODEL = 1024
HID = 2816
NHC = HID // 128
IN_W = 2852
RMS_EPS = 1e-6
GN_EPS = 1e-5
KIT = 20
CW = 3272
NEG = -3.0e38

ENGS = ("pe", "act", "dve", "pool", "sp")
CH = 20000
DCH = 1500


class Prog:
    def __init__(self, nc):
        self.nc = nc
        self.q = {e: [] for e in ENGS}
        self.n = {e: 0 for e in ENGS}
        self.seen = {e: {} for e in ENGS}
        self.lastw = {}
        self.readers = {}
        self.dcount = {}
        self.dbatch = {}
        self.dopen = {}

    def _deps(self, eng, reads, writes):
        deps = {}

        def add(src, idx, raw):
            if src == eng and eng in ("pe", "sp"):
                return
            if deps.get(src, -1) < idx:
                deps[src] = idx

        for r in reads:
            if r in self.lastw:
                add(*self.lastw[r], True)
        for w in writes:
            if w in self.lastw:
                add(*self.lastw[w], False)
            for (s, i) in self.readers.get(w, ()):
                add(s, i, False)
        out = []
        for src, idx in deps.items():
            if self.seen[eng].get(src, -1) >= idx:
                continue
            self.seen[eng][src] = idx
            out.append((src, idx))
        return out

    def _commit(self, sig, reads, writes):
        for r in reads:
            self.readers.setdefault(r, []).append(sig)
        for w in writes:
            self.lastw[w] = sig
            self.readers[w] = []

    def op(self, eng, fn, reads=(), writes=()):
        waits = self._deps(eng, reads, writes)
        idx = self.n[eng]
        self.n[eng] += 1
        self.q[eng].append(("op", fn, waits, idx))
        self._commit((eng, idx), reads, writes)

    def dma(self, queue, fn, stream, reads=(), writes=(), batch=False):
        waits = self._deps(queue, reads, writes)
        idx = self.dcount.get(stream, 0)
        self.dcount[stream] = idx + 1
        self.dbatch.setdefault(stream, []).append(None)
        self.dopen.setdefault(stream, []).append(idx)
        if not batch:
            self.close(stream)
        self.q[queue].append(("dma", fn, waits, (stream, idx)))
        self._commit(("dma:" + stream, idx), reads, writes)

    def close(self, stream):
        ids = self.dopen.get(stream, [])
        if ids:
            for i in ids:
                self.dbatch[stream][i] = ids[-1]
            self.dopen[stream] = []

    def emit(self):
        nc = self.nc
        for s in list(self.dopen):
            self.close(s)
        with contextlib.ExitStack() as st:
            sems = {}
            for e in ENGS:
                if e == "sp":
                    continue
                ne = max(1, (self.n[e] + CH - 1) // CH)
                sems[e] = [st.enter_context(nc.semaphore(f"s_{e}_{k}")) for k in range(ne)]
            for s, c in self.dcount.items():
                ne = max(1, (c + DCH - 1) // DCH)
                sems["dma:" + s] = [st.enter_context(nc.semaphore(f"d_{s}_{k}")) for k in range(ne)]

            def wait_args(src, idx):
                if src.startswith("dma:"):
                    end = self.dbatch[src[4:]][idx]
                    return sems[src][end // DCH], 16 * (end % DCH + 1)
                return sems[src][idx // CH], idx % CH + 1

            def run(name, eng):
                for kind, fn, waits, info in self.q[name]:
                    for (src, idx) in waits:
                        sem, val = wait_args(src, idx)
                        eng.wait_ge(sem, val)
                    ins = fn(eng)
                    if kind == "op":
                        ins.then_inc(sems[name][info // CH], 1)
                    else:
                        stream, idx = info
                        ins.then_inc(sems["dma:" + stream][idx // DCH], 16)
                if name == "sp":
                    for s, c in self.dcount.items():
                        sem, val = wait_args("dma:" + s, c - 1)
                        eng.wait_ge(sem, val)

            with nc.Block() as block:
                @block.tensor
                def _(eng):
                    run("pe", eng)

                @block.scalar
                def _(eng):
                    run("act", eng)

                @block.vector
                def _(eng):
                    run("dve", eng)

                @block.gpsimd
                def _(eng):
                    run("pool", eng)

                @block.sync
                def _(eng):
                    run("sp", eng)


def build(nc, S, NSEQ, KTOP):
    NT = S // 128
    GT = min(4, NT)
    NG = NT // GT
    LS = 2 if GT >= 2 else 1
    P = Prog(nc)

    def din(name, shape, dt=F32):
        return nc.dram_tensor(name, list(shape), dt, kind="ExternalInput").ap()

    x_d = din("x", [NSEQ * S, D_MODEL])
    w_in_d = din("w_in", [D_MODEL, IN_W])
    w_out_d = din("w_out", [D_MODEL, D_MODEL])
    w_gate_d = din("w_gate", [D_MODEL, HID])
    w_up_d = din("w_up", [D_MODEL, HID])
    w_down_d = din("w_down", [HID, D_MODEL])
    cst_d = din("cst", [128, CW])
    bias_d = din("bias_t", [3, 128, 1024])
    tb_d = din("tb", [128, 512])
    rot_d = din("rot", [NT, 128, 128])
    y_d = nc.dram_tensor("y", [NSEQ * S, D_MODEL], F32, kind="ExternalOutput").ap()
    wg_s = nc.dram_tensor("wg_s", [128, NHC, 8, 128], BF16, kind="Internal").ap()
    wu_s = nc.dram_tensor("wu_s", [128, NHC, 8, 128], BF16, kind="Internal").ap()
    wd_s = nc.dram_tensor("wd_s", [NHC, 128, D_MODEL], BF16, kind="Internal").ap()

    def sb(name, shape, dt):
        return nc.alloc_sbuf_tensor(name, list(shape), dt)

    Win = sb("Win", [128, 8, IN_W], BF16)
    Wout = sb("Wout", [128, 8, D_MODEL], BF16)
    Wukv = sb("Wukv", [128, 128], BF16)
    CST = sb("CST", [128, CW], F32)
    c_off = {}
    o = 0
    for nm, w in [("ident", 128), ("n1w", 8), ("n2w", 8), ("kvnw", 1), ("qnw", 64), ("knw", 64),
                  ("gnw", 512), ("gnb", 512), ("wuk", 64), ("wuv", 64), ("DQ", 512), ("DK", 512),
                  ("DKtok", 8), ("DMASK", 128), ("GC", 4), ("BM", 512), ("LASTM", 128), ("pw", 32),
                  ("negh", 8)]:
        c_off[nm] = (o, w)
        o += w
    assert o <= CW

    def C(nm, a=0, b=None):
        o0, w = c_off[nm]
        return CST[:, o0 + a: o0 + (w if b is None else b)]

    identb = sb("identb", [128, 128], BF16)
    Bp = sb("Bp", [128, 2, 1024], F32)
    TB = sb("TB", [128, 512], F32)
    ONESM = sb("ONESM", [128, 128], BF16)
    LASTM = sb("LASTMb", [128, 128], BF16)
    kT = sb("kT", [64, S], BF16)
    vA = sb("vA", [128, NT, 65], BF16)
    ikT4 = sb("ikT4", [128, S], BF16)
    stf = sb("stf", [128, 4, 64], F32)
    stb = sb("stb", [128, 4, 64], BF16)
    X = sb("X", [128, GT, D_MODEL], F32)
    xT = sb("xT", [128, 8, GT * 128], BF16)
    qT = sb("qT", [64, GT, 1024], BF16)
    iqTb = sb("iqTb", [128, GT, 4, 128], BF16)
    obT = sb("obT", [128, GT, 4, 128], BF16)
    aw = sb("aw", [128, GT, 4], F32)
    sgn = sb("sgn", [128, GT, 4], F32)
    ROT = sb("ROT", [128, 128], F32)
    xb = sb("xb", [128, D_MODEL], BF16)
    NF = 4
    ftmp = [sb(f"ft{i}", [128, 512], F32) for i in range(NF)]
    NB = 4
    btmp = [sb(f"bt{i}", [128, 512], BF16) for i in range(NB)]
    qbTL = [sb(f"qbT{i}", [128, 4, 128], BF16) for i in range(2)]
    kbTL = [sb(f"kbT{i}", [128, 4, 128], BF16) for i in range(2)]
    vtokL = [sb(f"vtok{i}", [128, 512], BF16) for i in range(2)]
    ktokL = [sb(f"ktok{i}", [128, 512], BF16) for i in range(2)]
    sgtL = [sb(f"sgt{i}", [128, 512], BF16) for i in range(2)]
    scb = [sb("scb0", [128, 256], BF16)]
    xb2 = sb("xb2", [128, 128], BF16)
    DMASKb = sb("DMASKb", [128, 128], BF16)
    cT = sb("cT", [128, 128], BF16)
    scm = [sb(f"scm{i}", [128, 256], BF16) for i in range(2)]
    dS = sb("dS", [128, 64], F32)
    catT = sb("catT", [128, 8, 128], BF16)
    NE = 3
    Eb = [sb(f"E{i}", [128, 512], BF16) for i in range(NE)]
    Pm = [sb(f"Pm{i}", [128, 512], BF16) for i in range(NE)]
    st = sb("st", [128, 64], F32)
    bs = sb("bs", [128, 8 * LS + LS * KIT + 16], F32)
    RW = 9216
    R = sb("R", [128, RW], F32)
    Rb = R[:].bitcast(BF16)
    sc = R[:, 0:LS * S].rearrange("p (a s) -> p a s", a=LS)
    msk = Rb[:, 2 * LS * S: 2 * LS * S + S]
    mskT = Rb[:, 2 * LS * S + S: 2 * LS * S + 2 * S].rearrange("p (a b) -> p a b", b=128)
    assert 2 * LS * S + 2 * S <= 2 * RW
    hT = Rb[:, 0:NHC * GT * 128].rearrange("p (c t) -> p c t", c=NHC)
    o_w = NHC * 512
    wgs = [Rb[:, o_w + s * 2048: o_w + s * 2048 + 1024].rearrange("p (k j) -> p k j", k=8) for s in range(2)]
    wus = [Rb[:, o_w + s * 2048 + 1024: o_w + s * 2048 + 2048].rearrange("p (k j) -> p k j", k=8) for s in range(2)]
    NWD = 3
    wds = [Rb[:, o_w + 4096 + s * 1024: o_w + 4096 + (s + 1) * 1024] for s in range(NWD)]
    assert o_w + 4096 + NWD * 1024 <= 2 * RW
    stg = [R[:, s * IN_W:(s + 1) * IN_W] for s in range(2)]
    stgb = [Rb[:, 2 * 2 * IN_W + s * HID: 2 * 2 * IN_W + (s + 1) * HID] for s in range(2)]
    assert 4 * IN_W + 2 * HID <= 2 * RW
    Gp = [nc.alloc_psum_tensor(f"G{i}", [128, 512], F32) for i in range(4)]
    PV = [nc.alloc_psum_tensor(f"PV{i}", [128, 512], F32) for i in range(2)]
    Tp = nc.alloc_psum_tensor("Tp", [128, 1024], BF16)
    Yp = nc.alloc_psum_tensor("Yp", [128, 512], F32)
    gctr = [0]

    def nextG():
        i = gctr[0] % 4
        gctr[0] += 1
        return Gp[i], f"G{i}"

    fctr = [0]

    def nextF():
        i = fctr[0] % NF
        fctr[0] += 1
        return ftmp[i], f"ft{i}"

    bctr = [0]

    def nextB():
        i = bctr[0] % NB
        bctr[0] += 1
        return btmp[i], f"bt{i}"

    op = P.op
    RA = ["RA"]
    RB = ["RB"]

    P.dma("sp", lambda e: e.dma_start(out=CST[:], in_=cst_d), "init", writes=["CST"], batch=True)
    P.dma("sp", lambda e: e.dma_start(out=TB[:], in_=tb_d), "init", writes=["TB"], batch=True)
    P.dma("sp", lambda e: e.dma_start(out=Bp[:, 0, :], in_=bias_d[0]), "init", writes=["Bp0"], batch=True)
    P.dma("sp", lambda e: e.dma_start(out=Bp[:, 1, :], in_=bias_d[1]), "init", writes=["Bp1"], batch=True)
    P.dma("sp", lambda e: e.dma_start(out=ftmp[0][:], in_=bias_d[2][:, 0:512]), "init", writes=["ft0"], batch=True)
    P.dma("sp", lambda e: e.dma_start(out=ftmp[1][:], in_=bias_d[2][:, 512:1024]), "init", writes=["ft1"], batch=True)
    P.close("init")
    op("dve", lambda e: e.tensor_copy(out=identb[:], in_=C("ident")), ["CST"], ["identb"])
    op("dve", lambda e: e.tensor_copy(out=LASTM[:], in_=C("LASTM")), ["CST"], ["LASTM"])
    op("dve", lambda e: e.memset(ONESM[:], 1.0), [], ["ONESM"])
    op("dve", lambda e: e.tensor_copy(out=DMASKb[:], in_=C("DMASK")), ["CST"], ["DMASKb"])
    op("dve", lambda e: e.tensor_scalar(out=C("gnw"), in0=C("gnw"), scalar1=0.5, scalar2=None, op0=ALU.mult), ["CST"], ["CST"])
    op("dve", lambda e: e.tensor_scalar(out=C("gnb"), in0=C("gnb"), scalar1=0.5, scalar2=None, op0=ALU.mult), ["CST"], ["CST"])
    op("dve", lambda e: e.memset(bs[:], 0.0), [], ["bs"])
    op("dve", lambda e: e.memset(st[:], 0.0), [], ["stall"])
    for kd in range(2):
        for hf in range(2):
            op("dve", lambda e, kd=kd, hf=hf: e.tensor_tensor(
                out=Bp[:, kd, hf * 512:(hf + 1) * 512], in0=Bp[:, kd, hf * 512:(hf + 1) * 512],
                in1=ftmp[hf][:], op=ALU.subtract), [f"Bp{kd}", f"ft{hf}"], [f"Bp{kd}"])
    op("dve", lambda e: e.tensor_scalar(out=Wukv[:], in0=CST[:, c_off["wuk"][0]: c_off["wuk"][0] + 128],
                                        scalar1=C("kvnw"), scalar2=None, op0=ALU.mult), ["CST"], ["Wukv"])
    op("pool", lambda e: e.memset(vA[:, :, 64:65], 1.0), [], [f"vA{t}" for t in range(NT)])
    ecyc = ["dve", "pool"]
    ei = [0]

    def ceng():
        ei[0] += 1
        return ecyc[ei[0] % 2]

    for k in range(8):
        s_ = k % 2
        P.dma("sp", lambda e, k=k, s_=s_: e.dma_start(out=stg[s_], in_=w_in_d[k * 128:(k + 1) * 128, :]),
              f"stg{s_}", reads=[], writes=[f"stg{s_}"])
        for (a, b, dst) in [(0, 512, 0), (512, 804, 2560), (804, 2852, 512)]:
            en = ceng()
            op(en, lambda e, k=k, s_=s_, a=a, b=b, dst=dst: e.tensor_scalar(
                out=Win[:, k, dst:dst + (b - a)], in0=stg[s_][:, a:b], scalar1=C("n1w", k, k + 1), scalar2=None,
                op0=ALU.mult), [f"stg{s_}", "CST"], ["Win"])
    for k in range(8):
        s_ = k % 2
        P.dma("sp", lambda e, k=k, s_=s_: e.dma_start(out=stg[s_][:, 0:1024], in_=w_out_d[k * 128:(k + 1) * 128, :]),
              f"stg{s_}", writes=[f"stg{s_}"])
        op(ceng(), lambda e, k=k, s_=s_: e.tensor_copy(out=Wout[:, k, :], in_=stg[s_][:, 0:1024]),
           [f"stg{s_}"], ["Wout"])
    wst_keys = []
    for (src_d, dst_d, nm) in [(w_gate_d, wg_s, "wg_s"), (w_up_d, wu_s, "wu_s")]:
        for k in range(8):
            s_ = k % 2
            P.dma("sp", lambda e, k=k, s_=s_, src_d=src_d: e.dma_start(out=stg[s_][:, 0:HID], in_=src_d[k * 128:(k + 1) * 128, :]),
                  f"stg{s_}", writes=[f"stg{s_}"])
            op(ceng(), lambda e, k=k, s_=s_: e.tensor_scalar(
                out=stgb[s_], in0=stg[s_][:, 0:HID], scalar1=C("n2w", k, k + 1), scalar2=None, op0=ALU.mult),
               [f"stg{s_}", "CST"], [f"stgb{s_}"])
            P.dma("sp", lambda e, k=k, s_=s_, dst_d=dst_d: e.dma_start(
                out=dst_d[:, :, k, :], in_=stgb[s_].rearrange("p (c j) -> p c j", j=128)),
                f"stgb{s_}", reads=[f"stgb{s_}"], writes=[f"{nm}{k}"])
            wst_keys.append(f"{nm}{k}")
    for c2 in range(NHC // 2):
        s_ = c2 % 2
        P.dma("sp", lambda e, c2=c2, s_=s_: e.dma_start(
            out=stg[s_][:, 0:2048].rearrange("p (c n) -> p c n", c=2),
            in_=w_down_d[c2 * 256:(c2 + 1) * 256, :].rearrange("(c p) n -> p c n", p=128)),
            f"stg{s_}", writes=[f"stg{s_}"])
        op(ceng(), lambda e, s_=s_: e.tensor_copy(out=stgb[s_][:, 0:2048], in_=stg[s_][:, 0:2048]),
           [f"stg{s_}"], [f"stgb{s_}"])
        P.dma("sp", lambda e, c2=c2, s_=s_: e.dma_start(
            out=wd_s[2 * c2:2 * c2 + 2].rearrange("c p n -> p c n"),
            in_=stgb[s_][:, 0:2048].rearrange("p (c n) -> p c n", c=2)),
            f"stgb{s_}", reads=[f"stgb{s_}"], writes=[f"wd_s{c2}"])
        wst_keys.append(f"wd_s{c2}")
    op("pool", lambda e: e.memset(st[:, 60:61], 0.0), wst_keys, ["stg0", "stg1", "stgb0", "stgb1", "RA", "RB", "wg_s", "wu_s", "wd_s"])

    def rstd_from_ss(col, inv_n, eps):
        op("dve", lambda e: e.tensor_scalar(out=st[:, col:col + 1], in0=st[:, col:col + 1], scalar1=inv_n,
                                            scalar2=eps, op0=ALU.mult, op1=ALU.add), [f"st{col}"], [f"st{col}"])
        op("pool", lambda e: e.tensor_tensor(out=st[:, col:col + 1], in0=st[:, col:col + 1], in1=C("negh", 0, 1),
                                             op=ALU.pow), [f"st{col}", "CST"], [f"st{col}"])

    def transposes(src_aps, dst_ap, dst_keys, src_keys, rows=128, cols=128, evac="act"):
        n = len(src_aps)
        for i, a in enumerate(src_aps):
            op("pe", lambda e, i=i, a=a: e.transpose(out=Tp[0:cols, i * 128:(i + 1) * 128], in_=a, identity=identb[:]),
               src_keys + ["identb"], ["Tp"])
        srcv = Tp[0:cols, 0:n * 128].rearrange("p (a b) -> p a b", b=128)
        if evac == "act":
            op("act", lambda e: e.copy(out=dst_ap, in_=srcv), ["Tp"], dst_keys)
        else:
            op("dve", lambda e: e.tensor_copy(out=dst_ap, in_=srcv), ["Tp"], dst_keys)


    def retention(j, ti):
        l = j % 2
        qb_, kb_, vt_, kt_, sg_ = qbTL[l], kbTL[l], vtokL[l], ktokL[l], sgtL[l]
        for pq in range(4):
            sm = scm[pq % 2]
            smk = f"scm{pq % 2}"
            sb_, sbk = scb[0], "scb0"
            for hh in range(2):
                r0, r1 = hh * 64, hh * 64 + 64
                g, gk = nextG()
                op("pe", lambda e, g=g, pq=pq, r0=r0, r1=r1: e.matmul(
                    g[:, 0:128], lhsT=kb_[r0:r1, pq, :], rhs=qb_[r0:r1, pq, :], start=True, stop=True),
                   [f"kbT{l}", f"qbT{l}"], [gk])
                op("act", lambda e, g=g, sb_=sb_, hh=hh: e.copy(out=sb_[:, hh * 128:(hh + 1) * 128], in_=g[:, 0:128]), [gk], [sbk])
            op("pool", lambda e, sb_=sb_, sm=sm: e.tensor_tensor(
                out=sm[:].rearrange("p (a b) -> p a b", a=2), in0=sb_[:].rearrange("p (a b) -> p a b", a=2),
                in1=DMASKb[:].unsqueeze(1).broadcast_to([128, 2, 128]), op=ALU.mult), [sbk, "DMASKb"], [smk])
            for hh in range(2):
                h = 2 * pq + hh
                r0, r1 = hh * 64, hh * 64 + 64
                op("pe", lambda e, sm=sm, h=h, hh=hh: e.matmul(
                    Yp[:, h * 64:(h + 1) * 64], lhsT=sm[:, hh * 128:(hh + 1) * 128], rhs=vt_[:, h * 64:(h + 1) * 64],
                    start=True, stop=False), [smk, f"vtok{l}"], ["Yp"])
                op("pe", lambda e, pq=pq, r0=r0, r1=r1, h=h: e.matmul(
                    Yp[:, h * 64:(h + 1) * 64], lhsT=qb_[r0:r1, pq, :], rhs=stb[r0:r1, pq, :], start=False, stop=True),
                   [f"qbT{l}", "stb"], ["Yp"])
            g, gk = nextG()
            op("pe", lambda e, g=g, pq=pq: e.matmul(
                g[:, 0:128], lhsT=kt_[:, pq * 128:(pq + 1) * 128], rhs=vt_[:, pq * 128:(pq + 1) * 128],
                start=True, stop=True), [f"ktok{l}", f"vtok{l}"], [gk])
            for hh in range(2):
                r0, r1 = hh * 64, hh * 64 + 64
                op("act", lambda e, g=g, r0=r0, r1=r1, pq=pq: e.activation(
                    out=dS[r0:r1, :], in_=g[r0:r1, r0:r1], func=AF.Copy, scale=C("GC", pq, pq + 1)[r0:r1, :]),
                   [gk, "CST"], ["dS"])
            op("pool", lambda e, pq=pq: e.tensor_scalar(out=stf[:, pq, :], in0=stf[:, pq, :], scalar1=C("GC", pq, pq + 1),
                                                        scalar2=None, op0=ALU.mult), ["stf", "CST"], ["stf"])
            op("pool", lambda e, pq=pq: e.tensor_tensor(out=stf[:, pq, :], in0=stf[:, pq, :], in1=dS[:], op=ALU.add),
               ["stf", "dS"], ["stf"])
            op("pool", lambda e, pq=pq: e.tensor_copy(out=stb[:, pq, :], in_=stf[:, pq, :]), ["stf"], ["stb"])
        for h in range(8):
            op("act", lambda e, h=h: e.activation(out=xb2[:, 0:64], in_=Yp[:, h * 64:(h + 1) * 64], func=AF.Identity,
                                                  accum_out=st[:, 16 + h:17 + h]), ["Yp"], ["xb2", "st16"])
            op("act", lambda e, h=h: e.activation(out=xb2[:, 64:128], in_=Yp[:, h * 64:(h + 1) * 64], func=AF.Square,
                                                  accum_out=st[:, 24 + h:25 + h]), ["Yp"], ["xb2", "st24"])
        op("pool", lambda e: e.tensor_scalar(out=st[:, 16:24], in0=st[:, 16:24], scalar1=1.0 / 64, scalar2=None,
                                             op0=ALU.mult), ["st16"], ["st16"])
        op("pool", lambda e: e.tensor_tensor(out=st[:, 32:40], in0=st[:, 16:24], in1=st[:, 16:24], op=ALU.mult),
           ["st16"], ["st32"])
        op("pool", lambda e: e.tensor_scalar(out=st[:, 24:32], in0=st[:, 24:32], scalar1=1.0 / 64, scalar2=GN_EPS,
                                             op0=ALU.mult, op1=ALU.add), ["st24"], ["st24"])
        op("pool", lambda e: e.tensor_tensor(out=st[:, 24:32], in0=st[:, 24:32], in1=st[:, 32:40], op=ALU.subtract),
           ["st24", "st32"], ["st24"])
        op("pool", lambda e: e.tensor_tensor(out=st[:, 24:32], in0=st[:, 24:32], in1=C("negh"), op=ALU.pow),
           ["st24", "CST"], ["st24"])
        op("pool", lambda e: e.tensor_tensor(out=st[:, 32:40], in0=st[:, 16:24], in1=st[:, 24:32], op=ALU.mult),
           ["st16", "st24"], ["st32"])
        op("pool", lambda e: e.tensor_scalar(out=st[:, 32:40], in0=st[:, 32:40], scalar1=-1.0, scalar2=None, op0=ALU.mult),
           ["st32"], ["st32"])
        yn, ynk = nextF()
        for h in range(8):
            op("act", lambda e, h=h, yn=yn: e.activation(out=yn[:, h * 64:(h + 1) * 64], in_=Yp[:, h * 64:(h + 1) * 64],
                                                         func=AF.Identity, scale=st[:, 24 + h:25 + h], bias=st[:, 32 + h:33 + h]),
               ["Yp", "st24", "st32"], [ynk])
        op("pool", lambda e, yn=yn: e.tensor_tensor(out=yn[:], in0=yn[:], in1=C("gnw"), op=ALU.mult), [ynk, "CST"], [ynk])
        op("pool", lambda e, yn=yn: e.tensor_tensor(out=yn[:], in0=yn[:], in1=C("gnb"), op=ALU.add), [ynk, "CST"], [ynk])
        ob, obk = nextB()
        op("pool", lambda e, yn=yn, ob=ob: e.tensor_tensor(out=ob[:], in0=yn[:], in1=sg_[:], op=ALU.mult), [ynk, f"sgt{l}"], [obk])
        transposes([ob[:, c * 128:(c + 1) * 128] for c in range(4)], obT[:, j, :, :], [f"obT{j}"], [obk])

    for seq in range(NSEQ):
        op("pool", lambda e: e.memset(stf[:], 0.0), [], ["stf"])
        op("pool", lambda e: e.memset(stb[:], 0.0), [], ["stb"])
        for gi in range(NG):
            tiles = [gi * GT + j for j in range(GT)]
            op("pool", lambda e: e.memset(st[:, 61:62], 0.0), [], RA + RB)
            for j, ti in enumerate(tiles):
                row0 = seq * S + ti * 128
                pos = ti * 128
                Xj = f"X{j}"
                P.dma("sp", lambda e, j=j, row0=row0: e.dma_start(out=X[:, j, :], in_=x_d[row0:row0 + 128, :]),
                      f"x{j}", writes=[Xj])
                P.dma("sp", lambda e, ti=ti: e.dma_start(out=ROT[:], in_=rot_d[ti]), "rot", writes=["ROT"])
                op("act", lambda e, j=j: e.activation(out=xb[:], in_=X[:, j, :], func=AF.Square, accum_out=st[:, 0:1]),
                   [Xj], ["xb", "st0"])
                rstd_from_ss(0, 1.0 / D_MODEL, RMS_EPS)
                op("act", lambda e, j=j: e.activation(out=xb[:], in_=X[:, j, :], func=AF.Copy, scale=st[:, 0:1]),
                   [Xj, "st0"], ["xb"])
                transposes([xb[:, k * 128:(k + 1) * 128] for k in range(8)], xT[:, :, j * 128:(j + 1) * 128],
                           [f"xT{j}"], ["xb"])
                zbanks = [(Gp[0], "G0"), (Gp[1], "G1"), (Gp[2], "G2"), (Gp[3], "G3"), (PV[0], "PV0"), (PV[1], "PV1")]
                for b in range(6):
                    w_ = 512 if b < 5 else IN_W - 2560
                    g, gk = zbanks[b]
                    for k in range(8):
                        op("pe", lambda e, g=g, k=k, j=j, b=b, w_=w_: e.matmul(
                            g[:, 0:w_], lhsT=xT[:, k, j * 128:(j + 1) * 128], rhs=Win[:, k, b * 512:b * 512 + w_],
                            start=(k == 0), stop=(k == 7)), [f"xT{j}", "Win"], [gk])
                for b in range(6):
                    g, gk = zbanks[b]
                    if b == 0:
                        f1, f1k = nextF()
                        op("act", lambda e, g=g, f1=f1: e.activation(out=f1[:], in_=g[:], func=AF.Square), [gk], [f1k])
                        op("dve", lambda e, f1=f1: e.tensor_reduce(out=st[:, 8:16], in_=f1[:].rearrange("p (h d) -> p h d", h=8),
                                                                   axis=AX.X, op=ALU.add), [f1k], ["st8"])
                        op("dve", lambda e: e.tensor_scalar(out=st[:, 8:16], in0=st[:, 8:16], scalar1=1.0 / 64, scalar2=RMS_EPS,
                                                            op0=ALU.mult, op1=ALU.add), ["st8"], ["st8"])
                        op("pool", lambda e: e.tensor_tensor(out=st[:, 8:16], in0=st[:, 8:16], in1=C("negh"), op=ALU.pow),
                           ["st8", "CST"], ["st8"])
                        f2, f2k = nextF()
                        op("dve", lambda e, g=g, f2=f2: e.tensor_tensor(
                            out=f2[:].rearrange("p (h d) -> p h d", h=8), in0=g[:].rearrange("p (h d) -> p h d", h=8),
                            in1=st[:, 8:16].unsqueeze(2).broadcast_to([128, 8, 64]), op=ALU.mult), [gk, "st8"], [f2k])
                        b1, b1k = nextB()
                        op("pool", lambda e, f2=f2, b1=b1: e.tensor_tensor(
                            out=b1[:].rearrange("p (h d) -> p h d", h=8), in0=f2[:].rearrange("p (h d) -> p h d", h=8),
                            in1=C("qnw").unsqueeze(1).broadcast_to([128, 8, 64]), op=ALU.mult), [f2k, "CST"], [b1k])
                        transposes([b1[:, h * 64:(h + 1) * 64] for h in range(8)],
                                   qT[:, j, :].rearrange("p (a b) -> p a b", b=128), [f"qT{j}"], [b1k], cols=64)
                    elif b in (1, 2):
                        fa, fak = nextF()
                        fb, fbk = nextF()
                        g3 = g[:].rearrange("p (h d) -> p h d", h=8)
                        op("dve", lambda e, fa=fa, g3=g3: e.tensor_tensor(
                            out=fa[:].rearrange("p (h d) -> p h d", h=8), in0=g3,
                            in1=ROT[:, 0:64].unsqueeze(1).broadcast_to([128, 8, 64]), op=ALU.mult), [gk, "ROT"], [fak])
                        op("dve", lambda e, fb=fb, g3=g3: e.tensor_tensor(
                            out=fb[:].rearrange("p (h d) -> p h d", h=8)[:, :, 0:32], in0=g3[:, :, 32:64],
                            in1=ROT[:, 64:96].unsqueeze(1).broadcast_to([128, 8, 32]), op=ALU.mult), [gk, "ROT"], [fbk])
                        op("dve", lambda e, fb=fb, g3=g3: e.tensor_tensor(
                            out=fb[:].rearrange("p (h d) -> p h d", h=8)[:, :, 32:64], in0=g3[:, :, 0:32],
                            in1=ROT[:, 96:128].unsqueeze(1).broadcast_to([128, 8, 32]), op=ALU.mult), [gk, "ROT", fbk], [fbk])
                        rb_, rbk = nextB()
                        op("pool", lambda e, fa=fa, fb=fb, rb_=rb_: e.tensor_tensor(out=rb_[:], in0=fa[:], in1=fb[:], op=ALU.add),
                           [fak, fbk], [rbk])
                        dstT, dk_, dcn = (qbTL[j % 2], f"qbT{j % 2}", "DQ") if b == 1 else (kbTL[j % 2], f"kbT{j % 2}", "DK")
                        for pq in range(4):
                            op("pe", lambda e, pq=pq, rb_=rb_: e.transpose(out=Tp[:, pq * 128:(pq + 1) * 128],
                                                                          in_=rb_[:, pq * 128:(pq + 1) * 128], identity=identb[:]),
                               [rbk, "identb"], ["Tp"])
                        op("dve", lambda e, dstT=dstT, dcn=dcn: e.tensor_tensor(
                            out=dstT[:].rearrange("p a b -> p (a b)"), in0=Tp[:, 0:512], in1=C(dcn), op=ALU.mult),
                           ["Tp", "CST"], [dk_])
                        if b == 2:
                            kt_, ktk = ktokL[j % 2], f"ktok{j % 2}"
                            op("pool", lambda e, rb_=rb_, kt_=kt_: e.tensor_tensor(
                                out=kt_[:].rearrange("p (h d) -> p h d", h=8), in0=rb_[:].rearrange("p (h d) -> p h d", h=8),
                                in1=C("DKtok").unsqueeze(2).broadcast_to([128, 8, 64]), op=ALU.mult), [rbk, "CST"], [ktk])
                    elif b == 3:
                        vt_, vtk = vtokL[j % 2], f"vtok{j % 2}"
                        op("act", lambda e, g=g, vt_=vt_: e.copy(out=vt_[:], in_=g[:]), [gk], [vtk])
                    elif b == 4:
                        th, thk = nextF()
                        op("act", lambda e, g=g, th=th: e.activation(out=th[:], in_=g[:], func=AF.Tanh, scale=0.5), [gk], [thk])
                        op("dve", lambda e, g=g, th=th, j=j: e.scalar_tensor_tensor(out=sgtL[j % 2][:], in0=th[:], scalar=1.0, in1=g[:],
                                                                                    op0=ALU.add, op1=ALU.mult), [gk, thk], [f"sgt{j % 2}"])
                    else:
                        op("act", lambda e, g=g: e.activation(out=xb[:, 0:128], in_=g[:, 0:128], func=AF.Square,
                                                              accum_out=st[:, 1:2]), [gk], ["xb", "st1"])
                        rstd_from_ss(1, 1.0 / 128, RMS_EPS)
                        cb_, cbk = nextB()
                        op("act", lambda e, g=g, cb_=cb_: e.activation(out=cb_[:, 0:128], in_=g[:, 0:128], func=AF.Copy,
                                                                       scale=st[:, 1:2]), [gk, "st1"], [cbk])
                        op("act", lambda e, g=g, cb_=cb_: e.copy(out=cb_[:, 128:256], in_=g[:, 128:256]), [gk], [cbk])
                        op("act", lambda e, g=g, cb_=cb_: e.copy(
                            out=cb_[:, 256:384].rearrange("p (a b) -> p a b", a=4),
                            in_=g[:, 256:288].unsqueeze(1).broadcast_to([128, 4, 32])), [gk], [cbk])
                        op("dve", lambda e, g=g, j=j: e.tensor_scalar(out=sgn[:, j, :], in0=g[:, 288:292], scalar1=0.0,
                                                                      scalar2=-0.5, op0=ALU.is_ge, op1=ALU.add), [gk], [f"sgn{j}"])
                        op("dve", lambda e, g=g, j=j: e.scalar_tensor_tensor(out=aw[:, j, :], in0=g[:, 288:292], scalar=2.0,
                                                                             in1=sgn[:, j, :], op0=ALU.mult, op1=ALU.mult),
                           [gk, f"sgn{j}"], [f"aw{j}"])
                        op("pe", lambda e, cb_=cb_: e.transpose(out=Tp[:, 0:128], in_=cb_[:, 0:128], identity=identb[:]),
                           [cbk, "identb"], ["Tp"])
                        op("act", lambda e: e.copy(out=cT[:], in_=Tp[:, 0:128]), ["Tp"], ["cT"])
                        g2, g2k = nextG()
                        op("pe", lambda e, g2=g2: e.matmul(g2[:, 0:128], lhsT=cT[:], rhs=Wukv[:], start=True, stop=True),
                           ["cT", "Wukv"], [g2k])
                        op("act", lambda e, g2=g2: e.activation(out=xb[:, 0:64], in_=g2[:, 0:64], func=AF.Square,
                                                                accum_out=st[:, 2:3]), [g2k], ["xb", "st2"])
                        rstd_from_ss(2, 1.0 / 64, RMS_EPS)
                        op("dve", lambda e, g2=g2, cb_=cb_: e.scalar_tensor_tensor(
                            out=cb_[:, 384:448], in0=g2[:, 0:64], scalar=st[:, 2:3], in1=C("knw"), op0=ALU.mult, op1=ALU.mult),
                           [g2k, "st2", "CST"], [cbk])
                        op("act", lambda e, g2=g2, ti=ti: e.copy(out=vA[:, ti, 0:64], in_=g2[:, 64:128]), [g2k], [f"vA{ti}"])
                        op("pe", lambda e, cb_=cb_: e.transpose(out=Tp[0:64, 0:128], in_=cb_[:, 384:448], identity=identb[:]),
                           [cbk, "identb"], ["Tp"])
                        op("act", lambda e, pos=pos: e.copy(out=kT[:, pos:pos + 128], in_=Tp[0:64, 0:128]), ["Tp"], [f"kT{ti}"])
                        op("pe", lambda e, cb_=cb_: e.transpose(out=Tp[:, 0:128], in_=cb_[:, 128:256], identity=identb[:]),
                           [cbk, "identb"], ["Tp"])
                        op("dve", lambda e, j=j: e.tensor_tensor(
                            out=iqTb[:, j, :, :], in0=Tp[:, 0:128].unsqueeze(1).broadcast_to([128, 4, 128]),
                            in1=C("BM").rearrange("p (a b) -> p a b", a=4), op=ALU.mult), ["Tp", "CST"], [f"iqTb{j}"])
                        op("pe", lambda e, cb_=cb_: e.transpose(out=Tp[:, 0:128], in_=cb_[:, 256:384], identity=identb[:]),
                           [cbk, "identb"], ["Tp"])
                        op("act", lambda e, pos=pos: e.copy(out=ikT4[:, pos:pos + 128], in_=Tp[:, 0:128]), ["Tp"], [f"ik{ti}"])

                N = (ti + 1) * 128
                if N > KTOP:
                    ls = j % LS
                    for kb in range((N + 511) // 512):
                        k0 = kb * 512
                        w_ = min(512, N - k0)
                        for h in range(4):
                            g, gk = nextG()
                            op("pe", lambda e, g=g, j=j, h=h, k0=k0, w_=w_: e.matmul(
                                g[:, 0:w_], lhsT=iqTb[:, j, h, :], rhs=ikT4[:, k0:k0 + w_], start=True, stop=True),
                               [f"iqTb{j}"] + [f"ik{q}" for q in range(k0 // 128, (k0 + w_) // 128)], [gk])
                            op("act", lambda e, g=g, j=j, h=h, w_=w_: e.activation(
                                out=g[:, 0:w_], in_=g[:, 0:w_], func=AF.Relu, scale=aw[:, j, h:h + 1]), [gk, f"aw{j}"], [gk])
                            if h == 0:
                                op("dve", lambda e, g=g, j=j, ls=ls, k0=k0, w_=w_: e.scalar_tensor_tensor(
                                    out=sc[:, ls, k0:k0 + w_], in0=g[:, 0:w_], scalar=sgn[:, j, 0:1], in1=TB[:, 0:w_],
                                    op0=ALU.mult, op1=ALU.add), [gk, f"sgn{j}", "TB"] + RA, [f"sc{ls}"])
                            else:
                                op("dve", lambda e, g=g, j=j, ls=ls, k0=k0, w_=w_, h=h: e.scalar_tensor_tensor(
                                    out=sc[:, ls, k0:k0 + w_], in0=g[:, 0:w_], scalar=sgn[:, j, h:h + 1], in1=sc[:, ls, k0:k0 + w_],
                                    op0=ALU.mult, op1=ALU.add), [gk, f"sgn{j}", f"sc{ls}"] + RA, [f"sc{ls}"])
                        if kb > 0:
                            op("pool", lambda e, ls=ls, k0=k0, w_=w_, kb=kb: e.tensor_scalar(
                                out=sc[:, ls, k0:k0 + w_], in0=sc[:, ls, k0:k0 + w_], scalar1=-1e-30 * 512 * kb, scalar2=None,
                                op0=ALU.add), [f"sc{ls}"] + RA, [f"sc{ls}"])

                if (j + 1) % LS == 0:
                    sub = list(range(j + 1 - LS, j + 1))
                    bl = [(jj, tiles[jj]) for jj in sub if (tiles[jj] + 1) * 128 > KTOP]
                    nb_ = len(bl)
                    MX, MN, RNG, MID, CNT, PMH, THR = 0, LS, 2 * LS, 3 * LS, 4 * LS, 5 * LS, 6 * LS
                    STP = 8 * LS
                    if nb_ > 0:
                        for (jj, tt) in bl:
                            ls = jj % LS
                            N = (tt + 1) * 128
                            op("dve", lambda e, ls=ls, N=N: e.tensor_reduce(out=bs[:, MX + ls:MX + ls + 1], in_=sc[:, ls, 0:N],
                                                                             axis=AX.X, op=ALU.max), [f"sc{ls}"], ["bs"])
                            op("dve", lambda e, ls=ls, N=N: e.tensor_reduce(out=bs[:, MN + ls:MN + ls + 1], in_=sc[:, ls, 0:N],
                                                                             axis=AX.X, op=ALU.min), [f"sc{ls}"], ["bs"])
                            op("dve", lambda e, ls=ls, N=N: e.memset(sc[0:64, ls, N - 64:N], NEG), [f"sc{ls}"], [f"sc{ls}"])
                        if nb_ < LS:
                            for ls in range(LS):
                                if ls not in [jj % LS for (jj, _) in bl]:
                                    op("dve", lambda e, ls=ls: e.memset(bs[:, MX + ls:MX + ls + 1], 1.0), [], ["bs"])
                                    op("dve", lambda e, ls=ls: e.memset(bs[:, MN + ls:MN + ls + 1], 0.0), [], ["bs"])
                        op("dve", lambda e: e.tensor_tensor(out=bs[:, RNG:RNG + LS], in0=bs[:, MX:MX + LS], in1=bs[:, MN:MN + LS],
                                                            op=ALU.subtract), ["bs"], ["bs"])
                        op("dve", lambda e: e.scalar_tensor_tensor(out=bs[:, MID:MID + LS], in0=bs[:, RNG:RNG + LS], scalar=0.5,
                                                                   in1=bs[:, MN:MN + LS], op0=ALU.mult, op1=ALU.add), ["bs"], ["bs"])
                        op("dve", lambda e: e.tensor_tensor(
                            out=bs[:, STP:STP + LS * KIT].rearrange("p (a k) -> p a k", a=LS),
                            in0=bs[:, RNG:RNG + LS].unsqueeze(2).broadcast_to([128, LS, KIT]),
                            in1=C("pw", 0, KIT).unsqueeze(1).broadcast_to([128, LS, KIT]), op=ALU.mult), ["bs", "CST"], ["bs"])
                        stp3 = bs[:, STP:STP + LS * KIT].rearrange("p (a k) -> p a k", a=LS)
                        for it in range(KIT):
                            for (jj, tt) in bl:
                                ls = jj % LS
                                N = (tt + 1) * 128
                                op("dve", lambda e, ls=ls, N=N: e.tensor_scalar(
                                    out=msk[:, 0:N], in0=sc[:, ls, 0:N], scalar1=bs[:, MID + ls:MID + ls + 1], scalar2=None,
                                    op0=ALU.is_ge, op1=ALU.add, accum_out=bs[:, CNT + ls:CNT + ls + 1]),
                                   [f"sc{ls}", "bs"] + RA, ["msk", "bs"])
                            op("dve", lambda e: e.tensor_scalar(out=bs[:, PMH:PMH + LS], in0=bs[:, CNT:CNT + LS],
                                                                scalar1=float(KTOP) - 0.5, scalar2=-0.5, op0=ALU.is_ge, op1=ALU.add),
                               ["bs"], ["bs"])
                            op("dve", lambda e, it=it: e.tensor_tensor(out=bs[:, PMH:PMH + LS], in0=bs[:, PMH:PMH + LS],
                                                                       in1=stp3[:, :, it], op=ALU.mult), ["bs"], ["bs"])
                            op("dve", lambda e: e.tensor_tensor(out=bs[:, MID:MID + LS], in0=bs[:, MID:MID + LS],
                                                                in1=bs[:, PMH:PMH + LS], op=ALU.add), ["bs"], ["bs"])
                        op("dve", lambda e: e.scalar_tensor_tensor(out=bs[:, THR:THR + LS], in0=bs[:, RNG:RNG + LS],
                                                                   scalar=-(2.0 ** -(KIT + 1)), in1=bs[:, MID:MID + LS],
                                                                   op0=ALU.mult, op1=ALU.add), ["bs"], ["bs"])
                    for jj in sub:
                        retention(jj, tiles[jj])
                    for jj in sub:
                        tt = tiles[jj]
                        nk = tt + 1
                        N = nk * 128
                        ls = jj % LS
                        bis = N > KTOP
                        if bis:
                            op("dve", lambda e, ls=ls, N=N: e.tensor_scalar(
                                out=msk[:, 0:N], in0=sc[:, ls, 0:N], scalar1=bs[:, THR + ls:THR + ls + 1], scalar2=None,
                                op0=ALU.is_ge), [f"sc{ls}", "bs"] + RA, ["msk"])
                            for c0 in range(0, nk, 8):
                                cn = min(8, nk - c0)
                                transposes([msk[:, (c0 + i) * 128:(c0 + i + 1) * 128] for i in range(cn)],
                                           mskT[:, c0:c0 + cn, :], ["mskT"], ["msk"] + RA)
                        steps = [(kt, hf) for kt in range(nk) for hf in range(2)]
                        pm_of = {}

                        def emit_st(si, jj=jj, tt=tt, nk=nk, bis=bis):
                            kt, hf = steps[si]
                            if bis:
                                mk_ap, mkk = mskT[:, kt, :], "mskT"
                            elif kt == nk - 1:
                                mk_ap, mkk = LASTM[:], "LASTM"
                            else:
                                mk_ap, mkk = ONESM[:], "ONESM"
                            g, gk = nextG()
                            op("pe", lambda e, g=g, kt=kt, jj=jj, hf=hf: e.matmul(
                                g[:], lhsT=kT[:, kt * 128:(kt + 1) * 128], rhs=qT[:, jj, hf * 512:(hf + 1) * 512],
                                start=True, stop=True), [f"kT{kt}", f"qT{jj}"], [gk])
                            ei_ = si % NE
                            E_, Ek = Eb[ei_], f"E{ei_}"
                            if kt >= tt - 1:
                                kd = 0 if kt == tt else 1
                                op("dve", lambda e, g=g, kd=kd, hf=hf: e.scalar_tensor_tensor(
                                    out=g[:], in0=g[:], scalar=0.125, in1=Bp[:, kd, hf * 512:(hf + 1) * 512],
                                    op0=ALU.mult, op1=ALU.add), [gk, f"Bp{kd}"], [gk])
                                op("act", lambda e, g=g, E_=E_: e.activation(out=E_[:], in_=g[:], func=AF.Exp), [gk], [Ek])
                            else:
                                op("act", lambda e, g=g, E_=E_: e.activation(out=E_[:], in_=g[:], func=AF.Exp, scale=0.125),
                                   [gk], [Ek])
                            Pm_, Pk = Pm[ei_], f"Pm{ei_}"
                            op("dve" if hf == 0 else "pool", lambda e, E_=E_, Pm_=Pm_, mk_ap=mk_ap: e.tensor_tensor(
                                out=Pm_[:].rearrange("p (h t) -> p h t", h=4), in0=E_[:].rearrange("p (h t) -> p h t", h=4),
                                in1=mk_ap.unsqueeze(1).broadcast_to([128, 4, 128]), op=ALU.mult), [Ek, mkk] + RA, [Pk])
                            pm_of[si] = (Pm_, Pk)

                        def emit_pv(si, nk=nk):
                            kt, hf = steps[si]
                            Pm_, Pk = pm_of[si]
                            for h4 in range(4):
                                op("pe", lambda e, Pm_=Pm_, h4=h4, hf=hf, kt=kt, nk=nk: e.matmul(
                                    PV[hf][:, h4 * 65:(h4 + 1) * 65], lhsT=Pm_[:, h4 * 128:(h4 + 1) * 128], rhs=vA[:, kt, :],
                                    start=(kt == 0 and h4 == 0), stop=(kt == nk - 1 and h4 == 3), skip_group_check=True),
                                   [Pk, f"vA{kt}"], [f"PV{hf}"])

                        LOOK = 2
                        ns = len(steps)
                        for si in range(min(LOOK, ns)):
                            emit_st(si)
                        for si in range(ns):
                            if si + LOOK < ns:
                                emit_st(si + LOOK)
                            emit_pv(si)
                        oa, oak = nextB()
                        for hf in range(2):
                            pv3 = PV[hf][:, 0:260].rearrange("p (h c) -> p h c", h=4)
                            op("dve", lambda e, pv3=pv3, hf=hf: e.reciprocal(out=st[:, 40 + hf * 4:44 + hf * 4], in_=pv3[:, :, 64]),
                               [f"PV{hf}"], [f"st4{hf}"])
                            op("dve", lambda e, pv3=pv3, hf=hf, oa=oa: e.tensor_tensor(
                                out=oa[:, hf * 256:(hf + 1) * 256].rearrange("p (h d) -> p h d", h=4), in0=pv3[:, :, 0:64],
                                in1=st[:, 40 + hf * 4:44 + hf * 4].unsqueeze(2).broadcast_to([128, 4, 64]), op=ALU.mult),
                               [f"PV{hf}", f"st4{hf}"], [oak])
                        transposes([oa[:, c * 128:(c + 1) * 128] for c in range(4)], catT[:, 0:4, :], ["catT"], [oak])
                        op("pool", lambda e, jj=jj: e.tensor_copy(out=catT[:, 4:8, :], in_=obT[:, jj, :, :]), [f"obT{jj}"], ["catT"])
                        for nbk in range(2):
                            g, gk = nextG()
                            for c in range(8):
                                op("pe", lambda e, g=g, c=c, nbk=nbk: e.matmul(
                                    g[:], lhsT=catT[:, c, :], rhs=Wout[:, c, nbk * 512:(nbk + 1) * 512], start=(c == 0), stop=(c == 7)),
                                   ["catT", "Wout"], [gk])
                            op("dve", lambda e, g=g, jj=jj, nbk=nbk: e.tensor_tensor(
                                out=X[:, jj, nbk * 512:(nbk + 1) * 512], in0=g[:], in1=X[:, jj, nbk * 512:(nbk + 1) * 512], op=ALU.add),
                               [gk, f"X{jj}"], [f"X{jj}"])
                        op("act", lambda e, jj=jj: e.activation(out=xb[:], in_=X[:, jj, :], func=AF.Square, accum_out=st[:, 3:4]),
                           [f"X{jj}"], ["xb", "st3"])
                        rstd_from_ss(3, 1.0 / D_MODEL, RMS_EPS)
                        op("act", lambda e, jj=jj: e.activation(out=xb[:], in_=X[:, jj, :], func=AF.Copy, scale=st[:, 3:4]),
                           [f"X{jj}", "st3"], ["xb"])
                        transposes([xb[:, k * 128:(k + 1) * 128] for k in range(8)], xT[:, :, jj * 128:(jj + 1) * 128],
                                   [f"xT{jj}"], ["xb"])
            op("pool", lambda e: e.memset(st[:, 62:63], 0.0), [], RA + RB)
            TW = GT * 128
            for c in range(NHC):
                s_ = c % 2
                P.dma("sp", lambda e, c=c, s_=s_: e.dma_start(out=wgs[s_], in_=wg_s[:, c]), f"wg{s_}",
                      reads=["wg_s"] + RB, writes=[f"wg{s_}"])
                P.dma("sp", lambda e, c=c, s_=s_: e.dma_start(out=wus[s_], in_=wu_s[:, c]), f"wu{s_}",
                      reads=["wu_s"] + RB, writes=[f"wu{s_}"])
                gg, ggk = nextG()
                gu, guk = nextG()
                for k in range(8):
                    op("pe", lambda e, gg=gg, k=k, s_=s_: e.matmul(gg[:, 0:TW], lhsT=wgs[s_][:, k, :], rhs=xT[:, k, :],
                                                                   start=(k == 0), stop=(k == 7)),
                       [f"wg{s_}"] + RB + [f"xT{q}" for q in range(GT)], [ggk])
                for k in range(8):
                    op("pe", lambda e, gu=gu, k=k, s_=s_: e.matmul(gu[:, 0:TW], lhsT=wus[s_][:, k, :], rhs=xT[:, k, :],
                                                                   start=(k == 0), stop=(k == 7)),
                       [f"wu{s_}"] + RB + [f"xT{q}" for q in range(GT)], [guk])
                sl, slk = nextF()
                op("act", lambda e, gg=gg, sl=sl: e.activation(out=sl[:, 0:TW], in_=gg[:, 0:TW], func=AF.Silu), [ggk], [slk])
                op("dve", lambda e, gu=gu, sl=sl, c=c: e.tensor_tensor(out=hT[:, c, :], in0=gu[:, 0:TW], in1=sl[:, 0:TW], op=ALU.mult),
                   [guk, slk] + RB, ["hT"])
            TpF = Tp[:].bitcast(F32)
            acc_banks = [(Gp[0][:], "G0"), (Gp[1][:], "G1"), (Gp[2][:], "G2"), (Gp[3][:], "G3"),
                         (PV[0][:], "PV0"), (PV[1][:], "PV1"), (Yp[:], "Yp"), (TpF, "Tp")]
            accs = {}
            for t_ in range(GT):
                for nbk in range(2):
                    accs[(t_, nbk)] = acc_banks[t_ * 2 + nbk]
            for c in range(NHC):
                s_ = c % NWD
                P.dma("sp", lambda e, c=c, s_=s_: e.dma_start(out=wds[s_], in_=wd_s[c]), f"wd{s_}",
                      reads=["wd_s"] + RB, writes=[f"wd{s_}"])
                for t_ in range(GT):
                    for nbk in range(2):
                        g, gk = accs[(t_, nbk)]
                        op("pe", lambda e, g=g, c=c, t_=t_, nbk=nbk, s_=s_: e.matmul(
                            g, lhsT=hT[:, c, t_ * 128:(t_ + 1) * 128], rhs=wds[s_][:, nbk * 512:(nbk + 1) * 512],
                            start=(c == 0), stop=(c == NHC - 1)), ["hT", f"wd{s_}"] + RB, [gk])
            for t_ in range(GT):
                for nbk in range(2):
                    g, gk = accs[(t_, nbk)]
                    op("dve", lambda e, g=g, t_=t_, nbk=nbk: e.tensor_tensor(
                        out=X[:, t_, nbk * 512:(nbk + 1) * 512], in0=g, in1=X[:, t_, nbk * 512:(nbk + 1) * 512], op=ALU.add),
                       [gk, f"X{t_}"], [f"X{t_}"])
                row0 = seq * S + tiles[t_] * 128
                P.dma("sp", lambda e, t_=t_, row0=row0: e.dma_start(out=y_d[row0:row0 + 128, :], in_=X[:, t_, :]),
                      f"y{t_}", reads=[f"X{t_}"])
    P.emit()
    return P


C_LAYOUT = [("ident", 128), ("n1w", 8), ("n2w", 8), ("kvnw", 1), ("qnw", 64), ("knw", 64),
            ("gnw", 512), ("gnb", 512), ("wuk", 64), ("wuv", 64), ("DQ", 512), ("DK", 512),
            ("DKtok", 8), ("DMASK", 128), ("GC", 4), ("BM", 512), ("LASTM", 128), ("pw", 32),
            ("negh", 8)]


def _t5_bucket_np(rel):
    import jax
    import jax.numpy as jnp
    cpu = jax.devices("cpu")[0]
    with jax.default_device(cpu):
        rel = jnp.asarray(rel, dtype=jnp.int32)
        half, max_exact, max_distance = 16, 8, 128
        ret = jnp.where(rel > 0, half, 0)
        n = jnp.abs(rel)
        nf = jnp.maximum(n, 1).astype(jnp.float32)
        large = max_exact + (jnp.log(nf / max_exact) / math.log(max_distance / max_exact)
                             * (half - max_exact)).astype(jnp.int32)
        large = jnp.minimum(large, half - 1)
        out = ret + jnp.where(n < max_exact, n, large)
        return np.asarray(out)


def _rot_tables(S):
    import jax
    import jax.numpy as jnp
    cpu = jax.devices("cpu")[0]
    with jax.default_device(cpu):
        half = 32
        inv = 10000.0 ** (-jnp.arange(half, dtype=jnp.float32) / half)
        ang = jnp.arange(S).astype(jnp.float32)[:, None] * inv[None, :]
        cos = np.asarray(jnp.cos(ang))
        sin = np.asarray(jnp.sin(ang))
    rot = np.concatenate([cos, cos, -sin, sin], axis=1).astype(np.float32)
    return np.ascontiguousarray(rot.reshape(S // 128, 128, 128))


def _host_consts(inp, S):
    f32 = np.float32
    secs = {}
    secs["ident"] = np.eye(128, dtype=f32)
    secs["n1w"] = np.ascontiguousarray(inp["norm1_w"][0].reshape(8, 128).T)
    secs["n2w"] = np.ascontiguousarray(inp["norm2_w"][0].reshape(8, 128).T)
    secs["kvnw"] = inp["kv_norm_w"][0].reshape(128, 1)
    secs["qnw"] = np.broadcast_to(inp["q_norm_w"][0][None, :], (128, 64))
    secs["knw"] = np.broadcast_to(inp["k_norm_w"][0][None, :], (128, 64))
    secs["gnw"] = np.broadcast_to(inp["ret_gn_w"][0][None, :], (128, 512))
    secs["gnb"] = np.broadcast_to(inp["ret_gn_b"][0][None, :], (128, 512))
    secs["wuk"] = inp["w_uk"][0]
    secs["wuv"] = inp["w_uv"][0]
    hh = np.arange(8, dtype=np.float64)
    log_g = np.log1p(-(2.0 ** (-5.0 - hh)))
    t = np.arange(128, dtype=np.float64)
    row_h = np.arange(128) // 64
    DQ = np.zeros((128, 4, 128)); DK = np.zeros((128, 4, 128)); GC = np.zeros((128, 4))
    for pq in range(4):
        lg = log_g[2 * pq + row_h]
        DQ[:, pq, :] = np.exp((t[None, :] + 1.0) * lg[:, None])
        DK[:, pq, :] = np.exp(-(t[None, :] + 1.0) * lg[:, None]) * 0.125
        GC[:, pq] = np.exp(128.0 * lg)
    secs["DQ"] = DQ.reshape(128, 512)
    secs["DK"] = DK.reshape(128, 512)
    secs["DKtok"] = np.exp(-(t[:, None] + 1.0) * log_g[None, :]) * 0.125
    jj = np.arange(128)
    secs["DMASK"] = (jj[None, :] >= jj[:, None]).astype(f32)
    secs["GC"] = GC
    BM = np.zeros((128, 4, 128), f32)
    for h in range(4):
        BM[h * 32:(h + 1) * 32, h, :] = 1.0
    secs["BM"] = BM.reshape(128, 512)
    LM = np.ones((128, 128), f32)
    LM[64:, :64] = 0.0
    secs["LASTM"] = LM
    secs["pw"] = np.broadcast_to((2.0 ** -(np.arange(32, dtype=np.float64) + 1.0))[None, :], (128, 32))
    secs["negh"] = np.full((128, 8), -0.5)
    cst = np.zeros((128, CW), f32)
    o = 0
    for nm, w in C_LAYOUT:
        cst[:, o:o + w] = np.asarray(secs[nm], dtype=f32)
        o += w
    s_ = np.arange(128)[:, None]
    t_ = np.arange(128)[None, :]
    rb = inp["rel_bias"]
    b0 = rb[_t5_bucket_np(s_ - t_)]
    b1 = rb[_t5_bucket_np(s_ - 128 - t_)]
    cb = rb[_t5_bucket_np(np.full((128, 128), -1000))]
    bias_t = np.stack([np.transpose(b, (0, 2, 1)).reshape(128, 1024) for b in (b0, b1, cb)]).astype(f32)
    tb = np.broadcast_to((-1e-30 * np.arange(512, dtype=np.float64)).astype(f32)[None, :], (128, 512))
    return cst, np.ascontiguousarray(bias_t), np.ascontiguousarray(tb), _rot_tables(S)


_NC_CACHE = {}


def run_cores(inp, xs, S, NSEQ, KTOP):
    key = (S, NSEQ, KTOP)
    nc = bass.Bass("TRN2", target_bir_lowering=False)
    build(nc, S, NSEQ, KTOP)
    cst, bias_t, tb, rot = _host_consts(inp, S)
    base = {
        "w_in": np.ascontiguousarray(inp["w_in"][0]), "w_out": np.ascontiguousarray(inp["w_out"][0]),
        "w_gate": np.ascontiguousarray(inp["w_gate"][0]), "w_up": np.ascontiguousarray(inp["w_up"][0]),
        "w_down": np.ascontiguousarray(inp["w_down"][0]), "cst": cst, "bias_t": bias_t, "tb": tb, "rot": rot,
    }
    in_maps = [dict(base, x=np.ascontiguousarray(x)) for x in xs]
    res = run_bass_kernel_spmd(nc, in_maps, core_ids=list(range(len(xs))))
    return [r["y"] for r in res.results]


def kernel(**inputs):
    inp = {k: np.asarray(v) for k, v in inputs.items()}
    x = inp["x"]
    B, S, D = x.shape
    ncores = 8
    nseq = B // ncores
    xs = [x[c * nseq:(c + 1) * nseq].reshape(nseq * S, D) for c in range(ncores)]
    ys = run_cores(inp, xs, S, nseq, min(256, S // 4))
    return np.concatenate([y.reshape(nseq, S, D) for y in ys], axis=0).astype(np.float32)
```

```python
import contextlib
import math
import numpy as np
import concourse.bass as bass
import concourse.mybir as mybir
from concourse.bass_utils import run_bass_kernel_spmd

F32 = mybir.dt.float32
BF16 = mybir.dt.bfloat16
AF = mybir.ActivationFunctionType
ALU = mybir.AluOpType
AX = mybir.AxisListType

D_MODEL = 1024
HID = 2816
NHC = HID // 128
IN_W = 2852
RMS_EPS = 1e-6
GN_EPS = 1e-5
KIT = 20
CW = 3272
NEG = -3.0e38

ENGS = ("pe", "act", "dve", "pool", "sp")
CH = 20000
DCH = 1500


class Prog:
    def __init__(self, nc):
        self.nc = nc
        self.q = {e: [] for e in ENGS}
        self.n = {e: 0 for e in ENGS}
        self.seen = {e: {} for e in ENGS}
        self.lastw = {}
        self.readers = {}
        self.dcount = {}
        self.dbatch = {}
        self.dopen = {}

    def _deps(self, eng, reads, writes):
        deps = {}

        def add(src, idx, raw):
            if src == eng and eng in ("pe", "sp"):
                return
            if deps.get(src, -1) < idx:
                deps[src] = idx

        for r in reads:
            if r in self.lastw:
                add(*self.lastw[r], True)
        for w in writes:
            if w in self.lastw:
                add(*self.lastw[w], False)
            for (s, i) in self.readers.get(w, ()):
                add(s, i, False)
        out = []
        for src, idx in deps.items():
            if self.seen[eng].get(src, -1) >= idx:
                continue
            self.seen[eng][src] = idx
            out.append((src, idx))
        return out

    def _commit(self, sig, reads, writes):
        for r in reads:
            self.readers.setdefault(r, []).append(sig)
        for w in writes:
            self.lastw[w] = sig
            self.readers[w] = []

    def op(self, eng, fn, reads=(), writes=()):
        waits = self._deps(eng, reads, writes)
        idx = self.n[eng]
        self.n[eng] += 1
        self.q[eng].append(("op", fn, waits, idx))
        self._commit((eng, idx), reads, writes)

    def dma(self, queue, fn, stream, reads=(), writes=(), batch=False):
        waits = self._deps(queue, reads, writes)
        idx = self.dcount.get(stream, 0)
        self.dcount[stream] = idx + 1
        self.dbatch.setdefault(stream, []).append(None)
        self.dopen.setdefault(stream, []).append(idx)
        if not batch:
            self.close(stream)
        self.q[queue].append(("dma", fn, waits, (stream, idx)))
        self._commit(("dma:" + stream, idx), reads, writes)

    def close(self, stream):
        ids = self.dopen.get(stream, [])
        if ids:
            for i in ids:
                self.dbatch[stream][i] = ids[-1]
            self.dopen[stream] = []

    def emit(self):
        nc = self.nc
        for s in list(self.dopen):
            self.close(s)
        with contextlib.ExitStack() as st:
            sems = {}
            for e in ENGS:
                if e == "sp":
                    continue
                ne = max(1, (self.n[e] + CH - 1) // CH)
                sems[e] = [st.enter_context(nc.semaphore(f"s_{e}_{k}")) for k in range(ne)]
            for s, c in self.dcount.items():
                ne = max(1, (c + DCH - 1) // DCH)
                sems["dma:" + s] = [st.enter_context(nc.semaphore(f"d_{s}_{k}")) for k in range(ne)]

            def wait_args(src, idx):
                if src.startswith("dma:"):
                    end = self.dbatch[src[4:]][idx]
                    return sems[src][end // DCH], 16 * (end % DCH + 1)
                return sems[src][idx // CH], idx % CH + 1

            def run(name, eng):
                for kind, fn, waits, info in self.q[name]:
                    for (src, idx) in waits:
                        sem, val = wait_args(src, idx)
                        eng.wait_ge(sem, val)
                    ins = fn(eng)
                    if kind == "op":
                        ins.then_inc(sems[name][info // CH], 1)
                    else:
                        stream, idx = info
                        ins.then_inc(sems["dma:" + stream][idx // DCH], 16)
                if name == "sp":
                    for s, c in self.dcount.items():
                        sem, val = wait_args("dma:" + s, c - 1)
                        eng.wait_ge(sem, val)

            with nc.Block() as block:
                @block.tensor
                def _(eng):
                    run("pe", eng)

                @block.scalar
                def _(eng):
                    run("act", eng)

                @block.vector
                def _(eng):
                    run("dve", eng)

                @block.gpsimd
                def _(eng):
                    run("pool", eng)

                @block.sync
                def _(eng):
                    run("sp", eng)


def build(nc, S, NSEQ, KTOP):
    NT = S // 128
    GT = min(4, NT)
    NG = NT // GT
    LS = 2 if GT >= 2 else 1
    P = Prog(nc)

    def din(name, shape, dt=F32):
        return nc.dram_tensor(name, list(shape), dt, kind="ExternalInput").ap()

    x_d = din("x", [NSEQ * S, D_MODEL])
    w_in_d = din("w_in", [D_MODEL, IN_W])
    w_out_d = din("w_out", [D_MODEL, D_MODEL])
    w_gate_d = din("w_gate", [D_MODEL, HID])
    w_up_d = din("w_up", [D_MODEL, HID])
    w_down_d = din("w_down", [HID, D_MODEL])
    cst_d = din("cst", [128, CW])
    bias_d = din("bias_t", [3, 128, 1024])
    tb_d = din("tb", [128, 512])
    rot_d = din("rot", [NT, 128, 128])
    y_d = nc.dram_tensor("y", [NSEQ * S, D_MODEL], F32, kind="ExternalOutput").ap()
    wg_s = nc.dram_tensor("wg_s", [128, NHC, 8, 128], BF16, kind="Internal").ap()
    wu_s = nc.dram_tensor("wu_s", [128, NHC, 8, 128], BF16, kind="Internal").ap()
    wd_s = nc.dram_tensor("wd_s", [NHC, 128, D_MODEL], BF16, kind="Internal").ap()

    def sb(name, shape, dt):
        return nc.alloc_sbuf_tensor(name, list(shape), dt)

    Win = sb("Win", [128, 8, IN_W], BF16)
    Wout = sb("Wout", [128, 8, D_MODEL], BF16)
    Wukv = sb("Wukv", [128, 128], BF16)
    CST = sb("CST", [128, CW], F32)
    c_off = {}
    o = 0
    for nm, w in [("ident", 128), ("n1w", 8), ("n2w", 8), ("kvnw", 1), ("qnw", 64), ("knw", 64),
                  ("gnw", 512), ("gnb", 512), ("wuk", 64), ("wuv", 64), ("DQ", 512), ("DK", 512),
                  ("DKtok", 8), ("DMASK", 128), ("GC", 4), ("BM", 512), ("LASTM", 128), ("pw", 32),
                  ("negh", 8)]:
        c_off[nm] = (o, w)
        o += w
    assert o <= CW

    def C(nm, a=0, b=None):
        o0, w = c_off[nm]
        return CST[:, o0 + a: o0 + (w if b is None else b)]

    identb = sb("identb", [128, 128], BF16)
    Bp = sb("Bp", [128, 2, 1024], F32)
    TB = sb("TB", [128, 512], F32)
    ONESM = sb("ONESM", [128, 128], BF16)
    LASTM = sb("LASTMb", [128, 128], BF16)
    kT = sb("kT", [64, S], BF16)
    vA = sb("vA", [128, NT, 65], BF16)
    ikT4 = sb("ikT4", [128, S], BF16)
    stf = sb("stf", [128, 4, 64], F32)
    stb = sb("stb", [128, 4, 64], BF16)
    X = sb("X", [128, GT, D_MODEL], F32)
    xT = sb("xT", [128, 8, GT * 128], BF16)
    qT = sb("qT", [64, GT, 1024], BF16)
    iqTb = sb("iqTb", [128, GT, 4, 128], BF16)
    obT = sb("obT", [128, GT, 4, 128], BF16)
    aw = sb("aw", [128, GT, 4], F32)
    sgn = sb("sgn", [128, GT, 4], F32)
    ROT = sb("ROT", [128, 128], F32)
    xb = sb("xb", [128, D_MODEL], BF16)
    NF = 4
    ftmp = [sb(f"ft{i}", [128, 512], F32) for i in range(NF)]
    NB = 4
    btmp = [sb(f"bt{i}", [128, 512], BF16) for i in range(NB)]
    qbTL = [sb(f"qbT{i}", [128, 4, 128], BF16) for i in range(2)]
    kbTL = [sb(f"kbT{i}", [128, 4, 128], BF16) for i in range(2)]
    vtokL = [sb(f"vtok{i}", [128, 512], BF16) for i in range(2)]
    ktokL = [sb(f"ktok{i}", [128, 512], BF16) for i in range(2)]
    sgtL = [sb(f"sgt{i}", [128, 512], BF16) for i in range(2)]
    scb = [sb("scb0", [128, 256], BF16)]
    xb2 = sb("xb2", [128, 128], BF16)
    DMASKb = sb("DMASKb", [128, 128], BF16)
    cT = sb("cT", [128, 128], BF16)
    scm = [sb(f"scm{i}", [128, 256], BF16) for i in range(2)]
    dS = sb("dS", [128, 64], F32)
    catT = sb("catT", [128, 8, 128], BF16)
    NE = 3
    Eb = [sb(f"E{i}", [128, 512], BF16) for i in range(NE)]
    Pm = [sb(f"Pm{i}", [128, 512], BF16) for i in range(NE)]
    st = sb("st", [128, 64], F32)
    bs = sb("bs", [128, 8 * LS + LS * KIT + 16], F32)
    RW = 9216
    R = sb("R", [128, RW], F32)
    Rb = R[:].bitcast(BF16)
    sc = R[:, 0:LS * S].rearrange("p (a s) -> p a s", a=LS)
    msk = Rb[:, 2 * LS * S: 2 * LS * S + S]
    mskT = Rb[:, 2 * LS * S + S: 2 * LS * S + 2 * S].rearrange("p (a b) -> p a b", b=128)
    assert 2 * LS * S + 2 * S <= 2 * RW
    hT = Rb[:, 0:NHC * GT * 128].rearrange("p (c t) -> p c t", c=NHC)
    o_w = NHC * 512
    wgs = [Rb[:, o_w + s * 2048: o_w + s * 2048 + 1024].rearrange("p (k j) -> p k j", k=8) for s in range(2)]
    wus = [Rb[:, o_w + s * 2048 + 1024: o_w + s * 2048 + 2048].rearrange("p (k j) -> p k j", k=8) for s in range(2)]
    NWD = 3
    wds = [Rb[:, o_w + 4096 + s * 1024: o_w + 4096 + (s + 1) * 1024] for s in range(NWD)]
    assert o_w + 4096 + NWD * 1024 <= 2 * RW
    stg = [R[:, s * IN_W:(s + 1) * IN_W] for s in range(2)]
    stgb = [Rb[:, 2 * 2 * IN_W + s * HID: 2 * 2 * IN_W + (s + 1) * HID] for s in range(2)]
    assert 4 * IN_W + 2 * HID <= 2 * RW
    Gp = [nc.alloc_psum_tensor(f"G{i}", [128, 512], F32) for i in range(4)]
    PV = [nc.alloc_psum_tensor(f"PV{i}", [128, 512], F32) for i in range(2)]
    Tp = nc.alloc_psum_tensor("Tp", [128, 1024], BF16)
    Yp = nc.alloc_psum_tensor("Yp", [128, 512], F32)
    gctr = [0]

    def nextG():
        i = gctr[0] % 4
        gctr[0] += 1
        return Gp[i], f"G{i}"

    fctr = [0]

    def nextF():
        i = fctr[0] % NF
        fctr[0] += 1
        return ftmp[i], f"ft{i}"

    bctr = [0]

    def nextB():
        i = bctr[0] % NB
        bctr[0] += 1
        return btmp[i], f"bt{i}"

    op = P.op
    RA = ["RA"]
    RB = ["RB"]

    P.dma("sp", lambda e: e.dma_start(out=CST[:], in_=cst_d), "init", writes=["CST"], batch=True)
    P.dma("sp", lambda e: e.dma_start(out=TB[:], in_=tb_d), "init", writes=["TB"], batch=True)
    P.dma("sp", lambda e: e.dma_start(out=Bp[:, 0, :], in_=bias_d[0]), "init", writes=["Bp0"], batch=True)
    P.dma("sp", lambda e: e.dma_start(out=Bp[:, 1, :], in_=bias_d[1]), "init", writes=["Bp1"], batch=True)
    P.dma("sp", lambda e: e.dma_start(out=ftmp[0][:], in_=bias_d[2][:, 0:512]), "init", writes=["ft0"], batch=True)
    P.dma("sp", lambda e: e.dma_start(out=ftmp[1][:], in_=bias_d[2][:, 512:1024]), "init", writes=["ft1"], batch=True)
    P.close("init")
    op("dve", lambda e: e.tensor_copy(out=identb[:], in_=C("ident")), ["CST"], ["identb"])
    op("dve", lambda e: e.tensor_copy(out=LASTM[:], in_=C("LASTM")), ["CST"], ["LASTM"])
    op("dve", lambda e: e.memset(ONESM[:], 1.0), [], ["ONESM"])
    op("dve", lambda e: e.tensor_copy(out=DMASKb[:], in_=C("DMASK")), ["CST"], ["DMASKb"])
    op("dve", lambda e: e.tensor_scalar(out=C("gnw"), in0=C("gnw"), scalar1=0.5, scalar2=None, op0=ALU.mult), ["CST"], ["CST"])
    op("dve", lambda e: e.tensor_scalar(out=C("gnb"), in0=C("gnb"), scalar1=0.5, scalar2=None, op0=ALU.mult), ["CST"], ["CST"])
    op("dve", lambda e: e.memset(bs[:], 0.0), [], ["bs"])
    op("dve", lambda e: e.memset(st[:], 0.0), [], ["stall"])
    for kd in range(2):
        for hf in range(2):
            op("dve", lambda e, kd=kd, hf=hf: e.tensor_tensor(
                out=Bp[:, kd, hf * 512:(hf + 1) * 512], in0=Bp[:, kd, hf * 512:(hf + 1) * 512],
                in1=ftmp[hf][:], op=ALU.subtract), [f"Bp{kd}", f"ft{hf}"], [f"Bp{kd}"])
    op("dve", lambda e: e.tensor_scalar(out=Wukv[:], in0=CST[:, c_off["wuk"][0]: c_off["wuk"][0] + 128],
                                        scalar1=C("kvnw"), scalar2=None, op0=ALU.mult), ["CST"], ["Wukv"])
    op("pool", lambda e: e.memset(vA[:, :, 64:65], 1.0), [], [f"vA{t}" for t in range(NT)])
    ecyc = ["dve", "pool"]
    ei = [0]

    def ceng():
        ei[0] += 1
        return ecyc[ei[0] % 2]

    for k in range(8):
        s_ = k % 2
        P.dma("sp", lambda e, k=k, s_=s_: e.dma_start(out=stg[s_], in_=w_in_d[k * 128:(k + 1) * 128, :]),
              f"stg{s_}", reads=[], writes=[f"stg{s_}"])
        for (a, b, dst) in [(0, 512, 0), (512, 804, 2560), (804, 2852, 512)]:
            en = ceng()
            op(en, lambda e, k=k, s_=s_, a=a, b=b, dst=dst: e.tensor_scalar(
                out=Win[:, k, dst:dst + (b - a)], in0=stg[s_][:, a:b], scalar1=C("n1w", k, k + 1), scalar2=None,
                op0=ALU.mult), [f"stg{s_}", "CST"], ["Win"])
    for k in range(8):
        s_ = k % 2
        P.dma("sp", lambda e, k=k, s_=s_: e.dma_start(out=stg[s_][:, 0:1024], in_=w_out_d[k * 128:(k + 1) * 128, :]),
              f"stg{s_}", writes=[f"stg{s_}"])
        op(ceng(), lambda e, k=k, s_=s_: e.tensor_copy(out=Wout[:, k, :], in_=stg[s_][:, 0:1024]),
           [f"stg{s_}"], ["Wout"])
    wst_keys = []
    for (src_d, dst_d, nm) in [(w_gate_d, wg_s, "wg_s"), (w_up_d, wu_s, "wu_s")]:
        for k in range(8):
            s_ = k % 2
            P.dma("sp", lambda e, k=k, s_=s_, src_d=src_d: e.dma_start(out=stg[s_][:, 0:HID], in_=src_d[k * 128:(k + 1) * 128, :]),
                  f"stg{s_}", writes=[f"stg{s_}"])
            op(ceng(), lambda e, k=k, s_=s_: e.tensor_scalar(
                out=stgb[s_], in0=stg[s_][:, 0:HID], scalar1=C("n2w", k, k + 1), scalar2=None, op0=ALU.mult),
               [f"stg{s_}", "CST"], [f"stgb{s_}"])
            P.dma("sp", lambda e, k=k, s_=s_, dst_d=dst_d: e.dma_start(
                out=dst_d[:, :, k, :], in_=stgb[s_].rearrange("p (c j) -> p c j", j=128)),
                f"stgb{s_}", reads=[f"stgb{s_}"], writes=[f"{nm}{k}"])
            wst_keys.append(f"{nm}{k}")
    for c2 in range(NHC // 2):
        s_ = c2 % 2
        P.dma("sp", lambda e, c2=c2, s_=s_: e.dma_start(
            out=stg[s_][:, 0:2048].rearrange("p (c n) -> p c n", c=2),
            in_=w_down_d[c2 * 256:(c2 + 1) * 256, :].rearrange("(c p) n -> p c n", p=128)),
            f"stg{s_}", writes=[f"stg{s_}"])
        op(ceng(), lambda e, s_=s_: e.tensor_copy(out=stgb[s_][:, 0:2048], in_=stg[s_][:, 0:2048]),
           [f"stg{s_}"], [f"stgb{s_}"])
        P.dma("sp", lambda e, c2=c2, s_=s_: e.dma_start(
            out=wd_s[2 * c2:2 * c2 + 2].rearrange("c p n -> p c n"),
            in_=stgb[s_][:, 0:2048].rearrange("p (c n) -> p c n", c=2)),
            f"stgb{s_}", reads=[f"stgb{s_}"], writes=[f"wd_s{c2}"])
        wst_keys.append(f"wd_s{c2}")
    op("pool", lambda e: e.memset(st[:, 60:61], 0.0), wst_keys, ["stg0", "stg1", "stgb0", "stgb1", "RA", "RB", "wg_s", "wu_s", "wd_s"])

    def rstd_from_ss(col, inv_n, eps):
        op("dve", lambda e: e.tensor_scalar(out=st[:, col:col + 1], in0=st[:, col:col + 1], scalar1=inv_n,
                                            scalar2=eps, op0=ALU.mult, op1=ALU.add), [f"st{col}"], [f"st{col}"])
        op("pool", lambda e: e.tensor_tensor(out=st[:, col:col + 1], in0=st[:, col:col + 1], in1=C("negh", 0, 1),
                                             op=ALU.pow), [f"st{col}", "CST"], [f"st{col}"])

    def transposes(src_aps, dst_ap, dst_keys, src_keys, rows=128, cols=128, evac="act"):
        n = len(src_aps)
        for i, a in enumerate(src_aps):
            op("pe", lambda e, i=i, a=a: e.transpose(out=Tp[0:cols, i * 128:(i + 1) * 128], in_=a, identity=identb[:]),
               src_keys + ["identb"], ["Tp"])
        srcv = Tp[0:cols, 0:n * 128].rearrange("p (a b) -> p a b", b=128)
        if evac == "act":
            op("act", lambda e: e.copy(out=dst_ap, in_=srcv), ["Tp"], dst_keys)
        else:
            op("dve", lambda e: e.tensor_copy(out=dst_ap, in_=srcv), ["Tp"], dst_keys)


    def retention(j, ti):
        l = j % 2
        qb_, kb_, vt_, kt_, sg_ = qbTL[l], kbTL[l], vtokL[l], ktokL[l], sgtL[l]
        for pq in range(4):
            sm = scm[pq % 2]
            smk = f"scm{pq % 2}"
            sb_, sbk = scb[0], "scb0"
            for hh in range(2):
                r0, r1 = hh * 64, hh * 64 + 64
                g, gk = nextG()
                op("pe", lambda e, g=g, pq=pq, r0=r0, r1=r1: e.matmul(
                    g[:, 0:128], lhsT=kb_[r0:r1, pq, :], rhs=qb_[r0:r1, pq, :], start=True, stop=True),
                   [f"kbT{l}", f"qbT{l}"], [gk])
                op("act", lambda e, g=g, sb_=sb_, hh=hh: e.copy(out=sb_[:, hh * 128:(hh + 1) * 128], in_=g[:, 0:128]), [gk], [sbk])
            op("pool", lambda e, sb_=sb_, sm=sm: e.tensor_tensor(
                out=sm[:].rearrange("p (a b) -> p a b", a=2), in0=sb_[:].rearrange("p (a b) -> p a b", a=2),
                in1=DMASKb[:].unsqueeze(1).broadcast_to([128, 2, 128]), op=ALU.mult), [sbk, "DMASKb"], [smk])
            for hh in range(2):
                h = 2 * pq + hh
                r0, r1 = hh * 64, hh * 64 + 64
                op("pe", lambda e, sm=sm, h=h, hh=hh: e.matmul(
                    Yp[:, h * 64:(h + 1) * 64], lhsT=sm[:, hh * 128:(hh + 1) * 128], rhs=vt_[:, h * 64:(h + 1) * 64],
                    start=True, stop=False), [smk, f"vtok{l}"], ["Yp"])
                op("pe", lambda e, pq=pq, r0=r0, r1=r1, h=h: e.matmul(
                    Yp[:, h * 64:(h + 1) * 64], lhsT=qb_[r0:r1, pq, :], rhs=stb[r0:r1, pq, :], start=False, stop=True),
                   [f"qbT{l}", "stb"], ["Yp"])
            g, gk = nextG()
            op("pe", lambda e, g=g, pq=pq: e.matmul(
                g[:, 0:128], lhsT=kt_[:, pq * 128:(pq + 1) * 128], rhs=vt_[:, pq * 128:(pq + 1) * 128],
                start=True, stop=True), [f"ktok{l}", f"vtok{l}"], [gk])
            for hh in range(2):
                r0, r1 = hh * 64, hh * 64 + 64
                op("act", lambda e, g=g, r0=r0, r1=r1, pq=pq: e.activation(
                    out=dS[r0:r1, :], in_=g[r0:r1, r0:r1], func=AF.Copy, scale=C("GC", pq, pq + 1)[r0:r1, :]),
                   [gk, "CST"], ["dS"])
            op("pool", lambda e, pq=pq: e.tensor_scalar(out=stf[:, pq, :], in0=stf[:, pq, :], scalar1=C("GC", pq, pq + 1),
                                                        scalar2=None, op0=ALU.mult), ["stf", "CST"], ["stf"])
            op("pool", lambda e, pq=pq: e.tensor_tensor(out=stf[:, pq, :], in0=stf[:, pq, :], in1=dS[:], op=ALU.add),
               ["stf", "dS"], ["stf"])
            op("pool", lambda e, pq=pq: e.tensor_copy(out=stb[:, pq, :], in_=stf[:, pq, :]), ["stf"], ["stb"])
        for h in range(8):
            op("act", lambda e, h=h: e.activation(out=xb2[:, 0:64], in_=Yp[:, h * 64:(h + 1) * 64], func=AF.Identity,
                                                  accum_out=st[:, 16 + h:17 + h]), ["Yp"], ["xb2", "st16"])
            op("act", lambda e, h=h: e.activation(out=xb2[:, 64:128], in_=Yp[:, h * 64:(h + 1) * 64], func=AF.Square,
                                                  accum_out=st[:, 24 + h:25 + h]), ["Yp"], ["xb2", "st24"])
        op("pool", lambda e: e.tensor_scalar(out=st[:, 16:24], in0=st[:, 16:24], scalar1=1.0 / 64, scalar2=None,
                                             op0=ALU.mult), ["st16"], ["st16"])
        op("pool", lambda e: e.tensor_tensor(out=st[:, 32:40], in0=st[:, 16:24], in1=st[:, 16:24], op=ALU.mult),
           ["st16"], ["st32"])
        op("pool", lambda e: e.tensor_scalar(out=st[:, 24:32], in0=st[:, 24:32], scalar1=1.0 / 64, scalar2=GN_EPS,
                                             op0=ALU.mult, op1=ALU.add), ["st24"], ["st24"])
        op("pool", lambda e: e.tensor_tensor(out=st[:, 24:32], in0=st[:, 24:32], in1=st[:, 32:40], op=ALU.subtract),
           ["st24", "st32"], ["st24"])
        op("pool", lambda e: e.tensor_tensor(out=st[:, 24:32], in0=st[:, 24:32], in1=C("negh"), op=ALU.pow),
           ["st24", "CST"], ["st24"])
        op("pool", lambda e: e.tensor_tensor(out=st[:, 32:40], in0=st[:, 16:24], in1=st[:, 24:32], op=ALU.mult),
           ["st16", "st24"], ["st32"])
        op("pool", lambda e: e.tensor_scalar(out=st[:, 32:40], in0=st[:, 32:40], scalar1=-1.0, scalar2=None, op0=ALU.mult),
           ["st32"], ["st32"])
        yn, ynk = nextF()
        for h in range(8):
            op("act", lambda e, h=h, yn=yn: e.activation(out=yn[:, h * 64:(h + 1) * 64], in_=Yp[:, h * 64:(h + 1) * 64],
                                                         func=AF.Identity, scale=st[:, 24 + h:25 + h], bias=st[:, 32 + h:33 + h]),
               ["Yp", "st24", "st32"], [ynk])
        op("pool", lambda e, yn=yn: e.tensor_tensor(out=yn[:], in0=yn[:], in1=C("gnw"), op=ALU.mult), [ynk, "CST"], [ynk])
        op("pool", lambda e, yn=yn: e.tensor_tensor(out=yn[:], in0=yn[:], in1=C("gnb"), op=ALU.add), [ynk, "CST"], [ynk])
        ob, obk = nextB()
        op("pool", lambda e, yn=yn, ob=ob: e.tensor_tensor(out=ob[:], in0=yn[:], in1=sg_[:], op=ALU.mult), [ynk, f"sgt{l}"], [obk])
        transposes([ob[:, c * 128:(c + 1) * 128] for c in range(4)], obT[:, j, :, :], [f"obT{j}"], [obk])

    for seq in range(NSEQ):
        op("pool", lambda e: e.memset(stf[:], 0.0), [], ["stf"])
        op("pool", lambda e: e.memset(stb[:], 0.0), [], ["stb"])
        for gi in range(NG):
            tiles = [gi * GT + j for j in range(GT)]
            op("pool", lambda e: e.memset(st[:, 61:62], 0.0), [], RA + RB)
            for j, ti in enumerate(tiles):
                row0 = seq * S + ti * 128
                pos = ti * 128
                Xj = f"X{j}"
                P.dma("sp", lambda e, j=j, row0=row0: e.dma_start(out=X[:, j, :], in_=x_d[row0:row0 + 128, :]),
                      f"x{j}", writes=[Xj])
                P.dma("sp", lambda e, ti=ti: e.dma_start(out=ROT[:], in_=rot_d[ti]), "rot", writes=["ROT"])
                op("act", lambda e, j=j: e.activation(out=xb[:], in_=X[:, j, :], func=AF.Square, accum_out=st[:, 0:1]),
                   [Xj], ["xb", "st0"])
                rstd_from_ss(0, 1.0 / D_MODEL, RMS_EPS)
                op("act", lambda e, j=j: e.activation(out=xb[:], in_=X[:, j, :], func=AF.Copy, scale=st[:, 0:1]),
                   [Xj, "st0"], ["xb"])
                transposes([xb[:, k * 128:(k + 1) * 128] for k in range(8)], xT[:, :, j * 128:(j + 1) * 128],
                           [f"xT{j}"], ["xb"])
                zbanks = [(Gp[0], "G0"), (Gp[1], "G1"), (Gp[2], "G2"), (Gp[3], "G3"), (PV[0], "PV0"), (PV[1], "PV1")]
                for b in range(6):
                    w_ = 512 if b < 5 else IN_W - 2560
                    g, gk = zbanks[b]
                    for k in range(8):
                        op("pe", lambda e, g=g, k=k, j=j, b=b, w_=w_: e.matmul(
                            g[:, 0:w_], lhsT=xT[:, k, j * 128:(j + 1) * 128], rhs=Win[:, k, b * 512:b * 512 + w_],
                            start=(k == 0), stop=(k == 7)), [f"xT{j}", "Win"], [gk])
                for b in range(6):
                    g, gk = zbanks[b]
                    if b == 0:
                        f1, f1k = nextF()
                        op("act", lambda e, g=g, f1=f1: e.activation(out=f1[:], in_=g[:], func=AF.Square), [gk], [f1k])
                        op("dve", lambda e, f1=f1: e.tensor_reduce(out=st[:, 8:16], in_=f1[:].rearrange("p (h d) -> p h d", h=8),
                                                                   axis=AX.X, op=ALU.add), [f1k], ["st8"])
                        op("dve", lambda e: e.tensor_scalar(out=st[:, 8:16], in0=st[:, 8:16], scalar1=1.0 / 64, scalar2=RMS_EPS,
                                                            op0=ALU.mult, op1=ALU.add), ["st8"], ["st8"])
                        op("pool", lambda e: e.tensor_tensor(out=st[:, 8:16], in0=st[:, 8:16], in1=C("negh"), op=ALU.pow),
                           ["st8", "CST"], ["st8"])
                        f2, f2k = nextF()
                        op("dve", lambda e, g=g, f2=f2: e.tensor_tensor(
                            out=f2[:].rearrange("p (h d) -> p h d", h=8), in0=g[:].rearrange("p (h d) -> p h d", h=8),
                            in1=st[:, 8:16].unsqueeze(2).broadcast_to([128, 8, 64]), op=ALU.mult), [gk, "st8"], [f2k])
                        b1, b1k = nextB()
                        op("pool", lambda e, f2=f2, b1=b1: e.tensor_tensor(
                            out=b1[:].rearrange("p (h d) -> p h d", h=8), in0=f2[:].rearrange("p (h d) -> p h d", h=8),
                            in1=C("qnw").unsqueeze(1).broadcast_to([128, 8, 64]), op=ALU.mult), [f2k, "CST"], [b1k])
                        transposes([b1[:, h * 64:(h + 1) * 64] for h in range(8)],
                                   qT[:, j, :].rearrange("p (a b) -> p a b", b=128), [f"qT{j}"], [b1k], cols=64)
                    elif b in (1, 2):
                        fa, fak = nextF()
                        fb, fbk = nextF()
                        g3 = g[:].rearrange("p (h d) -> p h d", h=8)
                        op("dve", lambda e, fa=fa, g3=g3: e.tensor_tensor(
                            out=fa[:].rearrange("p (h d) -> p h d", h=8), in0=g3,
                            in1=ROT[:, 0:64].unsqueeze(1).broadcast_to([128, 8, 64]), op=ALU.mult), [gk, "ROT"], [fak])
                        op("dve", lambda e, fb=fb, g3=g3: e.tensor_tensor(
                            out=fb[:].rearrange("p (h d) -> p h d", h=8)[:, :, 0:32], in0=g3[:, :, 32:64],
                            in1=ROT[:, 64:96].unsqueeze(1).broadcast_to([128, 8, 32]), op=ALU.mult), [gk, "ROT"], [fbk])
                        op("dve", lambda e, fb=fb, g3=g3: e.tensor_tensor(
                            out=fb[:].rearrange("p (h d) -> p h d", h=8)[:, :, 32:64], in0=g3[:, :, 0:32],
                            in1=ROT[:, 96:128].unsqueeze(1).broadcast_to([128, 8, 32]), op=ALU.mult), [gk, "ROT", fbk], [fbk])
                        rb_, rbk = nextB()
                        op("pool", lambda e, fa=fa, fb=fb, rb_=rb_: e.tensor_tensor(out=rb_[:], in0=fa[:], in1=fb[:], op=ALU.add),
                           [fak, fbk], [rbk])
                        dstT, dk_, dcn = (qbTL[j % 2], f"qbT{j % 2}", "DQ") if b == 1 else (kbTL[j % 2], f"kbT{j % 2}", "DK")
                        for pq in range(4):
                            op("pe", lambda e, pq=pq, rb_=rb_: e.transpose(out=Tp[:, pq * 128:(pq + 1) * 128],
                                                                          in_=rb_[:, pq * 128:(pq + 1) * 128], identity=identb[:]),
                               [rbk, "identb"], ["Tp"])
                        op("dve", lambda e, dstT=dstT, dcn=dcn: e.tensor_tensor(
                            out=dstT[:].rearrange("p a b -> p (a b)"), in0=Tp[:, 0:512], in1=C(dcn), op=ALU.mult),
                           ["Tp", "CST"], [dk_])
                        if b == 2:
                            kt_, ktk = ktokL[j % 2], f"ktok{j % 2}"
                            op("pool", lambda e, rb_=rb_, kt_=kt_: e.tensor_tensor(
                                out=kt_[:].rearrange("p (h d) -> p h d", h=8), in0=rb_[:].rearrange("p (h d) -> p h d", h=8),
                                in1=C("DKtok").unsqueeze(2).broadcast_to([128, 8, 64]), op=ALU.mult), [rbk, "CST"], [ktk])
                    elif b == 3:
                        vt_, vtk = vtokL[j % 2], f"vtok{j % 2}"
                        op("act", lambda e, g=g, vt_=vt_: e.copy(out=vt_[:], in_=g[:]), [gk], [vtk])
                    elif b == 4:
                        th, thk = nextF()
                        op("act", lambda e, g=g, th=th: e.activation(out=th[:], in_=g[:], func=AF.Tanh, scale=0.5), [gk], [thk])
                        op("dve", lambda e, g=g, th=th, j=j: e.scalar_tensor_tensor(out=sgtL[j % 2][:], in0=th[:], scalar=1.0, in1=g[:],
                                                                                    op0=ALU.add, op1=ALU.mult), [gk, thk], [f"sgt{j % 2}"])
                    else:
                        op("act", lambda e, g=g: e.activation(out=xb[:, 0:128], in_=g[:, 0:128], func=AF.Square,
                                                              accum_out=st[:, 1:2]), [gk], ["xb", "st1"])
                        rstd_from_ss(1, 1.0 / 128, RMS_EPS)
                        cb_, cbk = nextB()
                        op("act", lambda e, g=g, cb_=cb_: e.activation(out=cb_[:, 0:128], in_=g[:, 0:128], func=AF.Copy,
                                                                       scale=st[:, 1:2]), [gk, "st1"], [cbk])
                        op("act", lambda e, g=g, cb_=cb_: e.copy(out=cb_[:, 128:256], in_=g[:, 128:256]), [gk], [cbk])
                        op("act", lambda e, g=g, cb_=cb_: e.copy(
                            out=cb_[:, 256:384].rearrange("p (a b) -> p a b", a=4),
                            in_=g[:, 256:288].unsqueeze(1).broadcast_to([128, 4, 32])), [gk], [cbk])
                        op("dve", lambda e, g=g, j=j: e.tensor_scalar(out=sgn[:, j, :], in0=g[:, 288:292], scalar1=0.0,
                                                                      scalar2=-0.5, op0=ALU.is_ge, op1=ALU.add), [gk], [f"sgn{j}"])
                        op("dve", lambda e, g=g, j=j: e.scalar_tensor_tensor(out=aw[:, j, :], in0=g[:, 288:292], scalar=2.0,
                                                                             in1=sgn[:, j, :], op0=ALU.mult, op1=ALU.mult),
                           [gk, f"sgn{j}"], [f"aw{j}"])
                        op("pe", lambda e, cb_=cb_: e.transpose(out=Tp[:, 0:128], in_=cb_[:, 0:128], identity=identb[:]),
                           [cbk, "identb"], ["Tp"])
                        op("act", lambda e: e.copy(out=cT[:], in_=Tp[:, 0:128]), ["Tp"], ["cT"])
                        g2, g2k = nextG()
                        op("pe", lambda e, g2=g2: e.matmul(g2[:, 0:128], lhsT=cT[:], rhs=Wukv[:], start=True, stop=True),
                           ["cT", "Wukv"], [g2k])
                        op("act", lambda e, g2=g2: e.activation(out=xb[:, 0:64], in_=g2[:, 0:64], func=AF.Square,
                                                                accum_out=st[:, 2:3]), [g2k], ["xb", "st2"])
                        rstd_from_ss(2, 1.0 / 64, RMS_EPS)
                        op("dve", lambda e, g2=g2, cb_=cb_: e.scalar_tensor_tensor(
                            out=cb_[:, 384:448], in0=g2[:, 0:64], scalar=st[:, 2:3], in1=C("knw"), op0=ALU.mult, op1=ALU.mult),
                           [g2k, "st2", "CST"], [cbk])
                        op("act", lambda e, g2=g2, ti=ti: e.copy(out=vA[:, ti, 0:64], in_=g2[:, 64:128]), [g2k], [f"vA{ti}"])
                        op("pe", lambda e, cb_=cb_: e.transpose(out=Tp[0:64, 0:128], in_=cb_[:, 384:448], identity=identb[:]),
                           [cbk, "identb"], ["Tp"])
                        op("act", lambda e, pos=pos: e.copy(out=kT[:, pos:pos + 128], in_=Tp[0:64, 0:128]), ["Tp"], [f"kT{ti}"])
                        op("pe", lambda e, cb_=cb_: e.transpose(out=Tp[:, 0:128], in_=cb_[:, 128:256], identity=identb[:]),
                           [cbk, "identb"], ["Tp"])
                        op("dve", lambda e, j=j: e.tensor_tensor(
                            out=iqTb[:, j, :, :], in0=Tp[:, 0:128].unsqueeze(1).broadcast_to([128, 4, 128]),
                            in1=C("BM").rearrange("p (a b) -> p a b", a=4), op=ALU.mult), ["Tp", "CST"], [f"iqTb{j}"])
                        op("pe", lambda e, cb_=cb_: e.transpose(out=Tp[:, 0:128], in_=cb_[:, 256:384], identity=identb[:]),
                           [cbk, "identb"], ["Tp"])
                        op("act", lambda e, pos=pos: e.copy(out=ikT4[:, pos:pos + 128], in_=Tp[:, 0:128]), ["Tp"], [f"ik{ti}"])

                N = (ti + 1) * 128
                if N > KTOP:
                    ls = j % LS
                    for kb in range((N + 511) // 512):
                        k0 = kb * 512
                        w_ = min(512, N - k0)
                        for h in range(4):
                            g, gk = nextG()
                            op("pe", lambda e, g=g, j=j, h=h, k0=k0, w_=w_: e.matmul(
                                g[:, 0:w_], lhsT=iqTb[:, j, h, :], rhs=ikT4[:, k0:k0 + w_], start=True, stop=True),
                               [f"iqTb{j}"] + [f"ik{q}" for q in range(k0 // 128, (k0 + w_) // 128)], [gk])
                            op("act", lambda e, g=g, j=j, h=h, w_=w_: e.activation(
                                out=g[:, 0:w_], in_=g[:, 0:w_], func=AF.Relu, scale=aw[:, j, h:h + 1]), [gk, f"aw{j}"], [gk])
                            if h == 0:
                                op("dve", lambda e, g=g, j=j, ls=ls, k0=k0, w_=w_: e.scalar_tensor_tensor(
                                    out=sc[:, ls, k0:k0 + w_], in0=g[:, 0:w_], scalar=sgn[:, j, 0:1], in1=TB[:, 0:w_],
                                    op0=ALU.mult, op1=ALU.add), [gk, f"sgn{j}", "TB"] + RA, [f"sc{ls}"])
                            else:
                                op("dve", lambda e, g=g, j=j, ls=ls, k0=k0, w_=w_, h=h: e.scalar_tensor_tensor(
                                    out=sc[:, ls, k0:k0 + w_], in0=g[:, 0:w_], scalar=sgn[:, j, h:h + 1], in1=sc[:, ls, k0:k0 + w_],
                                    op0=ALU.mult, op1=ALU.add), [gk, f"sgn{j}", f"sc{ls}"] + RA, [f"sc{ls}"])
                        if kb > 0:
                            op("pool", lambda e, ls=ls, k0=k0, w_=w_, kb=kb: e.tensor_scalar(
                                out=sc[:, ls, k0:k0 + w_], in0=sc[:, ls, k0:k0 + w_], scalar1=-1e-30 * 512 * kb, scalar2=None,
                                op0=ALU.add), [f"sc{ls}"] + RA, [f"sc{ls}"])

                if (j + 1) % LS == 0:
                    sub = list(range(j + 1 - LS, j + 1))
                    bl = [(jj, tiles[jj]) for jj in sub if (tiles[jj] + 1) * 128 > KTOP]
                    nb_ = len(bl)
                    MX, MN, RNG, MID, CNT, PMH, THR = 0, LS, 2 * LS, 3 * LS, 4 * LS, 5 * LS, 6 * LS
                    STP = 8 * LS
                    if nb_ > 0:
                        for (jj, tt) in bl:
                            ls = jj % LS
                            N = (tt + 1) * 128
                            op("dve", lambda e, ls=ls, N=N: e.tensor_reduce(out=bs[:, MX + ls:MX + ls + 1], in_=sc[:, ls, 0:N],
                                                                             axis=AX.X, op=ALU.max), [f"sc{ls}"], ["bs"])
                            op("dve", lambda e, ls=ls, N=N: e.tensor_reduce(out=bs[:, MN + ls:MN + ls + 1], in_=sc[:, ls, 0:N],
                                                                             axis=AX.X, op=ALU.min), [f"sc{ls}"], ["bs"])
                            op("dve", lambda e, ls=ls, N=N: e.memset(sc[0:64, ls, N - 64:N], NEG), [f"sc{ls}"], [f"sc{ls}"])
                        if nb_ < LS:
                            for ls in range(LS):
                                if ls not in [jj % LS for (jj, _) in bl]:
                                    op("dve", lambda e, ls=ls: e.memset(bs[:, MX + ls:MX + ls + 1], 1.0), [], ["bs"])
                                    op("dve", lambda e, ls=ls: e.memset(bs[:, MN + ls:MN + ls + 1], 0.0), [], ["bs"])
                        op("dve", lambda e: e.tensor_tensor(out=bs[:, RNG:RNG + LS], in0=bs[:, MX:MX + LS], in1=bs[:, MN:MN + LS],
                                                            op=ALU.subtract), ["bs"], ["bs"])
                        op("dve", lambda e: e.scalar_tensor_tensor(out=bs[:, MID:MID + LS], in0=bs[:, RNG:RNG + LS], scalar=0.5,
                                                                   in1=bs[:, MN:MN + LS], op0=ALU.mult, op1=ALU.add), ["bs"], ["bs"])
                        op("dve", lambda e: e.tensor_tensor(
                            out=bs[:, STP:STP + LS * KIT].rearrange("p (a k) -> p a k", a=LS),
                            in0=bs[:, RNG:RNG + LS].unsqueeze(2).broadcast_to([128, LS, KIT]),
                            in1=C("pw", 0, KIT).unsqueeze(1).broadcast_to([128, LS, KIT]), op=ALU.mult), ["bs", "CST"], ["bs"])
                        stp3 = bs[:, STP:STP + LS * KIT].rearrange("p (a k) -> p a k", a=LS)
                        for it in range(KIT):
                            for (jj, tt) in bl:
                                ls = jj % LS
                                N = (tt + 1) * 128
                                op("dve", lambda e, ls=ls, N=N: e.tensor_scalar(
                                    out=msk[:, 0:N], in0=sc[:, ls, 0:N], scalar1=bs[:, MID + ls:MID + ls + 1], scalar2=None,
                                    op0=ALU.is_ge, op1=ALU.add, accum_out=bs[:, CNT + ls:CNT + ls + 1]),
                                   [f"sc{ls}", "bs"] + RA, ["msk", "bs"])
                            op("dve", lambda e: e.tensor_scalar(out=bs[:, PMH:PMH + LS], in0=bs[:, CNT:CNT + LS],
                                                                scalar1=float(KTOP) - 0.5, scalar2=-0.5, op0=ALU.is_ge, op1=ALU.add),
                               ["bs"], ["bs"])
                            op("dve", lambda e, it=it: e.tensor_tensor(out=bs[:, PMH:PMH + LS], in0=bs[:, PMH:PMH + LS],
                                                                       in1=stp3[:, :, it], op=ALU.mult), ["bs"], ["bs"])
                            op("dve", lambda e: e.tensor_tensor(out=bs[:, MID:MID + LS], in0=bs[:, MID:MID + LS],
                                                                in1=bs[:, PMH:PMH + LS], op=ALU.add), ["bs"], ["bs"])
                        op("dve", lambda e: e.scalar_tensor_tensor(out=bs[:, THR:THR + LS], in0=bs[:, RNG:RNG + LS],
                                                                   scalar=-(2.0 ** -(KIT + 1)), in1=bs[:, MID:MID + LS],
                                                                   op0=ALU.mult, op1=ALU.add), ["bs"], ["bs"])
                    for jj in sub:
                        retention(jj, tiles[jj])
                    for jj in sub:
                        tt = tiles[jj]
                        nk = tt + 1
                        N = nk * 128
                        ls = jj % LS
                        bis = N > KTOP
                        if bis:
                            op("dve", lambda e, ls=ls, N=N: e.tensor_scalar(
                                out=msk[:, 0:N], in0=sc[:, ls, 0:N], scalar1=bs[:, THR + ls:THR + ls + 1], scalar2=None,
                                op0=ALU.is_ge), [f"sc{ls}", "bs"] + RA, ["msk"])
                            for c0 in range(0, nk, 8):
                                cn = min(8, nk - c0)
                                transposes([msk[:, (c0 + i) * 128:(c0 + i + 1) * 128] for i in range(cn)],
                                           mskT[:, c0:c0 + cn, :], ["mskT"], ["msk"] + RA)
                        steps = [(kt, hf) for kt in range(nk) for hf in range(2)]
                        pm_of = {}

                        def emit_st(si, jj=jj, tt=tt, nk=nk, bis=bis):
                            kt, hf = steps[si]
                            if bis:
                                mk_ap, mkk = mskT[:, kt, :], "mskT"
                            elif kt == nk - 1:
                                mk_ap, mkk = LASTM[:], "LASTM"
                            else:
                                mk_ap, mkk = ONESM[:], "ONESM"
                            g, gk = nextG()
                            op("pe", lambda e, g=g, kt=kt, jj=jj, hf=hf: e.matmul(
                                g[:], lhsT=kT[:, kt * 128:(kt + 1) * 128], rhs=qT[:, jj, hf * 512:(hf + 1) * 512],
                                start=True, stop=True), [f"kT{kt}", f"qT{jj}"], [gk])
                            ei_ = si % NE
                            E_, Ek = Eb[ei_], f"E{ei_}"
                            if kt >= tt - 1:
                                kd = 0 if kt == tt else 1
                                op("dve", lambda e, g=g, kd=kd, hf=hf: e.scalar_tensor_tensor(
                                    out=g[:], in0=g[:], scalar=0.125, in1=Bp[:, kd, hf * 512:(hf + 1) * 512],
                                    op0=ALU.mult, op1=ALU.add), [gk, f"Bp{kd}"], [gk])
                                op("act", lambda e, g=g, E_=E_: e.activation(out=E_[:], in_=g[:], func=AF.Exp), [gk], [Ek])
                            else:
                                op("act", lambda e, g=g, E_=E_: e.activation(out=E_[:], in_=g[:], func=AF.Exp, scale=0.125),
                                   [gk], [Ek])
                            Pm_, Pk = Pm[ei_], f"Pm{ei_}"
                            op("dve" if hf == 0 else "pool", lambda e, E_=E_, Pm_=Pm_, mk_ap=mk_ap: e.tensor_tensor(
                                out=Pm_[:].rearrange("p (h t) -> p h t", h=4), in0=E_[:].rearrange("p (h t) -> p h t", h=4),
                                in1=mk_ap.unsqueeze(1).broadcast_to([128, 4, 128]), op=ALU.mult), [Ek, mkk] + RA, [Pk])
                            pm_of[si] = (Pm_, Pk)

                        def emit_pv(si, nk=nk):
                            kt, hf = steps[si]
                            Pm_, Pk = pm_of[si]
                            for h4 in range(4):
                                op("pe", lambda e, Pm_=Pm_, h4=h4, hf=hf, kt=kt, nk=nk: e.matmul(
                                    PV[hf][:, h4 * 65:(h4 + 1) * 65], lhsT=Pm_[:, h4 * 128:(h4 + 1) * 128], rhs=vA[:, kt, :],
                                    start=(kt == 0 and h4 == 0), stop=(kt == nk - 1 and h4 == 3), skip_group_check=True),
                                   [Pk, f"vA{kt}"], [f"PV{hf}"])

                        LOOK = 2
                        ns = len(steps)
                        for si in range(min(LOOK, ns)):
                            emit_st(si)
                        for si in range(ns):
                            if si + LOOK < ns:
                                emit_st(si + LOOK)
                            emit_pv(si)
                        oa, oak = nextB()
                        for hf in range(2):
                            pv3 = PV[hf][:, 0:260].rearrange("p (h c) -> p h c", h=4)
                            op("dve", lambda e, pv3=pv3, hf=hf: e.reciprocal(out=st[:, 40 + hf * 4:44 + hf * 4], in_=pv3[:, :, 64]),
                               [f"PV{hf}"], [f"st4{hf}"])
                            op("dve", lambda e, pv3=pv3, hf=hf, oa=oa: e.tensor_tensor(
                                out=oa[:, hf * 256:(hf + 1) * 256].rearrange("p (h d) -> p h d", h=4), in0=pv3[:, :, 0:64],
                                in1=st[:, 40 + hf * 4:44 + hf * 4].unsqueeze(2).broadcast_to([128, 4, 64]), op=ALU.mult),
                               [f"PV{hf}", f"st4{hf}"], [oak])
                        transposes([oa[:, c * 128:(c + 1) * 128] for c in range(4)], catT[:, 0:4, :], ["catT"], [oak])
                        op("pool", lambda e, jj=jj: e.tensor_copy(out=catT[:, 4:8, :], in_=obT[:, jj, :, :]), [f"obT{jj}"], ["catT"])
                        for nbk in range(2):
                            g, gk = nextG()
                            for c in range(8):
                                op("pe", lambda e, g=g, c=c, nbk=nbk: e.matmul(
                                    g[:], lhsT=catT[:, c, :], rhs=Wout[:, c, nbk * 512:(nbk + 1) * 512], start=(c == 0), stop=(c == 7)),
                                   ["catT", "Wout"], [gk])
                            op("dve", lambda e, g=g, jj=jj, nbk=nbk: e.tensor_tensor(
                                out=X[:, jj, nbk * 512:(nbk + 1) * 512], in0=g[:], in1=X[:, jj, nbk * 512:(nbk + 1) * 512], op=ALU.add),
                               [gk, f"X{jj}"], [f"X{jj}"])
                    for jj in sub:
                        op("act", lambda e, jj=jj: e.activation(out=xb[:], in_=X[:, jj, :], func=AF.Square, accum_out=st[:, 3:4]),
                           [f"X{jj}"], ["xb", "st3"])
                        rstd_from_ss(3, 1.0 / D_MODEL, RMS_EPS)
                        op("act", lambda e, jj=jj: e.activation(out=xb[:], in_=X[:, jj, :], func=AF.Copy, scale=st[:, 3:4]),
                           [f"X{jj}", "st3"], ["xb"])
                        transposes([xb[:, k * 128:(k + 1) * 128] for k in range(8)], xT[:, :, jj * 128:(jj + 1) * 128],
                                   [f"xT{jj}"], ["xb"])
            op("pool", lambda e: e.memset(st[:, 62:63], 0.0), [], RA + RB)
            TW = GT * 128
            for c in range(NHC):
                s_ = c % 2
                P.dma("sp", lambda e, c=c, s_=s_: e.dma_start(out=wgs[s_], in_=wg_s[:, c]), f"wg{s_}",
                      reads=["wg_s"] + RB, writes=[f"wg{s_}"])
                P.dma("sp", lambda e, c=c, s_=s_: e.dma_start(out=wus[s_], in_=wu_s[:, c]), f"wu{s_}",
                      reads=["wu_s"] + RB, writes=[f"wu{s_}"])
                gg, ggk = nextG()
                gu, guk = nextG()
                for k in range(8):
                    op("pe", lambda e, gg=gg, k=k, s_=s_: e.matmul(gg[:, 0:TW], lhsT=wgs[s_][:, k, :], rhs=xT[:, k, :],
                                                                   start=(k == 0), stop=(k == 7)),
                       [f"wg{s_}"] + RB + [f"xT{q}" for q in range(GT)], [ggk])
                for k in range(8):
                    op("pe", lambda e, gu=gu, k=k, s_=s_: e.matmul(gu[:, 0:TW], lhsT=wus[s_][:, k, :], rhs=xT[:, k, :],
                                                                   start=(k == 0), stop=(k == 7)),
                       [f"wu{s_}"] + RB + [f"xT{q}" for q in range(GT)], [guk])
                sl, slk = nextF()
                op("act", lambda e, gg=gg, sl=sl: e.activation(out=sl[:, 0:TW], in_=gg[:, 0:TW], func=AF.Silu), [ggk], [slk])
                op("dve", lambda e, gu=gu, sl=sl, c=c: e.tensor_tensor(out=hT[:, c, :], in0=gu[:, 0:TW], in1=sl[:, 0:TW], op=ALU.mult),
                   [guk, slk] + RB, ["hT"])
            TpF = Tp[:].bitcast(F32)
            acc_banks = [(Gp[0][:], "G0"), (Gp[1][:], "G1"), (Gp[2][:], "G2"), (Gp[3][:], "G3"),
                         (PV[0][:], "PV0"), (PV[1][:], "PV1"), (Yp[:], "Yp"), (TpF, "Tp")]
            accs = {}
            for t_ in range(GT):
                for nbk in range(2):
                    accs[(t_, nbk)] = acc_banks[t_ * 2 + nbk]
            for c in range(NHC):
                s_ = c % NWD
                P.dma("sp", lambda e, c=c, s_=s_: e.dma_start(out=wds[s_], in_=wd_s[c]), f"wd{s_}",
                      reads=["wd_s"] + RB, writes=[f"wd{s_}"])
                for t_ in range(GT):
                    for nbk in range(2):
                        g, gk = accs[(t_, nbk)]
                        op("pe", lambda e, g=g, c=c, t_=t_, nbk=nbk, s_=s_: e.matmul(
                            g, lhsT=hT[:, c, t_ * 128:(t_ + 1) * 128], rhs=wds[s_][:, nbk * 512:(nbk + 1) * 512],
                            start=(c == 0), stop=(c == NHC - 1)), ["hT", f"wd{s_}"] + RB, [gk])
            for t_ in range(GT):
                for nbk in range(2):
                    g, gk = accs[(t_, nbk)]
                    op("dve", lambda e, g=g, t_=t_, nbk=nbk: e.tensor_tensor(
                        out=X[:, t_, nbk * 512:(nbk + 1) * 512], in0=g, in1=X[:, t_, nbk * 512:(nbk + 1) * 512], op=ALU.add),
                       [gk, f"X{t_}"], [f"X{t_}"])
                row0 = seq * S + tiles[t_] * 128
                P.dma("sp", lambda e, t_=t_, row0=row0: e.dma_start(out=y_d[row0:row0 + 128, :], in_=X[:, t_, :]),
                      f"y{t_}", reads=[f"X{t_}"])
    P.emit()
    return P


C_LAYOUT = [("ident", 128), ("n1w", 8), ("n2w", 8), ("kvnw", 1), ("qnw", 64), ("knw", 64),
            ("gnw", 512), ("gnb", 512), ("wuk", 64), ("wuv", 64), ("DQ", 512), ("DK", 512),
            ("DKtok", 8), ("DMASK", 128), ("GC", 4), ("BM", 512), ("LASTM", 128), ("pw", 32),
            ("negh", 8)]


def _t5_bucket_np(rel):
    import jax
    import jax.numpy as jnp
    cpu = jax.devices("cpu")[0]
    with jax.default_device(cpu):
        rel = jnp.asarray(rel, dtype=jnp.int32)
        half, max_exact, max_distance = 16, 8, 128
        ret = jnp.where(rel > 0, half, 0)
        n = jnp.abs(rel)
        nf = jnp.maximum(n, 1).astype(jnp.float32)
        large = max_exact + (jnp.log(nf / max_exact) / math.log(max_distance / max_exact)
                             * (half - max_exact)).astype(jnp.int32)
        large = jnp.minimum(large, half - 1)
        out = ret + jnp.where(n < max_exact, n, large)
        return np.asarray(out)


def _rot_tables(S):
    import jax
    import jax.numpy as jnp
    cpu = jax.devices("cpu")[0]
    with jax.default_device(cpu):
        half = 32
        inv = 10000.0 ** (-jnp.arange(half, dtype=jnp.float32) / half)
        ang = jnp.arange(S).astype(jnp.float32)[:, None] * inv[None, :]
        cos = np.asarray(jnp.cos(ang))
        sin = np.asarray(jnp.sin(ang))
    rot = np.concatenate([cos, cos, -sin, sin], axis=1).astype(np.float32)
    return np.ascontiguousarray(rot.reshape(S // 128, 128, 128))


def _host_consts(inp, S):
    f32 = np.float32
    secs = {}
    secs["ident"] = np.eye(128, dtype=f32)
    secs["n1w"] = np.ascontiguousarray(inp["norm1_w"][0].reshape(8, 128).T)
    secs["n2w"] = np.ascontiguousarray(inp["norm2_w"][0].reshape(8, 128).T)
    secs["kvnw"] = inp["kv_norm_w"][0].reshape(128, 1)
    secs["qnw"] = np.broadcast_to(inp["q_norm_w"][0][None, :], (128, 64))
    secs["knw"] = np.broadcast_to(inp["k_norm_w"][0][None, :], (128, 64))
    secs["gnw"] = np.broadcast_to(inp["ret_gn_w"][0][None, :], (128, 512))
    secs["gnb"] = np.broadcast_to(inp["ret_gn_b"][0][None, :], (128, 512))
    secs["wuk"] = inp["w_uk"][0]
    secs["wuv"] = inp["w_uv"][0]
    hh = np.arange(8, dtype=np.float64)
    log_g = np.log1p(-(2.0 ** (-5.0 - hh)))
    t = np.arange(128, dtype=np.float64)
    row_h = np.arange(128) // 64
    DQ = np.zeros((128, 4, 128)); DK = np.zeros((128, 4, 128)); GC = np.zeros((128, 4))
    for pq in range(4):
        lg = log_g[2 * pq + row_h]
        DQ[:, pq, :] = np.exp((t[None, :] + 1.0) * lg[:, None])
        DK[:, pq, :] = np.exp(-(t[None, :] + 1.0) * lg[:, None]) * 0.125
        GC[:, pq] = np.exp(128.0 * lg)
    secs["DQ"] = DQ.reshape(128, 512)
    secs["DK"] = DK.reshape(128, 512)
    secs["DKtok"] = np.exp(-(t[:, None] + 1.0) * log_g[None, :]) * 0.125
    jj = np.arange(128)
    secs["DMASK"] = (jj[None, :] >= jj[:, None]).astype(f32)
    secs["GC"] = GC
    BM = np.zeros((128, 4, 128), f32)
    for h in range(4):
        BM[h * 32:(h + 1) * 32, h, :] = 1.0
    secs["BM"] = BM.reshape(128, 512)
    LM = np.ones((128, 128), f32)
    LM[64:, :64] = 0.0
    secs["LASTM"] = LM
    secs["pw"] = np.broadcast_to((2.0 ** -(np.arange(32, dtype=np.float64) + 1.0))[None, :], (128, 32))
    secs["negh"] = np.full((128, 8), -0.5)
    cst = np.zeros((128, CW), f32)
    o = 0
    for nm, w in C_LAYOUT:
        cst[:, o:o + w] = np.asarray(secs[nm], dtype=f32)
        o += w
    s_ = np.arange(128)[:, None]
    t_ = np.arange(128)[None, :]
    rb = inp["rel_bias"]
    b0 = rb[_t5_bucket_np(s_ - t_)]
    b1 = rb[_t5_bucket_np(s_ - 128 - t_)]
    cb = rb[_t5_bucket_np(np.full((128, 128), -1000))]
    bias_t = np.stack([np.transpose(b, (0, 2, 1)).reshape(128, 1024) for b in (b0, b1, cb)]).astype(f32)
    tb = np.broadcast_to((-1e-30 * np.arange(512, dtype=np.float64)).astype(f32)[None, :], (128, 512))
    return cst, np.ascontiguousarray(bias_t), np.ascontiguousarray(tb), _rot_tables(S)


_NC_CACHE = {}


def run_cores(inp, xs, S, NSEQ, KTOP):
    key = (S, NSEQ, KTOP)
    nc = bass.Bass("TRN2", target_bir_lowering=False)
    build(nc, S, NSEQ, KTOP)
    cst, bias_t, tb, rot = _host_consts(inp, S)
    base = {
        "w_in": np.ascontiguousarray(inp["w_in"][0]), "w_out": np.ascontiguousarray(inp["w_out"][0]),
        "w_gate": np.ascontiguousarray(inp["w_gate"][0]), "w_up": np.ascontiguousarray(inp["w_up"][0]),
        "w_down": np.ascontiguousarray(inp["w_down"][0]), "cst": cst, "bias_t": bias_t, "tb": tb, "rot": rot,
    }
    in_maps = [dict(base, x=np.ascontiguousarray(x)) for x in xs]
    res = run_bass_kernel_spmd(nc, in_maps, core_ids=list(range(len(xs))))
    return [r["y"] for r in res.results]


def kernel(**inputs):
    inp = {k: np.asarray(v) for k, v in inputs.items()}
    x = inp["x"]
    B, S, D = x.shape
    ncores = 8
    nseq = B // ncores
    xs = [x[c * nseq:(c + 1) * nseq].reshape(nseq * S, D) for c in range(ncores)]
    ys = run_cores(inp, xs, S, nseq, min(256, S // 4))
    return np.concatenate([y.reshape(nseq, S, D) for y in ys], axis=0).astype(np.float32)
```
